# Optimizing a Trainium2 kernel written in Bass

```python
import math
import jax
import jax.numpy as jnp
from jax import lax
import numpy as np

D_MODEL = 1024
BATCH = 2
SEQ = 8192
DEPTH = 2

MEM_LEN = 256
N_BRANCH = 4
BRANCH_WIDTH = 256
BLOCK_Q = 128
EPS = 1e-5
MAX_POS_OFFSET = 4096
NEG_BIG = -1e30

MLA_HEADS = 4
MLA_Q_LORA = 256
MLA_KV_LORA = 128
MLA_NOPE = 64
MLA_ROPE = 32
MLA_V = 64
ROPE_THETA = 10000.0

SWA_HEADS = 4
SWA_KV_HEADS = 2
SWA_HEAD_DIM = 64
SWA_WINDOW = 128

HGRN_HEADS = 4
HGRN_DK = 64
HGRN_DV = 64
HGRN_CHUNK = 64

SB_HEADS = 4
SB_HEAD_DIM = 64

REL_BUCKETS = 32
REL_MAX_DIST = 128

XA_HEADS = 4
XA_HEAD_DIM = 64

F_DENSE = 2816
N_EXPERTS = 8
TOP_K = 2
F_EXPERT = 3584
MOE_BLOCK = 256

DEEPNORM_ALPHA = (2 * DEPTH) ** 0.25
DEEPNORM_BETA = (8 * DEPTH) ** -0.25
N_DENSE = (DEPTH + 1) // 2
N_MOE = DEPTH // 2

IN_SPLITS = (
    ('mla_cq', MLA_Q_LORA),
    ('mla_ckv', MLA_KV_LORA),
    ('mla_kr', MLA_ROPE),
    ('swa_q', SWA_HEADS * SWA_HEAD_DIM),
    ('swa_k', SWA_KV_HEADS * SWA_HEAD_DIM),
    ('swa_v', SWA_KV_HEADS * SWA_HEAD_DIM),
    ('hgrn_q', HGRN_HEADS * HGRN_DK),
    ('hgrn_f', HGRN_HEADS * HGRN_DK),
    ('hgrn_i', HGRN_HEADS * HGRN_DV),
    ('hgrn_g', HGRN_HEADS * HGRN_DV),
    ('sb_q', SB_HEADS * SB_HEAD_DIM),
    ('sb_k', SB_HEADS * SB_HEAD_DIM),
    ('sb_v', SB_HEADS * SB_HEAD_DIM),
    ('gates', N_BRANCH * D_MODEL),
)
IN_COLS = sum(width for _, width in IN_SPLITS)

kernel_name = 'hybrid_mla_swa_hgrn2_stickbreak_moe_block'

F32 = jnp.float32


def _split_cols(h):
    out = {}
    off = 0
    for name, width in IN_SPLITS:
        out[name] = h[..., off:off + width]
        off += width
    return out


def _layernorm(x, g, b):
    xf = x.astype(F32)
    mu = jnp.mean(xf, axis=-1, keepdims=True)
    xc = xf - mu
    var = jnp.mean(xc * xc, axis=-1, keepdims=True)
    return (xc * lax.rsqrt(var + EPS) * g.astype(F32) + b.astype(F32)).astype(x.dtype)


def _rmsnorm(x, g):
    xf = x.astype(F32)
    return (xf * lax.rsqrt(jnp.mean(xf * xf, axis=-1, keepdims=True) + EPS) * g.astype(F32)).astype(x.dtype)


def _rope(x, positions):
    half = x.shape[-1] // 2
    inv_freq = ROPE_THETA ** (-jnp.arange(half, dtype=F32) / half)
    ang = positions.astype(F32)[..., None] * inv_freq
    ang = ang.reshape(ang.shape[:2] + (1,) * (x.ndim - 3) + (half,))
    cos, sin = jnp.cos(ang), jnp.sin(ang)
    x1, x2 = x[..., :half].astype(F32), x[..., half:].astype(F32)
    return jnp.concatenate([x1 * cos - x2 * sin, x1 * sin + x2 * cos], axis=-1).astype(x.dtype)


def _to_qblocks(t):
    b, s = t.shape[:2]
    return jnp.moveaxis(t.reshape((b, s // BLOCK_Q, BLOCK_Q) + t.shape[2:]), 1, 0)


def _from_qblocks(t):
    t = jnp.moveaxis(t, 0, 1)
    return t.reshape((t.shape[0], t.shape[1] * t.shape[2]) + t.shape[3:])


def _rel_bucket(dist):
    exact = REL_BUCKETS // 2
    n = jnp.maximum(dist, 0)
    nf = jnp.maximum(n, 1).astype(F32)
    large = exact + (jnp.log(nf / exact) / math.log(REL_MAX_DIST / exact) * (REL_BUCKETS - exact)).astype(jnp.int32)
    large = jnp.clip(large, 0, REL_BUCKETS - 1)
    return jnp.where(n < exact, n, large)


def _mla(cq, ckv, kr, positions, q_norm, w_uq, kv_norm, w_ukv):
    b, s, _ = cq.shape
    q = (_rmsnorm(cq, q_norm) @ w_uq).reshape(b, s, MLA_HEADS, MLA_NOPE + MLA_ROPE)
    q_nope = q[..., :MLA_NOPE]
    q_rope = _rope(q[..., MLA_NOPE:], positions)
    kv = (_rmsnorm(ckv, kv_norm) @ w_ukv).reshape(b, s, MLA_HEADS, MLA_NOPE + MLA_V)
    k_nope, v = kv[..., :MLA_NOPE], kv[..., MLA_NOPE:]
    k_rope = _rope(kr, positions)
    scale = (MLA_NOPE + MLA_ROPE) ** -0.5
    k_idx = jnp.arange(s)

    def block(args):
        qn, qr, q_idx = args
        logits = (jnp.einsum('bqhd,bkhd->bhqk', qn, k_nope).astype(F32)
                  + jnp.einsum('bqhr,bkr->bhqk', qr, k_rope).astype(F32)) * scale
        causal = k_idx[None, :] <= q_idx[:, None]
        logits = jnp.where(causal[None, None], logits, NEG_BIG)
        p = jax.nn.softmax(logits, axis=-1).astype(v.dtype)
        return jnp.einsum('bhqk,bkhd->bqhd', p, v)

    o = lax.map(block, (_to_qblocks(q_nope), _to_qblocks(q_rope), k_idx.reshape(-1, BLOCK_Q)))
    return _from_qblocks(o).reshape(b, s, MLA_HEADS * MLA_V)


def _swa(q, k, v, positions, sinks, rel_table):
    b, s, _ = q.shape
    w = SWA_WINDOW
    nb = s // w
    g = SWA_HEADS // SWA_KV_HEADS
    qb = q.reshape(b, nb, w, SWA_KV_HEADS, g, SWA_HEAD_DIM)

    def band(t):
        tb = t.reshape((b, nb, w) + t.shape[2:])
        prev = jnp.concatenate([jnp.zeros_like(tb[:, :1]), tb[:, :-1]], axis=1)
        return jnp.concatenate([prev, tb], axis=2)

    kb = band(k.reshape(b, s, SWA_KV_HEADS, SWA_HEAD_DIM))
    vb = band(v.reshape(b, s, SWA_KV_HEADS, SWA_HEAD_DIM))
    pq = positions.reshape(b, nb, w)
    pk = band(positions)
    logits = jnp.einsum('bnqhgd,bnkhd->bnhgqk', qb, kb).astype(F32) * SWA_HEAD_DIM ** -0.5
    bucket = _rel_bucket(pq[..., :, None] - pk[..., None, :])
    bias = jnp.moveaxis(rel_table[bucket].astype(F32), -1, 2)
    logits = logits + bias.reshape(b, nb, SWA_KV_HEADS, g, w, 2 * w)
    i = jnp.arange(w)[:, None]
    j = jnp.arange(2 * w)[None, :]
    in_window = (j >= i + 1) & (j <= i + w)
    valid = in_window[None] & ((jnp.arange(nb)[:, None, None] > 0) | (j >= w)[None])
    logits = jnp.where(valid[None, :, None, None], logits, NEG_BIG)
    sink = sinks.astype(F32).reshape(1, 1, SWA_KV_HEADS, g, 1, 1)
    m = jnp.maximum(jnp.max(logits, axis=-1, keepdims=True), sink)
    p = jnp.exp(logits - m)
    p = p / (jnp.sum(p, axis=-1, keepdims=True) + jnp.exp(sink - m))
    o = jnp.einsum('bnhgqk,bnkhd->bnqhgd', p.astype(v.dtype), vb)
    return o.reshape(b, s, SWA_HEADS * SWA_HEAD_DIM)


def _hgrn2(q, f, i, g, lower_bound, norm_w):
    dt = g.dtype
    b, s, _ = q.shape
    c = HGRN_CHUNK
    nc = s // c
    qf = jax.nn.silu(q.astype(F32))
    lb = lower_bound.astype(F32)
    forget = lb + (1.0 - lb) * jax.nn.sigmoid(f.astype(F32))
    log_f = jnp.log(forget)
    kf = 1.0 - forget
    vf = i.astype(F32)

    def chunks(t, d):
        return t.reshape(b, nc, c, HGRN_HEADS, d).transpose(1, 0, 3, 2, 4)

    causal = jnp.tril(jnp.ones((c, c), dtype=bool))

    def step(state, inp):
        qc, kc, vc, gc = inp
        bc = jnp.cumsum(gc, axis=2)
        o_inter = jnp.einsum('bhtk,bhkv->bhtv', qc * jnp.exp(bc), state)
        diff = bc[:, :, :, None, :] - bc[:, :, None, :, :]
        decay = jnp.exp(jnp.where(causal[:, :, None], diff, NEG_BIG))
        att = jnp.einsum('bhtk,bhtsk,bhsk->bhts', qc, decay, kc)
        o = o_inter + jnp.einsum('bhts,bhsv->bhtv', att, vc)
        b_last = bc[:, :, -1:, :]
        state = (jnp.exp(b_last[:, :, 0, :, None]) * state
                 + jnp.einsum('bhsk,bhsv->bhkv', kc * jnp.exp(b_last - bc), vc))
        return state, o

    s0 = jnp.zeros((b, HGRN_HEADS, HGRN_DK, HGRN_DV), F32)
    _, o = lax.scan(step, s0, (chunks(qf, HGRN_DK), chunks(kf, HGRN_DK), chunks(vf, HGRN_DV), chunks(log_f, HGRN_DK)))
    o = o.transpose(1, 0, 3, 2, 4).reshape(b, s, HGRN_HEADS, HGRN_DV)
    gate = jax.nn.silu(g.astype(F32).reshape(b, s, HGRN_HEADS, HGRN_DV))
    o = _rmsnorm(o, norm_w.reshape(HGRN_HEADS, HGRN_DV)) * gate
    return o.reshape(b, s, HGRN_HEADS * HGRN_DV).astype(dt)


def _stick_breaking(q, k, v):
    b, s, _ = q.shape
    q = q.reshape(b, s, SB_HEADS, SB_HEAD_DIM)
    k = k.reshape(b, s, SB_HEADS, SB_HEAD_DIM)
    v = v.reshape(b, s, SB_HEADS, SB_HEAD_DIM)
    k_idx = jnp.arange(s)

    def block(args):
        qb, q_idx = args
        z = jnp.einsum('bqhd,bkhd->bhqk', qb, k).astype(F32) * SB_HEAD_DIM ** -0.5
        strict = (k_idx[None, :] < q_idx[:, None])[None, None]
        log_1m_beta = jnp.where(strict, jax.nn.log_sigmoid(-z), 0.0)
        log_remain = lax.cumsum(log_1m_beta, axis=3, reverse=True) - log_1m_beta
        a = jnp.where(strict, jnp.exp(jax.nn.log_sigmoid(z) + log_remain), 0.0)
        return jnp.einsum('bhqk,bkhd->bqhd', a.astype(v.dtype), v)

    o = lax.map(block, (_to_qblocks(q), k_idx.reshape(-1, BLOCK_Q)))
    return _from_qblocks(o).reshape(b, s, SB_HEADS * SB_HEAD_DIM)


def _hybrid_mixer(x, positions, lower_bound, rel_table, w_in, mla_q_norm, mla_w_uq, mla_kv_norm,
                  mla_w_ukv, swa_sinks, hgrn_norm, w_branch, w_out):
    b, s, _ = x.shape
    p = _split_cols(x @ w_in)
    y_mla = _mla(p['mla_cq'], p['mla_ckv'], p['mla_kr'], positions, mla_q_norm, mla_w_uq, mla_kv_norm, mla_w_ukv)
    y_swa = _swa(p['swa_q'], p['swa_k'], p['swa_v'], positions, swa_sinks, rel_table)
    y_hgrn = _hgrn2(p['hgrn_q'], p['hgrn_f'], p['hgrn_i'], p['hgrn_g'], lower_bound, hgrn_norm)
    y_sb = _stick_breaking(p['sb_q'], p['sb_k'], p['sb_v'])
    branches = jnp.stack([y_mla, y_swa, y_hgrn, y_sb], axis=2)
    gates = jax.nn.sigmoid(p['gates'].reshape(b, s, N_BRANCH, D_MODEL))
    merged = jnp.sum(gates * jnp.einsum('bsnc,ncd->bsnd', branches, w_branch), axis=2)
    return merged @ w_out


def _mem_xattn(x, mem, wq, wkv, wo):
    b, s, _ = x.shape
    m = mem.shape[1]
    q = (x @ wq).reshape(b, s, XA_HEADS, XA_HEAD_DIM)
    kv = (mem @ wkv).reshape(b, m, 2, XA_HEADS, XA_HEAD_DIM)
    k, v = kv[:, :, 0], kv[:, :, 1]
    logits = jnp.einsum('bshd,bmhd->bhsm', q, k).astype(F32) * XA_HEAD_DIM ** -0.5
    p = jax.nn.softmax(logits, axis=-1).astype(v.dtype)
    o = jnp.einsum('bhsm,bmhd->bshd', p, v).reshape(b, s, XA_HEADS * XA_HEAD_DIM)
    return o @ wo


def _swiglu(x, w13, w2):
    a, gate = jnp.split(x @ w13, 2, axis=-1)
    return (jax.nn.silu(a) * gate) @ w2


def _moe(x, router, w13, w2):
    b, s, d = x.shape
    n = b * s
    a = n * TOP_K
    xf = x.reshape(n, d)
    logits = (xf @ router).astype(F32)
    top_val, top_idx = lax.top_k(logits, TOP_K)
    top_w = jax.nn.softmax(top_val, axis=-1)
    e_flat = top_idx.reshape(-1)
    t_flat = jnp.repeat(jnp.arange(n, dtype=jnp.int32), TOP_K)
    w_flat = top_w.reshape(-1)
    order = jnp.argsort(e_flat)
    e_s, t_s, w_s = e_flat[order], t_flat[order], w_flat[order]
    counts = jnp.bincount(e_flat, length=N_EXPERTS)
    padded = (counts + MOE_BLOCK - 1) // MOE_BLOCK * MOE_BLOCK
    group_start = jnp.cumsum(counts) - counts
    padded_end = jnp.cumsum(padded)
    padded_start = padded_end - padded
    dest = padded_start[e_s] + jnp.arange(a, dtype=jnp.int32) - group_start[e_s]
    cap = a + N_EXPERTS * MOE_BLOCK
    n_blk = cap // MOE_BLOCK
    slot_tok = jnp.full((cap,), n, dtype=jnp.int32).at[dest].set(t_s)
    slot_w = jnp.zeros((cap,), F32).at[dest].set(w_s)
    blk_exp = jnp.minimum(jnp.searchsorted(padded_end, jnp.arange(n_blk) * MOE_BLOCK, side='right'), N_EXPERTS - 1)
    x_pad = jnp.concatenate([xf, jnp.zeros((1, d), x.dtype)], axis=0)
    xb = x_pad[slot_tok].reshape(n_blk, MOE_BLOCK, d)

    def expert_block(args):
        xe, e = args
        return _swiglu(xe, w13[e], w2[e])

    yb = lax.map(expert_block, (xb, blk_exp)).reshape(cap, d)
    y = jnp.zeros((n + 1, d), x.dtype).at[slot_tok].add(yb * slot_w[:, None].astype(x.dtype))
    return y[:n].reshape(b, s, d)


def setup_inputs(seed: int = 0) -> dict:
    key = jax.random.key(seed)
    ks = list(jax.random.split(key, 24))

    def nrm(k, shape, scale):
        return jax.random.normal(k, shape, F32) * scale

    x = nrm(ks[0], (BATCH, SEQ, D_MODEL), 1.0)
    mem = nrm(ks[1], (BATCH, MEM_LEN, D_MODEL), 1.0)
    offset = jax.random.randint(ks[2], (BATCH, 1), 0, MAX_POS_OFFSET, dtype=jnp.int32)
    positions = (offset + jnp.arange(SEQ, dtype=jnp.int32)[None, :]).astype(jnp.int32)
    rel_bias_table = nrm(ks[3], (REL_BUCKETS, SWA_HEADS), 0.5)
    hgrn_lb_logits = nrm(ks[4], (DEPTH, HGRN_HEADS * HGRN_DK), 0.5)
    w_in = nrm(ks[5], (DEPTH, D_MODEL, IN_COLS), D_MODEL ** -0.5)
    mla_q_norm = 1.0 + nrm(ks[6], (DEPTH, MLA_Q_LORA), 0.02)
    mla_w_uq = nrm(ks[7], (DEPTH, MLA_Q_LORA, MLA_HEADS * (MLA_NOPE + MLA_ROPE)), MLA_Q_LORA ** -0.5)
    mla_kv_norm = 1.0 + nrm(ks[8], (DEPTH, MLA_KV_LORA), 0.02)
    mla_w_ukv = nrm(ks[9], (DEPTH, MLA_KV_LORA, MLA_HEADS * (MLA_NOPE + MLA_V)), MLA_KV_LORA ** -0.5)
    swa_sinks = nrm(ks[10], (DEPTH, SWA_HEADS), 1.0)
    hgrn_norm = 1.0 + nrm(ks[11], (DEPTH, HGRN_HEADS * HGRN_DV), 0.02)
    w_branch = nrm(ks[12], (DEPTH, N_BRANCH, BRANCH_WIDTH, D_MODEL), BRANCH_WIDTH ** -0.5)
    w_out = nrm(ks[13], (DEPTH, D_MODEL, D_MODEL), D_MODEL ** -0.5 * DEEPNORM_BETA)
    ln_g = 1.0 + nrm(ks[14], (DEPTH, 3, D_MODEL), 0.02)
    ln_b = nrm(ks[15], (DEPTH, 3, D_MODEL), 0.02)
    xa_wq = nrm(ks[16], (DEPTH, D_MODEL, XA_HEADS * XA_HEAD_DIM), D_MODEL ** -0.5)
    xa_wkv = nrm(ks[17], (DEPTH, D_MODEL, 2 * XA_HEADS * XA_HEAD_DIM), D_MODEL ** -0.5)
    xa_wo = nrm(ks[18], (DEPTH, XA_HEADS * XA_HEAD_DIM, D_MODEL), (XA_HEADS * XA_HEAD_DIM) ** -0.5 * DEEPNORM_BETA)
    ffn_w13 = nrm(ks[19], (N_DENSE, D_MODEL, 2 * F_DENSE), D_MODEL ** -0.5)
    ffn_w2 = nrm(ks[20], (N_DENSE, F_DENSE, D_MODEL), F_DENSE ** -0.5 * DEEPNORM_BETA)
    moe_router = nrm(ks[21], (N_MOE, D_MODEL, N_EXPERTS), D_MODEL ** -0.5)
    moe_w13 = nrm(ks[22], (N_MOE, N_EXPERTS, D_MODEL, 2 * F_EXPERT), D_MODEL ** -0.5)
    moe_w2 = nrm(ks[23], (N_MOE, N_EXPERTS, F_EXPERT, D_MODEL), F_EXPERT ** -0.5 * DEEPNORM_BETA)
    return {'x': x, 'mem': mem, 'positions': positions, 'rel_bias_table': rel_bias_table,
            'hgrn_lb_logits': hgrn_lb_logits, 'w_in': w_in, 'mla_q_norm': mla_q_norm, 'mla_w_uq': mla_w_uq,
            'mla_kv_norm': mla_kv_norm, 'mla_w_ukv': mla_w_ukv, 'swa_sinks': swa_sinks, 'hgrn_norm': hgrn_norm,
            'w_branch': w_branch, 'w_out': w_out, 'ln_g': ln_g, 'ln_b': ln_b, 'xa_wq': xa_wq, 'xa_wkv': xa_wkv,
            'xa_wo': xa_wo, 'ffn_w13': ffn_w13, 'ffn_w2': ffn_w2, 'moe_router': moe_router,
            'moe_w13': moe_w13, 'moe_w2': moe_w2}


def reference(x, mem, positions, rel_bias_table, hgrn_lb_logits, w_in, mla_q_norm, mla_w_uq, mla_kv_norm,
              mla_w_ukv, swa_sinks, hgrn_norm, w_branch, w_out, ln_g, ln_b, xa_wq, xa_wkv, xa_wo,
              ffn_w13, ffn_w2, moe_router, moe_w13, moe_w2):
    sm = jax.nn.softmax(hgrn_lb_logits.astype(F32), axis=0)
    lower_bounds = jnp.cumsum(sm, axis=0) - sm[0]
    for l in range(DEPTH):
        y = _hybrid_mixer(x, positions, lower_bounds[l], rel_bias_table, w_in[l], mla_q_norm[l], mla_w_uq[l],
                          mla_kv_norm[l], mla_w_ukv[l], swa_sinks[l], hgrn_norm[l], w_branch[l], w_out[l])
        x = _layernorm(DEEPNORM_ALPHA * x + y, ln_g[l, 0], ln_b[l, 0])
        y = _mem_xattn(x, mem, xa_wq[l], xa_wkv[l], xa_wo[l])
        x = _layernorm(DEEPNORM_ALPHA * x + y, ln_g[l, 1], ln_b[l, 1])
        if l % 2 == 0:
            y = _swiglu(x, ffn_w13[l // 2], ffn_w2[l // 2])
        else:
            y = _moe(x, moe_router[l // 2], moe_w13[l // 2], moe_w2[l // 2])
        x = _layernorm(DEEPNORM_ALPHA * x + y, ln_g[l, 2], ln_b[l, 2])
    return x
```

```python
import math
import contextlib
import numpy as np
import ml_dtypes
import concourse.bass as bass
import concourse.mybir as mybir
from concourse.bass_utils import run_bass_kernel_spmd


F32 = mybir.dt.float32
BF16 = mybir.dt.bfloat16
I32 = mybir.dt.int32
AF = mybir.ActivationFunctionType
ALU = mybir.AluOpType
AX = mybir.AxisListType

ENGS = ("pe", "act", "dve", "pool", "sp")
SEM_CHUNK = 12000
RING = 8


class Op:
    __slots__ = ("eng", "fn", "reads", "writes", "is_dma", "deps", "pos", "gid",
                 "waits", "vc", "need_inc", "slot", "round", "cnt")

    def __init__(self, eng, fn, reads, writes, is_dma):
        self.eng = eng
        self.fn = fn
        self.reads = reads
        self.writes = writes
        self.is_dma = is_dma
        self.deps = ()
        self.waits = []
        self.vc = None
        self.need_inc = False
        self.slot = None
        self.round = None
        self.cnt = None


class Prog:
    def __init__(self, nc):
        self.nc = nc
        self.ops = []
        self.last_w = {}
        self.readers = {}

    def op(self, eng, fn, reads=(), writes=(), dma=False):
        o = Op(eng, fn, tuple(reads), tuple(writes), dma)
        o.gid = len(self.ops)
        deps = set()
        for k in o.reads:
            w = self.last_w.get(k)
            if w is not None:
                deps.add(w)
        for k in o.writes:
            w = self.last_w.get(k)
            if w is not None:
                deps.add(w)
            for r in self.readers.get(k, ()):
                deps.add(r)
        deps.discard(o.gid)
        o.deps = tuple(sorted(deps))
        for k in o.reads:
            self.readers.setdefault(k, []).append(o.gid)
        for k in o.writes:
            self.last_w[k] = o.gid
            self.readers[k] = []
        self.ops.append(o)
        return o

    def dma(self, eng, out, in_, reads=(), writes=(), **kw):
        return self.op(eng, lambda e: e.dma_start(out=out, in_=in_, **kw), reads, writes, dma=True)

    def I(self, eng, meth, *args, reads=(), writes=(), **kw):
        return self.op(eng, lambda e: getattr(e, meth)(*args, **kw), reads, writes)

    def finalize(self, final_wait_keys=()):
        nc = self.nc
        ops = self.ops
        streams = {e: [] for e in ENGS}
        for o in ops:
            o.pos = len(streams[o.eng])
            streams[o.eng].append(o)
        dma_count = {e: 0 for e in ENGS}
        for o in ops:
            if o.is_dma:
                i = dma_count[o.eng]
                dma_count[o.eng] += 1
                o.slot = (o.eng, i % RING)
                o.round = i // RING + 1
        know = {e: {} for e in ENGS}
        slot_last = {}

        def completion_key(d):
            return (d.slot, d.round) if d.is_dma else (d.eng, d.pos + 1)

        for o in ops:
            K = know[o.eng]
            need = {}
            if o.is_dma:
                prev = slot_last.get(o.slot)
                if prev is not None:
                    need[prev.slot] = (prev.round, prev)
                slot_last[o.slot] = o
            for di in o.deps:
                d = ops[di]
                if d.eng == o.eng and not d.is_dma and not o.is_dma:
                    if o.eng == "pe":
                        continue
                ck, cv = completion_key(d)
                if K.get(ck, 0) >= cv:
                    continue
                if ck not in need or need[ck][0] < cv:
                    need[ck] = (cv, d)
            items = sorted(need.items(), key=lambda kv: -kv[1][1].gid)
            for ck, (cv, d) in items:
                if K.get(ck, 0) >= cv:
                    continue
                o.waits.append(d)
                d.need_inc = True
                for k2, v2 in d.vc.items():
                    if K.get(k2, 0) < v2:
                        K[k2] = v2
            vc = dict(K)
            ck, cv = completion_key(o)
            vc[ck] = cv
            o.vc = vc
            if not o.is_dma and o.eng != "sp":
                pass
        finals = []
        for k in final_wait_keys:
            w = self.last_w.get(k)
            if w is not None:
                ops[w].need_inc = True
                finals.append(ops[w])
        sem_needed = {}
        for e in ENGS:
            c = 0
            for o in streams[e]:
                if o.is_dma:
                    o.need_inc = True
                    continue
                if o.need_inc:
                    c += 1
                    o.cnt = c
            sem_needed[e] = (c + SEM_CHUNK - 1) // SEM_CHUNK
        self.streams = streams
        self.finals = finals
        self.sem_needed = sem_needed
        self.dma_count = dma_count

    def emit(self, sem_stack=None):
        nc = self.nc
        streams = self.streams
        import contextlib
        with contextlib.ExitStack() as st:
            if sem_stack is not None:
                blk_stack, st = st, sem_stack
            else:
                blk_stack = st
            esems = {}
            for e in ENGS:
                esems[e] = [st.enter_context(nc.semaphore(f"s{id(self) % 100000}_{e}_{i}")) for i in range(self.sem_needed[e])]
            ssems = {}
            for e in ENGS:
                if self.dma_count[e] > 0:
                    for r in range(min(RING, self.dma_count[e])):
                        ssems[(e, r)] = st.enter_context(nc.semaphore(f"d{id(self) % 100000}_{e}_{r}"))
            block = blk_stack.enter_context(nc.Block())

            def wait_args(d):
                if d.is_dma:
                    return ssems[d.slot], 16 * d.round
                c = d.cnt - 1
                return esems[d.eng][c // SEM_CHUNK], (c % SEM_CHUNK) + 1

            def run(ename, eng):
                for o in streams[ename]:
                    for d in o.waits:
                        s, v = wait_args(d)
                        eng.wait_ge(s, v)
                    ins = o.fn(eng)
                    if o.need_inc:
                        if o.is_dma:
                            ins.then_inc(ssems[o.slot], 16)
                        else:
                            c = o.cnt - 1
                            ins.then_inc(esems[o.eng][c // SEM_CHUNK], 1)
                if ename == "sp":
                    last = {}
                    for o in self.ops:
                        if o.is_dma:
                            last[o.slot] = o.round
                    for slot, rnd in last.items():
                        eng.wait_ge(ssems[slot], 16 * rnd)

            @block.sync
            def _(eng):
                run("sp", eng)

            @block.tensor
            def _(eng):
                run("pe", eng)

            @block.scalar
            def _(eng):
                run("act", eng)

            @block.vector
            def _(eng):
                run("dve", eng)

            @block.gpsimd
            def _(eng):
                run("pool", eng)

    def stats(self):
        return {e: len(s) for e, s in self.streams.items()}


D = 1024
ALPHA = 4 ** 0.25
EPS = 1e-5
F_DENSE = 2816
F_EXP = 3584
NEXP = 8
TP = 1024
TB = 2


_UID = globals().get('_UID', [0])


class Ctx:
    def __init__(self, nc, P, st):
        self.nc, self.P, self.st = nc, P, st
        _UID[0] += 1
        self.uid = _UID[0]
        self.nbank = 0
        self.banks = [st.enter_context(nc.psum_tensor(f"u{_UID[0]}_ps{i}", [128, 512], F32)) for i in range(8)]
        self.wslot = 0

    def sb(self, name, shape, dt):
        return self.st.enter_context(self.nc.sbuf_tensor(f"u{self.uid}_sb_" + name, shape, dt))

    def bank(self):
        i = self.nbank % 8
        self.nbank += 1
        return self.banks[i], f"ps{i}"


def TS(tb):
    return slice(tb * 512, (tb + 1) * 512)


def phase2(cx, dr, is_moe, NTC):
    nc, P = cx.nc, cx.P
    I = P.I
    NPASS = NTC // TP
    sb = cx.sb
    r32 = sb("r32", [128, 8, TP], F32)
    xb = sb("xb", [128, 8, TP], BF16)
    big = sb("big", [128, 28, TP], BF16)
    gt = [sb(f"gt{n}", [128, 512], F32) for n in range(4)]
    rbv = lambda c: big[:, c // 2, (c % 2) * 512:(c % 2 + 1) * 512]
    sqv = lambda c: big[:, 4 + c // 2, (c % 2) * 512:(c % 2 + 1) * 512]
    rbk = lambda c: f"big{c // 2}_{c % 2}"
    sqk = lambda c: f"big{4 + c // 2}_{c % 2}"
    mean = sb("mean", [128, 512], F32)
    msq = sb("msq", [128, 512], F32)
    var = sb("var", [128, 512], F32)
    rstd = sb("rstd", [128, 512], F32)
    lnt = [sb(f"lnt{i}", [128, 512], F32) for i in range(2)]
    lng = sb("lng", [128, 3, 8], F32)
    lnb = sb("lnb", [128, 3, 8], F32)
    ones = sb("ones", [128, 128], BF16)
    memb = sb("memb", [128, 8, 256], BF16)
    KT = sb("KT", [64, 4, 256], BF16)
    Vx = sb("Vx", [128, 2, 256], BF16)
    PT = [sb(f"PT{i}", [128, 512], BF16) for i in range(2)]
    rden = sb("rden", [64, 512], F32)
    WSZ = 7168
    NW = 3
    wbuf = [sb(f"wbuf{i}", [128, WSZ], BF16) for i in range(NW)]
    sa = [sb(f"sa{i}", [128, 512], F32) for i in range(2)]
    R = lambda c, tb: f"r32_{c}_{tb}"
    X = lambda c, tb: f"xb_{c}_{tb}"
    B = lambda j, tb: f"big{j}_{tb}"

    I("dve", "memset", ones[:], 1.0, writes=["ones"])
    if "ygather" in dr:
        dr["sel4_sb"] = sb("sel4", [128, 4], F32)
        P.dma("sp", dr["sel4_sb"][:], dr["sel4"], writes=["sel4"])
    P.dma("sp", lng[:], dr["lng"], writes=["lng"])
    P.dma("sp", lnb[:], dr["lnb"], writes=["lnb"])

    def wload(src_ap, kc, ncols, parts=128):
        i = cx.wslot % NW
        cx.wslot += 1
        assert kc * ncols <= WSZ
        view = wbuf[i][0:parts, 0:kc * ncols].rearrange("p (c n) -> p c n", c=kc)
        P.dma("pool", view, src_ap, writes=[f"wbuf{i}"])
        return view, f"wbuf{i}"

    def layernorm(tb, li):
        ts = TS(tb)
        for c in range(8):
            I("pool", "tensor_copy", out=rbv(c), in_=r32[:, c, ts], reads=[R(c, tb)], writes=[rbk(c)])
            I("act", "activation", out=sqv(c), in_=r32[:, c, ts], func=AF.Square, reads=[R(c, tb)], writes=[sqk(c)])
        ps_s, ks = cx.bank()
        for c in range(8):
            I("pe", "matmul", ps_s[:], lhsT=ones[:], rhs=rbv(c), start=(c == 0), stop=(c == 7), reads=["ones", rbk(c)], writes=[ks])
        ps_q, kq = cx.bank()
        for c in range(8):
            I("pe", "matmul", ps_q[:], lhsT=ones[:], rhs=sqv(c), start=(c == 0), stop=(c == 7), reads=["ones", sqk(c)], writes=[kq])
        I("act", "activation", out=mean[:], in_=ps_s[:], func=AF.Copy, scale=1.0 / D, reads=[ks], writes=["mean"])
        I("pool", "tensor_tensor", out=msq[:], in0=mean[:], in1=mean[:], op=ALU.mult, reads=["mean"], writes=["msq"])
        I("dve", "scalar_tensor_tensor", out=var[:], in0=ps_q[:], scalar=1.0 / D, in1=msq[:], op0=ALU.mult, op1=ALU.subtract,
          reads=[kq, "msq"], writes=["var"])
        I("dve", "tensor_scalar", out=var[:], in0=var[:], scalar1=EPS, scalar2=None, op0=ALU.add, reads=["var"], writes=["var"])
        I("act", "activation", out=rstd[:], in_=var[:], func=AF.Ln, reads=["var"], writes=["rstd"])
        I("act", "activation", out=rstd[:], in_=rstd[:], func=AF.Exp, scale=-0.5, reads=["rstd"], writes=["rstd"])
        for c in range(8):
            t = lnt[c % 2]
            kt = f"lnt{c % 2}"
            I("dve", "tensor_tensor", out=t[:], in0=r32[:, c, ts], in1=mean[:], op=ALU.subtract, reads=[R(c, tb), "mean"], writes=[kt])
            I("pool", "tensor_tensor", out=t[:], in0=t[:], in1=rstd[:], op=ALU.mult, reads=[kt, "rstd"], writes=[kt])
            I("act", "activation", out=r32[:, c, ts], in_=t[:], func=AF.Identity, scale=lng[:, li, c:c + 1], bias=lnb[:, li, c:c + 1],
              reads=[kt, "lng", "lnb"], writes=[R(c, tb)])
            I("pool", "tensor_copy", out=xb[:, c, ts], in_=r32[:, c, ts], reads=[R(c, tb)], writes=[X(c, tb)])

    def resid_evac(ps, kps, m, tb):
        ts = TS(tb)
        I("dve", "scalar_tensor_tensor", out=r32[:, m, ts], in0=r32[:, m, ts], scalar=ALPHA, in1=ps[:], op0=ALU.mult, op1=ALU.add,
          reads=[kps, R(m, tb)], writes=[R(m, tb)])

    for ps_i in range(NPASS):
        t0 = ps_i * TP
        P.dma("sp", r32[:], dr["x32"].rearrange("(c p) t -> p c t", p=128)[:, :, t0:t0 + TP],
              writes=[R(c, tb) for c in range(8) for tb in range(TB)])
        for c in range(8):
            for tb in range(TB):
                I("pool", "tensor_copy", out=xb[:, c, TS(tb)], in_=r32[:, c, TS(tb)], reads=[R(c, tb)], writes=[X(c, tb)])
        if "ygather" not in dr:
            for n in range(4):
                P.dma("sp", big[:, 2 * n:2 * n + 2, :], dr["ybr"][n].rearrange("(c p) t -> p c t", p=128)[:, :, t0:t0 + TP],
                      writes=[B(2 * n + j, tb) for j in range(2) for tb in range(TB)])
        else:
            sel4 = dr["sel4_sb"]
            for tb in range(TB):
                for jq in range(4):
                    stg = big[:, 8 + 4 * (jq % 2):12 + 4 * (jq % 2), :].rearrange("p a (b t) -> p (a b) t", t=512)
                    kst = [B(8 + 4 * (jq % 2) + a, tb2) for a in range(4) for tb2 in range(TB)]
                    src = dr["ygather"](jq, ps_i * TB + tb)
                    for hh in range(2):
                        for c2 in range(2):
                            sv = src.rearrange("(c2 hh n d) t -> c2 hh d n t", c2=2, hh=2, n=4)[c2, hh]
                            dv = stg[hh * 64:(hh + 1) * 64].rearrange("p (n c2) t -> p c2 n t", c2=2)[:, c2]
                            P.dma("sp", dv, sv, writes=kst)
                    dst = big[:, 0:8, TS(tb)]
                    kd = [B(a, tb) for a in range(8)]
                    if jq == 0:
                        I("dve", "tensor_scalar", out=dst, in0=stg, scalar1=sel4[:, 0:1], scalar2=None, op0=ALU.mult, reads=kst + ["sel4"], writes=kd)
                    else:
                        I("dve", "scalar_tensor_tensor", out=dst, in0=stg, scalar=sel4[:, jq:jq + 1], in1=dst, op0=ALU.mult, op1=ALU.add,
                          reads=kst + kd + ["sel4"], writes=kd)
        for m in range(8):
            wg, kwg = wload(dr["wg"].rearrange("(c p) n -> p c n", p=128)[:, :, m * 512:(m + 1) * 512], 8, 512)
            wbr, kwbr = wload(dr["wbr"].rearrange("(c p) n -> p c n", p=128)[:, :, m * 128:(m + 1) * 128], 8, 128)
            for tb in range(TB):
                ts = TS(tb)
                for n in range(4):
                    ps, kps = cx.bank()
                    for c in range(8):
                        I("pe", "matmul", ps[:], lhsT=wg[:, c, n * 128:(n + 1) * 128], rhs=xb[:, c, ts], start=(c == 0), stop=(c == 7),
                          reads=[kwg, X(c, tb)], writes=[kps])
                    I("act", "activation", out=gt[n][:], in_=ps[:], func=AF.Sigmoid, reads=[kps], writes=[f"gt{n}"])
                for n in range(4):
                    ps, kps = cx.bank()
                    for c2 in range(2):
                        I("pe", "matmul", ps[:], lhsT=wbr[:, n * 2 + c2, :], rhs=big[:, n * 2 + c2, ts], start=(c2 == 0), stop=(c2 == 1),
                          reads=[kwbr, B(n * 2 + c2, tb)], writes=[kps])
                    I("dve", "tensor_tensor", out=gt[n][:], in0=gt[n][:], in1=ps[:], op=ALU.mult, reads=[kps, f"gt{n}"], writes=[f"gt{n}"])
                I("pool", "tensor_tensor", out=gt[0][:], in0=gt[0][:], in1=gt[1][:], op=ALU.add, reads=["gt0", "gt1"], writes=["gt0"])
                I("pool", "tensor_tensor", out=gt[2][:], in0=gt[2][:], in1=gt[3][:], op=ALU.add, reads=["gt2", "gt3"], writes=["gt2"])
                I("pool", "tensor_tensor", out=big[:, 8 + m, ts], in0=gt[0][:], in1=gt[2][:], op=ALU.add, reads=["gt0", "gt2"], writes=[B(8 + m, tb)])
        for m in range(8):
            wo_, kwo = wload(dr["wout"].rearrange("(c p) n -> p c n", p=128)[:, :, m * 128:(m + 1) * 128], 8, 128)
            for tb in range(TB):
                ps, kps = cx.bank()
                for c in range(8):
                    I("pe", "matmul", ps[:], lhsT=wo_[:, c, :], rhs=big[:, 8 + c, TS(tb)], start=(c == 0), stop=(c == 7),
                      reads=[kwo, B(8 + c, tb)], writes=[kps])
                resid_evac(ps, kps, m, tb)
        for tb in range(TB):
            layernorm(tb, 0)
        if ps_i == 0:
            P.dma("pool", memb[:], dr["memT"].rearrange("(c p) t -> p c t", p=128), writes=["memb"])
            wkv, kwkv = wload(dr["wkv"].rearrange("(c p) n -> p c n", p=128), 8, 512)
            for h in range(4):
                ps, kps = cx.bank()
                for c in range(8):
                    I("pe", "matmul", ps[0:64, 0:256], lhsT=wkv[:, c, h * 64:(h + 1) * 64], rhs=memb[:, c, :], start=(c == 0), stop=(c == 7),
                      reads=[kwkv, "memb"], writes=[kps])
                I("act", "activation", out=KT[:, h, :], in_=ps[0:64, 0:256], func=AF.Copy, reads=[kps], writes=["KT"])
            for mt in range(2):
                ps, kps = cx.bank()
                for c in range(8):
                    I("pe", "matmul", ps[:, 0:256], lhsT=memb[:, c, mt * 128:(mt + 1) * 128], rhs=wkv[:, c, 256:512], start=(c == 0), stop=(c == 7),
                      reads=[kwkv, "memb"], writes=[kps])
                I("act", "activation", out=Vx[:, mt, :], in_=ps[:, 0:256], func=AF.Copy, reads=[kps], writes=["Vx"])
        wq, kwq = wload(dr["wq"].rearrange("(c p) n -> p c n", p=128), 8, 256)
        for tb in range(TB):
            ts = TS(tb)
            for h in range(4):
                ps, kps = cx.bank()
                for c in range(8):
                    I("pe", "matmul", ps[0:64, :], lhsT=wq[:, c, h * 64:(h + 1) * 64], rhs=xb[:, c, ts], start=(c == 0), stop=(c == 7),
                      reads=[kwq, X(c, tb)], writes=[kps])
                I("act", "activation", out=big[0:64, 16 + h, ts], in_=ps[0:64, :], func=AF.Copy, scale=0.125, reads=[kps], writes=[B(16 + h, tb)])
            for h in range(4):
                for mt in range(2):
                    ps, kps = cx.bank()
                    I("pe", "matmul", ps[:], lhsT=KT[:, h, mt * 128:(mt + 1) * 128], rhs=big[0:64, 16 + h, ts], start=True, stop=True,
                      reads=["KT", B(16 + h, tb)], writes=[kps])
                    I("act", "activation", out=PT[mt][:], in_=ps[:], func=AF.Exp, reads=[kps], writes=[f"PT{mt}"])
                pso, kpo = cx.bank()
                for mt in range(2):
                    I("pe", "matmul", pso[0:64, :], lhsT=Vx[:, mt, h * 64:(h + 1) * 64], rhs=PT[mt][:], start=(mt == 0), stop=(mt == 1),
                      reads=["Vx", f"PT{mt}"], writes=[kpo])
                psd, kpd = cx.bank()
                for mt in range(2):
                    I("pe", "matmul", psd[0:64, :], lhsT=ones[:, 0:64], rhs=PT[mt][:], start=(mt == 0), stop=(mt == 1),
                      reads=["ones", f"PT{mt}"], writes=[kpd])
                I("dve", "reciprocal", out=rden[:], in_=psd[0:64, :], reads=[kpd], writes=["rden"])
                I("dve", "tensor_tensor", out=big[0:64, 20 + h, ts], in0=pso[0:64, :], in1=rden[:], op=ALU.mult,
                  reads=[kpo, "rden"], writes=[B(20 + h, tb)])
        wo2, kwo2 = wload(dr["wo"].rearrange("(h p) n -> p h n", p=64), 4, 1024, parts=64)
        for tb in range(TB):
            ts = TS(tb)
            for m in range(8):
                ps, kps = cx.bank()
                for h in range(4):
                    I("pe", "matmul", ps[:], lhsT=wo2[0:64, h, m * 128:(m + 1) * 128], rhs=big[0:64, 20 + h, ts], start=(h == 0), stop=(h == 3),
                      reads=[kwo2, B(20 + h, tb)], writes=[kps])
                resid_evac(ps, kps, m, tb)
        for tb in range(TB):
            layernorm(tb, 1)
        if not is_moe:
            ffn_expert(cx, dr["w13"], dr["w2"], F_DENSE, r32, xb, big, sa, wload, None, True)
        else:
            moe(cx, dr, r32, xb, big, sa, wload)
        for tb in range(TB):
            layernorm(tb, 2)
        P.dma("sp", dr["xo32"].rearrange("(c p) t -> p c t", p=128)[:, :, t0:t0 + TP], r32[:],
              reads=[R(c, tb) for c in range(8) for tb in range(TB)], writes=["xo32"])
        if "xob" in dr:
            P.dma("sp", dr["xob"].rearrange("(c p) t -> p c t", p=128)[:, :, t0:t0 + TP], xb[:],
                  reads=[X(c, tb) for c in range(8) for tb in range(TB)], writes=["xob"])
        if "xchunk" in dr:
            for fc in range(4):
                for tb in range(TB):
                    P.dma("sp", dr["xchunk"](fc, ps_i * TB + tb).rearrange("(c p) t -> p c t", p=128), xb[:, 2 * fc:2 * fc + 2, TS(tb)],
                          reads=[X(c, tb) for c in (2 * fc, 2 * fc + 1)], writes=["xob"])


def ffn_expert(cx, w13, w2, F, r32, xb, big, sa, wload, wbc, first_scale):
    P = cx.P
    I = P.I
    R = lambda c, tb: f"r32_{c}_{tb}"
    X = lambda c, tb: f"xb_{c}_{tb}"
    B = lambda j, tb: f"big{j}_{tb}"
    FT = F // 128
    w13v = w13.rearrange("(c p) n -> p c n", p=128)
    cnt = 0
    for f0 in range(0, FT, 2):
        nf = min(2, FT - f0)
        wt, kw = wload(w13v[:, :, f0 * 256:(f0 + nf) * 256], 8, nf * 256)
        for fi in range(nf):
            f = f0 + fi
            for tb in range(TB):
                ts = TS(tb)
                psa, ka = cx.bank()
                for c in range(8):
                    I("pe", "matmul", psa[:], lhsT=wt[:, c, fi * 256:fi * 256 + 128], rhs=xb[:, c, ts], start=(c == 0), stop=(c == 7),
                      reads=[kw, X(c, tb)], writes=[ka])
                psg, kg = cx.bank()
                for c in range(8):
                    I("pe", "matmul", psg[:], lhsT=wt[:, c, fi * 256 + 128:fi * 256 + 256], rhs=xb[:, c, ts], start=(c == 0), stop=(c == 7),
                      reads=[kw, X(c, tb)], writes=[kg])
                s = sa[cnt % 2]
                ksa = f"sa{cnt % 2}"
                cnt += 1
                I("act", "activation", out=s[:], in_=psa[:], func=AF.Silu, reads=[ka], writes=[ksa])
                if wbc is None:
                    I("dve", "tensor_tensor", out=big[:, f, ts], in0=s[:], in1=psg[:], op=ALU.mult, reads=[kg, ksa], writes=[B(f, tb)])
                else:
                    I("dve", "tensor_tensor", out=s[:], in0=s[:], in1=psg[:], op=ALU.mult, reads=[kg, ksa], writes=[ksa])
                    I("pool", "tensor_tensor", out=big[:, f, ts], in0=s[:], in1=wbc[0][:, ts], op=ALU.mult, reads=[ksa, wbc[1]], writes=[B(f, tb)])
    w2v = w2.rearrange("(c p) n -> p c n", p=128)
    for m0 in range(0, 8, 2):
        wt, kw = wload(w2v[:, :, m0 * 128:(m0 + 2) * 128], FT, 256)
        for mi in range(2):
            m = m0 + mi
            for tb in range(TB):
                ts = TS(tb)
                ps, kps = cx.bank()
                for f in range(FT):
                    I("pe", "matmul", ps[:], lhsT=wt[:, f, mi * 128:(mi + 1) * 128], rhs=big[:, f, ts], start=(f == 0), stop=(f == FT - 1),
                      reads=[kw, B(f, tb)], writes=[kps])
                if first_scale:
                    I("dve", "scalar_tensor_tensor", out=r32[:, m, ts], in0=r32[:, m, ts], scalar=ALPHA, in1=ps[:], op0=ALU.mult, op1=ALU.add,
                      reads=[kps, R(m, tb)], writes=[R(m, tb)])
                else:
                    I("dve", "tensor_tensor", out=r32[:, m, ts], in0=r32[:, m, ts], in1=ps[:], op=ALU.add, reads=[kps, R(m, tb)], writes=[R(m, tb)])


def moe(cx, dr, r32, xb, big, sa, wload):
    P = cx.P
    I = P.I
    sb = cx.sb
    R = lambda c, tb: f"r32_{c}_{tb}"
    NTT = TP // 128
    if not hasattr(cx, "moe_tiles"):
        cx.moe_tiles = dict(
            rt=sb("rt32", [128, 8, 8], F32), lg=sb("lg", [128, NTT, 8], F32), top=sb("top8", [128, 8], F32),
            w0=sb("w0", [128, 1], F32), w1=sb("w1", [128, 1], F32), dw=sb("dw", [128, 1], F32), dm=sb("dm", [128, 1], F32),
            eq=sb("eq", [128, 8], F32), ge=sb("ge", [128, 8], F32), wtok=sb("wtok", [128, NTT, 8], F32),
            wT=sb("wT", [8, TP], F32), sel=sb("sel", [8, 8, 128], F32), ident=sb("ident", [128, 128], F32),
            wbc=[sb(f"wbc{i}", [128, TP], F32) for i in range(2)])
        t = cx.moe_tiles
        P.dma("sp", t["rt"][:], dr["router"].rearrange("(c p) e -> p c e", p=128), writes=["rt32"])
        P.dma("sp", t["sel"][:], dr["sel"], writes=["sel"])
        P.dma("sp", t["ident"][:], dr["ident"], writes=["ident"])
    t = cx.moe_tiles
    rt, lg, top, w0, w1, dw, dm, eq, ge, wtok, wT, sel, ident, wbc = (t[k] for k in
        ("rt", "lg", "top", "w0", "w1", "dw", "dm", "eq", "ge", "wtok", "wT", "sel", "ident", "wbc"))
    psr, kr = cx.bank()
    for tt in range(NTT):
        for c in range(8):
            I("pe", "matmul", psr[:, tt * 8:(tt + 1) * 8], lhsT=r32[:, c, tt * 128:(tt + 1) * 128], rhs=rt[:, c, :], start=(c == 0), stop=(c == 7),
              reads=["rt32", R(c, tt // 4)], writes=[kr])
    I("dve", "tensor_copy", out=lg[:].rearrange("p a b -> p (a b)"), in_=psr[:, 0:NTT * 8], reads=[kr], writes=["lg"])
    psts = [cx.bank() for _ in range(TB)]
    for tt in range(NTT):
        pst, kt = psts[tt // 4]
        I("dve", "max", out=top[:], in_=lg[:, tt, :], reads=["lg"], writes=["top8"])
        I("dve", "tensor_tensor", out=dm[:], in0=top[:, 0:1], in1=top[:, 1:2], op=ALU.subtract, reads=["top8"], writes=["dm"])
        I("act", "activation", out=w0[:], in_=dm[:], func=AF.Sigmoid, reads=["dm"], writes=["w0"])
        I("dve", "tensor_scalar", out=w1[:], in0=w0[:], scalar1=-1.0, scalar2=1.0, op0=ALU.mult, op1=ALU.add, reads=["w0"], writes=["w1"])
        I("dve", "tensor_tensor", out=dw[:], in0=w0[:], in1=w1[:], op=ALU.subtract, reads=["w0", "w1"], writes=["dw"])
        I("dve", "tensor_scalar", out=eq[:], in0=lg[:, tt, :], scalar1=top[:, 0:1], scalar2=None, op0=ALU.is_equal, reads=["lg", "top8"], writes=["eq"])
        I("dve", "tensor_scalar", out=ge[:], in0=lg[:, tt, :], scalar1=top[:, 1:2], scalar2=None, op0=ALU.is_ge, reads=["lg", "top8"], writes=["ge"])
        I("dve", "tensor_scalar", out=eq[:], in0=eq[:], scalar1=dw[:, 0:1], scalar2=w1[:, 0:1], op0=ALU.mult, op1=ALU.add,
          reads=["eq", "dw", "w1"], writes=["eq"])
        I("dve", "tensor_tensor", out=wtok[:, tt, :], in0=eq[:], in1=ge[:], op=ALU.mult, reads=["eq", "ge"], writes=["wtok"])
        I("pe", "transpose", out=pst[0:8, (tt % 4) * 128:(tt % 4 + 1) * 128], in_=wtok[:, tt, :], identity=ident[:], reads=["wtok", "ident"], writes=[kt])
    for tb in range(TB):
        I("dve", "tensor_copy", out=wT[:, TS(tb)], in_=psts[tb][0][0:8, :], reads=[psts[tb][1]], writes=["wT"])
    for c in range(8):
        for tb in range(TB):
            I("pool", "tensor_scalar", out=r32[:, c, TS(tb)], in0=r32[:, c, TS(tb)], scalar1=ALPHA, scalar2=None, op0=ALU.mult,
              reads=[R(c, tb)], writes=[R(c, tb)])
    for ex in range(NEXP):
        wb_ = wbc[ex % 2]
        kwb = f"wbc{ex % 2}"
        for tb in range(TB):
            ps, kps = cx.bank()
            I("pe", "matmul", ps[:], lhsT=sel[:, ex, :], rhs=wT[:, TS(tb)], start=True, stop=True, reads=["sel", "wT"], writes=[kps])
            I("act", "activation", out=wb_[:, TS(tb)], in_=ps[:], func=AF.Copy, reads=[kps], writes=[kwb])
        ffn_expert(cx, dr["w13"][ex], dr["w2"][ex], F_EXP, r32, xb, big, sa, wload, (wb_, kwb), False)


EPS = 1e-5
C_CQ, C_CKV, C_KR, C_KRS = 0, 256, 384, 416
C_SWQ, C_SWK, C_SWV = 448, 512, 576
C_HQ, C_HF, C_HG, C_HI = 640, 704, 768, 832
C_SBQ, C_SBK, C_SBV = 896, 960, 1024
NCOL = 1088


_UID = globals().get('_UID', [0])


class Ctx1:
    def __init__(self, nc, P, st):
        self.nc, self.P, self.st = nc, P, st
        _UID[0] += 1
        self.uid = _UID[0]
        self.banks = [st.enter_context(nc.psum_tensor(f"u{_UID[0]}_ps{i}", [128, 512], F32)) for i in range(8)]
        self.free = list(range(8))
        self.rr = 0

    def sb(self, name, shape, dt):
        return self.st.enter_context(self.nc.sbuf_tensor(f"u{self.uid}_sb_" + name, shape, dt))

    def bank(self):
        i = self.free[self.rr % len(self.free)]
        self.rr += 1
        return self.banks[i], f"ps{i}"

    def hold(self):
        i = self.free.pop(self.rr % len(self.free))
        return self.banks[i], f"ps{i}", i

    def release(self, i):
        self.free.append(i)
        self.free.sort()


def phase1(cx, dr, S, layer, do=("mla", "swa", "hgrn", "sb")):
    nc, P = cx.nc, cx.P
    I = P.I
    sb = cx.sb
    NB = S // 512
    W = sb("W", [128, 8, NCOL], BF16)
    P.dma("pool", W[:], dr["wh"].rearrange("(c p) n -> p c n", p=128), writes=["W"])
    xbuf = [sb(f"xbuf{i}", [128, 8, 512], BF16) for i in range(2)]
    ones = sb("ones", [128, 128], BF16)
    I("dve", "memset", ones[:], 1.0, writes=["ones"])
    masks = sb("masks", [128, 8, 512], BF16)
    P.dma("sp", masks[:], dr["masks"], writes=["masks"])
    cx.xcnt = 0

    def xload(tb):
        i = cx.xcnt % 2
        cx.xcnt += 1
        for csl, src in dr["xsrc"](tb):
            P.dma("pool", xbuf[i][:, csl, :], src, writes=[f"xbuf{i}"])
        return xbuf[i], f"xbuf{i}"

    def proj_fm(xb_, kx, col0, M, out_ps, kps, tslice=slice(0, 512)):
        for c in range(8):
            I("pe", "matmul", out_ps, lhsT=W[:, c, col0:col0 + M], rhs=xb_[:, c, tslice], start=(c == 0), stop=(c == 7), reads=["W", kx], writes=[kps])

    def proj_tm(xb_, kx, col0, N, out_ps, kps, tok0, ntok):
        for c in range(8):
            I("pe", "matmul", out_ps, lhsT=xb_[:, c, tok0:tok0 + ntok], rhs=W[:, c, col0:col0 + N], start=(c == 0), stop=(c == 7), reads=["W", kx], writes=[kps])

    QT = sb("QT", [128, S], BF16)
    KT = sb("KT", [128, S], BF16)
    Vm = sb("Vm", [128, S // 128, 64], BF16)
    PTn = 4
    PT = [sb(f"PT{i}", [128, 512], BF16) for i in range(PTn)]
    rden = sb("rden", [64, 512], F32)
    yout = [sb(f"yout{i}", [64, 512], BF16) for i in range(2)]
    tA = [sb(f"tA{i}", [128, 512], F32) for i in range(4)]
    tB = [sb(f"tB{i}", [128, 512], BF16) for i in range(4)]
    cx.ycnt = 0

    def ystore(branch, qb, src_ps, kps, scale_ap, kscale):
        i = cx.ycnt % 2
        cx.ycnt += 1
        I("dve", "tensor_tensor", out=yout[i][:], in0=src_ps, in1=scale_ap, op=ALU.mult, reads=[kps, kscale], writes=[f"yout{i}"])
        P.dma("sp", dr["ydst"](branch, qb), yout[i][:], reads=[f"yout{i}"], writes=["ybr"])

    if "mla" in do:
        wuq = sb("wuq", [128, 2, 128], BF16)
        wukv = sb("wukv", [128, 128], BF16)
        P.dma("pool", wuq[:], dr["wuq"].rearrange("(c p) n -> p c n", p=128), writes=["wuq"])
        P.dma("pool", wukv[:], dr["wukv"], writes=["wukv"])
        qn = sb("qn", [128, 2], F32)
        kvn = sb("kvn", [128, 1], F32)
        rc = sb("ropec", [128, 2], F32)
        P.dma("sp", qn[:], dr["qnorm"], writes=["qn"])
        P.dma("sp", kvn[:], dr["kvnorm"], writes=["kvn"])
        P.dma("sp", rc[:], dr["ropec"], writes=["ropec"])
        posi = sb("posi", [128, 512], I32)
        cqn = sb("cqn", [128, 2, 512], BF16)
        ckvn = sb("ckvn", [128, 512], BF16)
        RS = slice(64, 96)
        for tb in range(NB):
            ts = slice(tb * 512, (tb + 1) * 512)
            xb_, kx = xload(tb)
            P.dma("sp", posi[RS, :], dr["posb"][:, ts], writes=["posi"])
            tt, tf = tA[0], tA[1]
            I("dve", "tensor_copy", out=tt[RS, :], in_=posi[RS, :], reads=["posi"], writes=["tA0"])
            I("dve", "tensor_scalar", out=tt[RS, :], in0=tt[RS, :], scalar1=rc[RS, 0:1], scalar2=None, op0=ALU.mult, reads=["tA0", "ropec"], writes=["tA0"])
            sincos = []
            for which, off in (("sin", 0.0), ("cos", 0.25)):
                dst = tA[2] if which == "sin" else tA[3]
                kd = "tA2" if which == "sin" else "tA3"
                I("dve", "tensor_scalar", out=dst[RS, :], in0=tt[RS, :], scalar1=off, scalar2=None, op0=ALU.add, reads=["tA0"], writes=[kd])
                I("dve", "tensor_copy", out=posi[RS, :], in_=dst[RS, :], reads=[kd], writes=["posi"])
                I("dve", "tensor_copy", out=tf[RS, :], in_=posi[RS, :], reads=["posi"], writes=["tA1"])
                I("dve", "tensor_tensor", out=dst[RS, :], in0=dst[RS, :], in1=tf[RS, :], op=ALU.subtract, reads=[kd, "tA1"], writes=[kd])
                I("dve", "tensor_scalar", out=tf[RS, :], in0=dst[RS, :], scalar1=0.5, scalar2=None, op0=ALU.is_gt, reads=[kd], writes=["tA1"])
                I("dve", "tensor_tensor", out=dst[RS, :], in0=dst[RS, :], in1=tf[RS, :], op=ALU.subtract, reads=[kd, "tA1"], writes=[kd])
                I("dve", "tensor_scalar", out=tf[RS, :], in0=dst[RS, :], scalar1=-0.5, scalar2=None, op0=ALU.is_lt, reads=[kd], writes=["tA1"])
                I("dve", "tensor_tensor", out=dst[RS, :], in0=dst[RS, :], in1=tf[RS, :], op=ALU.add, reads=[kd, "tA1"], writes=[kd])
                I("act", "activation", out=dst[RS, :], in_=dst[RS, :], func=AF.Sin, scale=2.0 * math.pi, reads=[kd], writes=[kd])
            sin_t, cos_t = tA[2], tA[3]
            pcq = [cx.bank() for _ in range(2)]
            for j in range(2):
                proj_fm(xb_, kx, C_CQ + j * 128, 128, pcq[j][0][:], pcq[j][1])
                I("act", "activation", out=tB[2 + j][:], in_=pcq[j][0][:], func=AF.Square, reads=[pcq[j][1]], writes=[f"tB{2 + j}"])
            pss, kss = cx.bank()
            for j in range(2):
                I("pe", "matmul", pss[:], lhsT=ones[:], rhs=tB[2 + j][:], start=(j == 0), stop=(j == 1), reads=["ones", f"tB{2 + j}"], writes=[kss])
            rq = tA[1]
            I("act", "activation", out=rq[:], in_=pss[:], func=AF.Ln, scale=1.0 / 256, bias=EPS, reads=[kss], writes=["tA1"])
            I("act", "activation", out=rq[:], in_=rq[:], func=AF.Exp, scale=-0.5, reads=["tA1"], writes=["tA1"])
            for j in range(2):
                I("dve", "scalar_tensor_tensor", out=cqn[:, j, :], in0=pcq[j][0][:], scalar=qn[:, j:j + 1], in1=rq[:], op0=ALU.mult, op1=ALU.mult,
                  reads=[pcq[j][1], "qn", "tA1"], writes=[f"cqn{j}"])
            pq, kq = cx.bank()
            for j in range(2):
                I("pe", "matmul", pq[0:96, :], lhsT=wuq[:, j, 0:96], rhs=cqn[:, j, :], start=(j == 0), stop=(j == 1), reads=["wuq", f"cqn{j}"], writes=[kq])
            pq2, kq2 = cx.bank()
            for j in range(2):
                I("pe", "matmul", pq2[64:96, :], lhsT=wuq[:, j, 96:128], rhs=cqn[:, j, :], start=(j == 0), stop=(j == 1), reads=["wuq", f"cqn{j}"], writes=[kq2])
            I("act", "activation", out=QT[0:64, ts], in_=pq[0:64, :], func=AF.Copy, reads=[kq], writes=[f"QT{tb}"])
            I("dve", "tensor_tensor", out=tt[RS, :], in0=pq[RS, :], in1=cos_t[RS, :], op=ALU.mult, reads=[kq, "tA3"], writes=["tA0"])
            I("dve", "scalar_tensor_tensor", out=tf[RS, :], in0=pq2[RS, :], scalar=rc[RS, 1:2], in1=sin_t[RS, :], op0=ALU.mult, op1=ALU.mult,
              reads=[kq2, "ropec", "tA2"], writes=["tA1"])
            I("pool", "tensor_tensor", out=QT[RS, ts], in0=tt[RS, :], in1=tf[RS, :], op=ALU.add, reads=["tA0", "tA1"], writes=[f"QT{tb}"])
            pkv, kkv = cx.bank()
            proj_fm(xb_, kx, C_CKV, 128, pkv[:], kkv)
            I("act", "activation", out=tB[2][:], in_=pkv[:], func=AF.Square, reads=[kkv], writes=["tB2"])
            pss2, kss2 = cx.bank()
            I("pe", "matmul", pss2[:], lhsT=ones[:], rhs=tB[2][:], start=True, stop=True, reads=["ones", "tB2"], writes=[kss2])
            I("act", "activation", out=rq[:], in_=pss2[:], func=AF.Ln, scale=1.0 / 128, bias=EPS, reads=[kss2], writes=["tA1"])
            I("act", "activation", out=rq[:], in_=rq[:], func=AF.Exp, scale=-0.5, reads=["tA1"], writes=["tA1"])
            I("dve", "scalar_tensor_tensor", out=ckvn[:], in0=pkv[:], scalar=kvn[:, 0:1], in1=rq[:], op0=ALU.mult, op1=ALU.mult,
              reads=[kkv, "kvn", "tA1"], writes=["ckvn"])
            pk, kk = cx.bank()
            I("pe", "matmul", pk[0:64, :], lhsT=wukv[:, 0:64], rhs=ckvn[:], start=True, stop=True, reads=["wukv", "ckvn"], writes=[kk])
            I("act", "activation", out=KT[0:64, ts], in_=pk[0:64, :], func=AF.Copy, reads=[kk], writes=[f"KT{tb}"])
            pkr, kkr = cx.bank()
            proj_fm(xb_, kx, C_KR, 32, pkr[RS, :], kkr)
            pkr2, kkr2 = cx.bank()
            proj_fm(xb_, kx, C_KRS, 32, pkr2[RS, :], kkr2)
            I("dve", "tensor_tensor", out=tt[RS, :], in0=pkr[RS, :], in1=cos_t[RS, :], op=ALU.mult, reads=[kkr, "tA3"], writes=["tA0"])
            I("dve", "scalar_tensor_tensor", out=tf[RS, :], in0=pkr2[RS, :], scalar=rc[RS, 1:2], in1=sin_t[RS, :], op0=ALU.mult, op1=ALU.mult,
              reads=[kkr2, "ropec", "tA2"], writes=["tA1"])
            I("pool", "tensor_tensor", out=KT[RS, ts], in0=tt[RS, :], in1=tf[RS, :], op=ALU.add, reads=["tA0", "tA1"], writes=[f"KT{tb}"])
            pv, kv_ = cx.bank()
            for sbk in range(4):
                I("pe", "matmul", pv[:, sbk * 64:(sbk + 1) * 64], lhsT=ckvn[:, sbk * 128:(sbk + 1) * 128], rhs=wukv[:, 64:128], start=True, stop=True,
                  reads=["wukv", "ckvn"], writes=[kv_])
            I("act", "activation", out=Vm[:, tb * 4:(tb + 1) * 4, :], in_=pv[:, 0:256].rearrange("p (a b) -> p a b", a=4), func=AF.Copy,
              reads=[kv_], writes=[f"Vm{tb}"])
        scale = 96 ** -0.5
        for qb in range(NB):
            pso, kpo, io = cx.hold()
            psd, kpd, id_ = cx.hold()
            qs = slice(qb * 512, (qb + 1) * 512)
            nt = 4 * (qb + 1)

            def stA(t):
                ps, kps = cx.bank()
                I("pe", "matmul", ps[:], lhsT=KT[0:96, t * 128:(t + 1) * 128], rhs=QT[0:96, qs], start=True, stop=True,
                  reads=[f"KT{t // 4}", f"QT{qb}"], writes=[kps])
                p = PT[t % PTn]
                I("act", "activation", out=p[:], in_=ps[:], func=AF.Exp, scale=scale, reads=[kps], writes=[f"PT{t % PTn}"])
                if t >= 4 * qb:
                    I("pool", "tensor_tensor", out=p[:], in0=p[:], in1=masks[:, t - 4 * qb, :], op=ALU.mult, reads=[f"PT{t % PTn}", "masks"], writes=[f"PT{t % PTn}"])

            def stB(t):
                p = PT[t % PTn]
                I("pe", "matmul", pso[0:64, :], lhsT=Vm[:, t, :], rhs=p[:], start=(t == 0), stop=(t == nt - 1), reads=[f"Vm{t // 4}", f"PT{t % PTn}"], writes=[kpo])
                I("pe", "matmul", psd[0:64, :], lhsT=ones[:, 0:64], rhs=p[:], start=(t == 0), stop=(t == nt - 1), reads=["ones", f"PT{t % PTn}"], writes=[kpd])

            for s_ in range(nt + 2):
                if s_ < nt:
                    stA(s_)
                if 0 <= s_ - 2 < nt:
                    stB(s_ - 2)
            I("dve", "reciprocal", out=rden[:], in_=psd[0:64, :], reads=[kpd], writes=["rden"])
            ystore(0, qb, pso[0:64, :], kpo, rden[:], "rden")
            cx.release(io)
            cx.release(id_)

    if "sb" in do:
        tri = sb("tri", [128, 2, 128], BF16)
        P.dma("sp", tri[:], dr["tri"], writes=["tri"])
        for tb in range(NB):
            ts = slice(tb * 512, (tb + 1) * 512)
            xb_, kx = xload(tb)
            pq, kq = cx.bank()
            proj_fm(xb_, kx, C_SBQ, 64, pq[0:64, :], kq)
            I("act", "activation", out=QT[0:64, ts], in_=pq[0:64, :], func=AF.Copy, scale=0.125, reads=[kq], writes=[f"QT{tb}"])
            pk, kk = cx.bank()
            proj_fm(xb_, kx, C_SBK, 64, pk[0:64, :], kk)
            I("act", "activation", out=KT[0:64, ts], in_=pk[0:64, :], func=AF.Copy, reads=[kk], writes=[f"KT{tb}"])
            pv, kv_ = cx.bank()
            for sbk in range(4):
                proj_tm(xb_, kx, C_SBV, 64, pv[:, sbk * 64:(sbk + 1) * 64], kv_, sbk * 128, 128)
            I("act", "activation", out=Vm[:, tb * 4:(tb + 1) * 4, :], in_=pv[:, 0:256].rearrange("p (a b) -> p a b", a=4), func=AF.Copy,
              reads=[kv_], writes=[f"Vm{tb}"])
        S32 = sb("S32", [128, 512], F32)
        carry = [sb(f"carry{i}", [128, 512], BF16) for i in range(3)]
        spb = [sb(f"spb{i}", [128, 512], BF16) for i in range(3)]
        AT = [sb(f"AT{i}", [128, 512], BF16) for i in range(3)]
        for qb in range(NB):
            pso, kpo, io = cx.hold()
            qs = slice(qb * 512, (qb + 1) * 512)
            nt = 4 * (qb + 1)
            zb = {}

            def kb_of(t):
                return 4 * qb + 3 - t

            def stA(t):
                kb = kb_of(t)
                ps, kps = cx.bank()
                zb[t] = (ps, kps)
                I("pe", "matmul", ps[:], lhsT=KT[0:64, kb * 128:(kb + 1) * 128], rhs=QT[0:64, qs], start=True, stop=True, skip_group_check=True,
                  reads=[f"KT{kb // 4}", f"QT{qb}"], writes=[kps])
                e1 = tA[t % 2]
                I("act", "activation", out=e1[:], in_=ps[:], func=AF.Exp, reads=[kps], writes=[f"tA{t % 2}"])
                sp_ = spb[t % 3]
                I("act", "activation", out=sp_[:], in_=e1[:], func=AF.Ln, bias=1.0, reads=[f"tA{t % 2}"], writes=[f"spb{t % 3}"])
                if t < 4:
                    I("pool", "tensor_tensor", out=sp_[:], in0=sp_[:], in1=masks[:, 4 + (3 - t), :], op=ALU.mult, reads=[f"spb{t % 3}", "masks"], writes=[f"spb{t % 3}"])
                if t == 0:
                    I("dve", "tensor_copy", out=S32[:], in_=sp_[:], reads=[f"spb{t % 3}"], writes=["S32"])
                else:
                    I("dve", "tensor_tensor", out=S32[:], in0=S32[:], in1=sp_[:], op=ALU.add, reads=["S32", f"spb{t % 3}"], writes=["S32"])
                if t + 1 < nt:
                    I("pool", "tensor_copy", out=carry[(t + 1) % 3][:], in_=S32[:], reads=["S32"], writes=[f"carry{(t + 1) % 3}"])

            def stB(t):
                ps, kps = zb[t]
                I("pe", "matmul", ps[:], lhsT=tri[:, 0, :], rhs=spb[t % 3][:], start=False, stop=(t == 0), skip_group_check=True,
                  reads=["tri", f"spb{t % 3}"], writes=[kps])
                if t > 0:
                    I("pe", "matmul", ps[:], lhsT=tri[:, 1, :], rhs=carry[t % 3][:], start=False, stop=True, skip_group_check=True,
                      reads=["tri", f"carry{t % 3}"], writes=[kps])
                a = AT[t % 3]
                I("act", "activation", out=a[:], in_=ps[:], func=AF.Exp, reads=[kps], writes=[f"AT{t % 3}"])
                if t < 4:
                    I("pool", "tensor_tensor", out=a[:], in0=a[:], in1=masks[:, 4 + (3 - t), :], op=ALU.mult, reads=[f"AT{t % 3}", "masks"], writes=[f"AT{t % 3}"])

            def stC(t):
                kb = kb_of(t)
                I("pe", "matmul", pso[0:64, :], lhsT=Vm[:, kb, :], rhs=AT[t % 3][:], start=(t == 0), stop=(t == nt - 1), reads=[f"Vm{kb // 4}", f"AT{t % 3}"], writes=[kpo])

            for s_ in range(nt + 2):
                if s_ < nt:
                    stA(s_)
                if 0 <= s_ - 1 < nt:
                    stB(s_ - 1)
                if 0 <= s_ - 2 < nt:
                    stC(s_ - 2)
            i = cx.ycnt % 2
            cx.ycnt += 1
            I("act", "activation", out=yout[i][:], in_=pso[0:64, :], func=AF.Copy, reads=[kpo], writes=[f"yout{i}"])
            P.dma("sp", dr["ydst"](3, qb), yout[i][:], reads=[f"yout{i}"], writes=["ybr"])
            cx.release(io)

    if "swa" in do:
        swc = sb("swc", [128, 34], F32)
        P.dma("sp", swc[:], dr["swc"], writes=["swc"])
        dcur = sb("dcur", [128, 128], F32)
        dprev = sb("dprev", [128, 128], F32)
        P.dma("sp", dcur[:], dr["dist"][0], writes=["dcur"])
        P.dma("sp", dprev[:], dr["dist"][1], writes=["dprev"])
        diff = sb("swdiff", [128, 31], F32)
        I("dve", "tensor_tensor", out=diff[:], in0=swc[:, 1:32], in1=swc[:, 0:31], op=ALU.subtract, reads=["swc"], writes=["swdiff"])
        esink = sb("esink", [128, 1], F32)
        I("act", "activation", out=esink[:], in_=swc[:, 32:33], func=AF.Exp, reads=["swc"], writes=["esink"])
        EB = [sb(f"EB{i}", [128, 512], F32) for i in range(3)]
        acc = tA[0]
        stp = tA[1]
        los = dr["bucket_lo"]
        for which, dt_, kd in ((0, dcur, "dcur"), (1, dprev, "dprev")):
            I("dve", "tensor_scalar", out=acc[:, 0:128], in0=dt_[:], scalar1=0.0, scalar2=swc[:, 0:1], op0=ALU.mult, op1=ALU.add, reads=[kd, "swc"], writes=["tA0"])
            for b in range(1, 32):
                I("dve", "tensor_scalar", out=stp[:, 0:128], in0=dt_[:], scalar1=float(los[b - 1]), scalar2=diff[:, b - 1:b], op0=ALU.is_ge, op1=ALU.mult,
                  reads=[kd, "swdiff"], writes=["tA1"])
                I("dve", "tensor_tensor", out=acc[:, 0:128], in0=acc[:, 0:128], in1=stp[:, 0:128], op=ALU.add, reads=["tA0", "tA1"], writes=["tA0"])
            I("act", "activation", out=acc[:, 0:128], in_=acc[:, 0:128], func=AF.Exp, reads=["tA0"], writes=["tA0"])
            if which == 0:
                I("dve", "tensor_scalar", out=stp[:, 0:128], in0=dt_[:], scalar1=0.0, scalar2=None, op0=ALU.is_ge, reads=[kd], writes=["tA1"])
            else:
                I("dve", "tensor_scalar", out=stp[:, 0:128], in0=dt_[:], scalar1=127.0, scalar2=None, op0=ALU.is_le, reads=[kd], writes=["tA1"])
            for r in range(4):
                I("dve", "tensor_tensor", out=EB[which][:, r * 128:(r + 1) * 128], in0=acc[:, 0:128], in1=stp[:, 0:128], op=ALU.mult,
                  reads=["tA0", "tA1"], writes=[f"EB{which}"])
        I("pool", "tensor_copy", out=EB[2][:], in_=EB[1][:], reads=["EB1"], writes=["EB2"])
        I("pool", "memset", EB[2][:, 0:128], 0.0, reads=[], writes=["EB2"])
        for tb in range(NB):
            ts = slice(tb * 512, (tb + 1) * 512)
            xb_, kx = xload(tb)
            pq, kq = cx.bank()
            proj_fm(xb_, kx, C_SWQ, 64, pq[0:64, :], kq)
            I("act", "activation", out=QT[0:64, ts], in_=pq[0:64, :], func=AF.Copy, scale=0.125, reads=[kq], writes=[f"QT{tb}"])
            pk, kk = cx.bank()
            proj_fm(xb_, kx, C_SWK, 64, pk[0:64, :], kk)
            I("act", "activation", out=KT[0:64, ts], in_=pk[0:64, :], func=AF.Copy, reads=[kk], writes=[f"KT{tb}"])
            pv, kv_ = cx.bank()
            for sbk in range(4):
                proj_tm(xb_, kx, C_SWV, 64, pv[:, sbk * 64:(sbk + 1) * 64], kv_, sbk * 128, 128)
            I("act", "activation", out=Vm[:, tb * 4:(tb + 1) * 4, :], in_=pv[:, 0:256].rearrange("p (a b) -> p a b", a=4), func=AF.Copy,
              reads=[kv_], writes=[f"Vm{tb}"])
        for g in range(NB):
            qs = slice(g * 512, (g + 1) * 512)
            pc, kc = cx.bank()
            pp, kp = cx.bank()
            for j in range(4):
                blk = 4 * g + j
                I("pe", "matmul", pc[:, j * 128:(j + 1) * 128], lhsT=KT[0:64, blk * 128:(blk + 1) * 128], rhs=QT[0:64, blk * 128:(blk + 1) * 128],
                  start=True, stop=True, reads=[f"KT{g}", f"QT{g}"], writes=[kc])
                pb = max(blk - 1, 0)
                I("pe", "matmul", pp[:, j * 128:(j + 1) * 128], lhsT=KT[0:64, pb * 128:(pb + 1) * 128], rhs=QT[0:64, blk * 128:(blk + 1) * 128],
                  start=True, stop=True, reads=[f"KT{pb // 4}", f"QT{g}"], writes=[kp])
            ec, ep = tA[2], tA[3]
            I("act", "activation", out=ec[:], in_=pc[:], func=AF.Exp, reads=[kc], writes=["tA2"])
            I("act", "activation", out=ep[:], in_=pp[:], func=AF.Exp, reads=[kp], writes=["tA3"])
            pcb, ppb = tB[0], tB[1]
            I("dve", "tensor_tensor", out=pcb[:], in0=ec[:], in1=EB[0][:], op=ALU.mult, reads=["tA2", "EB0"], writes=["tB0"])
            ebp = 2 if g == 0 else 1
            I("pool", "tensor_tensor", out=ppb[:], in0=ep[:], in1=EB[ebp][:], op=ALU.mult, reads=["tA3", f"EB{ebp}"], writes=["tB1"])
            pso, kpo = cx.bank()
            psd, kpd = cx.bank()
            for j in range(4):
                blk = 4 * g + j
                pb = max(blk - 1, 0)
                cs = slice(j * 128, (j + 1) * 128)
                I("pe", "matmul", pso[0:64, cs], lhsT=Vm[:, blk, :], rhs=pcb[:, cs], start=True, stop=False, reads=[f"Vm{g}", "tB0"], writes=[kpo])
                I("pe", "matmul", pso[0:64, cs], lhsT=Vm[:, pb, :], rhs=ppb[:, cs], start=False, stop=True, reads=[f"Vm{pb // 4}", "tB1"], writes=[kpo])
                I("pe", "matmul", psd[0:64, cs], lhsT=ones[:, 0:64], rhs=pcb[:, cs], start=True, stop=False, reads=["ones", "tB0"], writes=[kpd])
                I("pe", "matmul", psd[0:64, cs], lhsT=ones[:, 0:64], rhs=ppb[:, cs], start=False, stop=True, reads=["ones", "tB1"], writes=[kpd])
            I("dve", "tensor_scalar", out=rden[:], in0=psd[0:64, :], scalar1=esink[0:64, 0:1], scalar2=None, op0=ALU.add, reads=[kpd, "esink"], writes=["rden"])
            I("dve", "reciprocal", out=rden[:], in_=rden[:], reads=["rden"], writes=["rden"])
            ystore(1, g, pso[0:64, :], kpo, rden[:], "rden")

    if "hgrn" in do:
        hgrn(cx, dr, S, layer, W, xload, proj_fm, proj_tm, ones, tA, tB, yout)


def hgrn(cx, dr, S, layer, W, xload, proj_fm, proj_tm, ones, tA, tB, yout):
    import os
    HG = int(os.environ.get("HG_STOP", "99"))
    nc, P = cx.nc, cx.P
    I = P.I
    sb = cx.sb
    SEG = min(2048, S)
    NSEG = S // SEG
    NBS = SEG // 512
    NCS = SEG // 32
    hc = sb("hgc", [64, 4], F32)
    P.dma("sp", hc[:], dr["hgc"], writes=["hgc"])
    ident = sb("identh", [64, 64], F32)
    P.dma("sp", ident[:], dr["ident64"], writes=["identh"])
    rmask = sb("rmask", [64, 512], F32)
    P.dma("sp", rmask[:], dr["rmask"], writes=["rmask"])
    m64 = sb("m64", [64, 512], F32)
    P.dma("sp", m64[:], dr["m64"], writes=["m64"])
    lb = sb("lb", [64, 1], F32)
    oml = sb("oml", [64, 1], F32)
    e2 = sb("e2", [64, 2], F32)
    I("act", "activation", out=e2[:], in_=hc[:, 0:2], func=AF.Exp, reads=["hgc"], writes=["e2"])
    I("dve", "tensor_tensor", out=lb[:], in0=e2[:, 0:1], in1=e2[:, 1:2], op=ALU.add, reads=["e2"], writes=["lb"])
    I("dve", "reciprocal", out=lb[:], in_=lb[:], reads=["lb"], writes=["lb"])
    I("dve", "tensor_tensor", out=lb[:], in0=lb[:], in1=e2[:, 1:2], op=ALU.mult, reads=["lb", "e2"], writes=["lb"])
    I("dve", "tensor_scalar", out=lb[:], in0=lb[:], scalar1=float(layer), scalar2=None, op0=ALU.mult, reads=["lb"], writes=["lb"])
    I("dve", "tensor_scalar", out=oml[:], in0=lb[:], scalar1=-1.0, scalar2=1.0, op0=ALU.mult, op1=ALU.add, reads=["lb"], writes=["oml"])
    QD = sb("QD", [64, SEG], BF16)
    KD = sb("KD", [64, SEG], BF16)
    SG = sb("SG", [64, SEG], BF16)
    KEt = sb("KEt", [64, 2, SEG // 64, 64], BF16)
    I("pool", "memset", KEt[:], 0.0, writes=[f"KEt{bi}" for bi in range(NBS)])
    Vt = sb("Vt", [64, SEG // 64, 64], BF16)
    Us = sb("Us", [64, NCS, 64], BF16)
    Ds = sb("Ds", [64, NCS], F32)
    Sst = sb("Sst", [64, NCS + 1, 64], F32)
    Sb = sb("Sb", [64, NCS, 64], BF16)
    attm = sb("attm", [64, 512], BF16)
    hq, hf, hl, hk, hb, he = (sb(f"h{n}", [64, 512], F32) for n in ("q", "f", "l", "k", "b", "e"))
    ke = sb("hke", [64, 512], F32)
    rs = sb("hrs", [64, 512], F32)
    sqb = sb("hsq", [64, 512], BF16)
    I("dve", "memset", Sst[:, 0, :], 0.0, writes=["Sst"])
    for sg in range(NSEG):
        for bi in range(NBS):
            tb = sg * NBS + bi
            ls = slice(bi * 512, (bi + 1) * 512)
            xb_, kx = xload(tb)
            pq, kq = cx.bank()
            proj_fm(xb_, kx, C_HQ, 64, pq[0:64, :], kq)
            pf, kf_ = cx.bank()
            proj_fm(xb_, kx, C_HF, 64, pf[0:64, :], kf_)
            pg, kg = cx.bank()
            proj_fm(xb_, kx, C_HG, 64, pg[0:64, :], kg)
            pv, kv_ = cx.bank()
            for pc_ in range(8):
                proj_tm(xb_, kx, C_HI, 64, pv[0:64, pc_ * 64:(pc_ + 1) * 64], kv_, pc_ * 64, 64)
            I("act", "activation", out=Vt[:, bi * 8:(bi + 1) * 8, :], in_=pv[0:64, :].rearrange("p (a b) -> p a b", a=8), func=AF.Copy, reads=[kv_], writes=[f"Vt{bi}"])
            I("act", "activation", out=hq[:], in_=pq[0:64, :], func=AF.Silu, reads=[kq], writes=["hq"])
            I("act", "activation", out=SG[:, ls], in_=pg[0:64, :], func=AF.Silu, reads=[kg], writes=[f"SG{bi}"])
            I("act", "activation", out=hf[:], in_=pf[0:64, :], func=AF.Sigmoid, reads=[kf_], writes=["hf"])
            I("dve", "tensor_scalar", out=hf[:], in0=hf[:], scalar1=oml[:, 0:1], scalar2=lb[:, 0:1], op0=ALU.mult, op1=ALU.add, reads=["hf", "oml", "lb"], writes=["hf"])
            I("act", "activation", out=hl[:], in_=hf[:], func=AF.Ln, reads=["hf"], writes=["hl"])
            I("pool", "tensor_scalar", out=hk[:], in0=hf[:], scalar1=-1.0, scalar2=1.0, op0=ALU.mult, op1=ALU.add, reads=["hf"], writes=["hk"])
            if HG < 1:
                continue
            I("dve", "tensor_tensor_scan", out=hb[:], data0=rmask[:], data1=hl[:], initial=0.0, op0=ALU.mult, op1=ALU.add, reads=["rmask", "hl"], writes=["hb"])
            I("act", "activation", out=he[:], in_=hb[:], func=AF.Exp, reads=["hb"], writes=["he"])
            I("dve", "tensor_tensor", out=QD[:, ls], in0=hq[:], in1=he[:], op=ALU.mult, reads=["hq", "he"], writes=[f"QD{bi}"])
            I("act", "activation", out=he[:], in_=hb[:], func=AF.Exp, scale=-1.0, reads=["hb"], writes=["he"])
            I("pool", "tensor_tensor", out=ke[:], in0=hk[:], in1=he[:], op=ALU.mult, reads=["hk", "he"], writes=["hke"])
            I("pool", "tensor_copy", out=KD[:, ls], in_=ke[:], reads=["hke"], writes=[f"KD{bi}"])
            if HG < 2:
                continue
            I("act", "activation", out=Ds[:, bi * 16:(bi + 1) * 16], in_=hb[:].rearrange("p (c t) -> p c t", t=32)[:, :, 31], func=AF.Exp,
              reads=["hb"], writes=[f"Ds{bi}"])
            for c in range(16):
                I("pool", "tensor_scalar", out=ke[:, c * 32:(c + 1) * 32], in0=ke[:, c * 32:(c + 1) * 32], scalar1=Ds[:, bi * 16 + c:bi * 16 + c + 1], scalar2=None,
                  op0=ALU.mult, reads=["hke", f"Ds{bi}"], writes=["hke"])
            if HG < 3:
                continue
            pt, kt = cx.bank()
            for pc_ in range(8):
                I("pe", "transpose", out=pt[0:64, pc_ * 64:(pc_ + 1) * 64], in_=ke[:, pc_ * 64:(pc_ + 1) * 64], identity=ident[:], reads=["hke", "identh"], writes=[kt])
            I("dve", "tensor_copy", out=KEt[0:32, 0, bi * 8:(bi + 1) * 8, :], in_=pt[0:32, :].rearrange("p (a b) -> p a b", a=8), reads=[kt], writes=[f"KEt{bi}"])
            I("dve", "tensor_copy", out=KEt[32:64, 1, bi * 8:(bi + 1) * 8, :], in_=pt[32:64, :].rearrange("p (a b) -> p a b", a=8), reads=[kt], writes=[f"KEt{bi}"])
            if HG < 4:
                continue
            pu = [cx.bank() for _ in range(2)]
            for c in range(16):
                pc_, half = c // 2, c % 2
                hs = slice(half * 32, (half + 1) * 32)
                pb_, kb_ = pu[c // 8]
                I("pe", "matmul", pb_[0:64, (c % 8) * 64:(c % 8 + 1) * 64], lhsT=KEt[:, half, bi * 8 + pc_, :], rhs=Vt[:, bi * 8 + pc_, :], start=True, stop=True,
                  reads=[f"KEt{bi}", f"Vt{bi}"], writes=[kb_])
            for k2 in range(2):
                I("act", "activation", out=Us[:, bi * 16 + k2 * 8:bi * 16 + k2 * 8 + 8, :], in_=pu[k2][0][0:64, :].rearrange("p (a b) -> p a b", a=8),
                  func=AF.Copy, reads=[pu[k2][1]], writes=[f"Us{bi}"])
        if HG < 5:
            continue
        allU = [f"Us{bi}" for bi in range(NBS)]
        allD = [f"Ds{bi}" for bi in range(NBS)]
        for v in range(64):
            I("dve", "tensor_tensor_scan", out=Sst[:, 1:NCS + 1, v], data0=Ds[:, 0:NCS], data1=Us[:, :, v], initial=Sst[:, 0, v:v + 1],
              op0=ALU.mult, op1=ALU.add, reads=allU + allD + ["Sst"], writes=["Sst"])
        I("pool", "tensor_copy", out=Sb[:], in_=Sst[:, 0:NCS, :], reads=["Sst"], writes=["Sb"])
        if HG < 6:
            continue
        for bi in range(NBS):
            tb = sg * NBS + bi
            ls = slice(bi * 512, (bi + 1) * 512)
            pa, ka = cx.bank()
            for pc_ in range(8):
                cs = slice(bi * 512 + pc_ * 64, bi * 512 + (pc_ + 1) * 64)
                I("pe", "matmul", pa[0:64, pc_ * 64:(pc_ + 1) * 64], lhsT=KD[:, cs], rhs=QD[:, cs], start=True, stop=True, reads=[f"KD{bi}", f"QD{bi}"], writes=[ka])
            I("dve", "tensor_tensor", out=attm[:], in0=pa[0:64, :], in1=m64[:], op=ALU.mult, reads=[ka, "m64"], writes=["attm"])
            po, ko = cx.bank()
            for pc_ in range(8):
                for half in range(2):
                    c = bi * 16 + pc_ * 2 + half
                    oc = slice(pc_ * 64 + half * 32, pc_ * 64 + half * 32 + 32)
                    c32 = slice(bi * 512 + pc_ * 64 + half * 32, bi * 512 + pc_ * 64 + half * 32 + 32)
                    I("pe", "matmul", po[0:64, oc], lhsT=Vt[:, bi * 8 + pc_, :], rhs=attm[:, oc], start=True, stop=False, reads=[f"Vt{bi}", "attm"], writes=[ko])
                    I("pe", "matmul", po[0:64, oc], lhsT=Sb[:, c, :], rhs=QD[:, c32], start=False, stop=True, reads=["Sb", f"QD{bi}"], writes=[ko])
            I("act", "activation", out=sqb[:], in_=po[0:64, :], func=AF.Square, reads=[ko], writes=["hsq"])
            pn, kn = cx.bank()
            I("pe", "matmul", pn[0:64, :], lhsT=ones[0:64, 0:64], rhs=sqb[:], start=True, stop=True, reads=["ones", "hsq"], writes=[kn])
            I("act", "activation", out=rs[:], in_=pn[0:64, :], func=AF.Ln, scale=1.0 / 64, bias=EPS, reads=[kn], writes=["hrs"])
            I("act", "activation", out=rs[:], in_=rs[:], func=AF.Exp, scale=-0.5, reads=["hrs"], writes=["hrs"])
            I("dve", "tensor_tensor", out=rs[:], in0=po[0:64, :], in1=rs[:], op=ALU.mult, reads=[ko, "hrs"], writes=["hrs"])
            i = cx.ycnt % 2
            cx.ycnt += 1
            I("dve", "scalar_tensor_tensor", out=yout[i][:], in0=rs[:], scalar=hc[:, 2:3], in1=SG[:, ls], op0=ALU.mult, op1=ALU.mult,
              reads=["hrs", "hgc", f"SG{bi}"], writes=[f"yout{i}"])
            P.dma("sp", dr["ydst"](2, tb), yout[i][:], reads=[f"yout{i}"], writes=["ybr"])
        if sg + 1 < NSEG:
            I("dve", "tensor_copy", out=Sst[:, 0, :], in_=Sst[:, NCS, :], reads=["Sst"], writes=["Sst"])


S_FULL = 8192
NTC = 2048
f32 = np.float32

IN_SPLITS = (('mla_cq', 256), ('mla_ckv', 128), ('mla_kr', 32), ('swa_q', 256), ('swa_k', 128), ('swa_v', 128),
             ('hgrn_q', 256), ('hgrn_f', 256), ('hgrn_i', 256), ('hgrn_g', 256), ('sb_q', 256), ('sb_k', 256), ('sb_v', 256), ('gates', 4096))
OFFS = {}
_o = 0
for _n, _w in IN_SPLITS:
    OFFS[_n] = _o
    _o += _w


def p1_consts():
    c = {}
    k = np.arange(128)[:, None]
    q = np.arange(512)[None, :]
    m = np.zeros((128, 8, 512), f32)
    for j in range(4):
        m[:, j, :] = (j * 128 + k) <= q
        m[:, 4 + j, :] = (j * 128 + k) < q
    c["masks"] = m.astype(ml_dtypes.bfloat16)
    tri = np.zeros((128, 2, 128), f32)
    jj = np.arange(128)[:, None]
    kk = np.arange(128)[None, :]
    tri[:, 0, :] = -(jj >= kk).astype(f32)
    tri[:, 1, :] = -1
    c["tri"] = tri.astype(ml_dtypes.bfloat16)
    inv = (10000.0 ** (-np.arange(16, dtype=f32) / 16)).astype(f32)
    rc = np.zeros((128, 2), f32)
    rc[64:96, 0] = np.concatenate([inv, inv]) / f32(2 * np.pi)
    rc[64:80, 1] = -1
    rc[80:96, 1] = 1
    c["ropec"] = rc
    kq = np.arange(128)
    c["dist"] = np.stack([(kq[None, :] - kq[:, None]).astype(f32), (128 + kq[None, :] - kq[:, None]).astype(f32)])
    c["ident64"] = np.eye(64, dtype=f32)
    rm = np.ones((64, 512), f32)
    rm[:, ::32] = 0
    c["rmask"] = rm
    s_ = np.arange(64)[:, None]
    t_ = np.arange(64)[None, :]
    c["m64"] = np.tile(((s_ // 32 == t_ // 32) & (s_ <= t_)).astype(f32), (1, 8))
    return c


def bucket_lo():
    n = np.arange(128)
    nf = np.maximum(n, 1).astype(f32)
    large = 16 + (np.log(nf / f32(16)) / f32(np.log(128 / 16)) * f32(16)).astype(np.int32)
    bucket = np.where(n < 16, n, np.clip(large, 0, 31))
    return [int(np.min(np.nonzero(bucket >= b)[0])) if np.any(bucket >= b) else 100000 for b in range(1, 32)]


def p1_head_inputs(inp, l, b, h):
    w_in = inp["w_in"][l]

    def cols(name, a, b_):
        return w_in[:, OFFS[name] + a: OFFS[name] + b_]
    kr = cols("mla_kr", 0, 32)
    krs = np.concatenate([kr[:, 16:32], kr[:, 0:16]], axis=1)
    g = h // 2
    hs = slice(h * 64, h * 64 + 64)
    wh = np.concatenate([cols("mla_cq", 0, 256), cols("mla_ckv", 0, 128), kr, krs,
                         cols("swa_q", h * 64, h * 64 + 64), cols("swa_k", g * 64, g * 64 + 64), cols("swa_v", g * 64, g * 64 + 64),
                         cols("hgrn_q", h * 64, h * 64 + 64), cols("hgrn_f", h * 64, h * 64 + 64), cols("hgrn_g", h * 64, h * 64 + 64),
                         cols("hgrn_i", h * 64, h * 64 + 64),
                         cols("sb_q", h * 64, h * 64 + 64), cols("sb_k", h * 64, h * 64 + 64), cols("sb_v", h * 64, h * 64 + 64)], axis=1)
    uq = inp["mla_w_uq"][l][:, h * 96:(h + 1) * 96]
    wuq = np.concatenate([uq, uq[:, 80:96], uq[:, 64:80]], axis=1)
    wukv = inp["mla_w_ukv"][l][:, h * 128:(h + 1) * 128]
    swc = np.zeros((128, 34), f32)
    swc[:, 0:32] = inp["rel_bias_table"][:, h][None, :]
    swc[:, 32] = inp["swa_sinks"][l][h]
    hgc = np.zeros((64, 4), f32)
    hgc[:, 0] = inp["hgrn_lb_logits"][0, hs]
    hgc[:, 1] = inp["hgrn_lb_logits"][1, hs]
    hgc[:, 2] = inp["hgrn_norm"][l][hs]
    pos_b = inp["positions"][b]
    return dict(wh=np.ascontiguousarray(wh), wuq=np.ascontiguousarray(wuq), wukv=np.ascontiguousarray(wukv),
                qnorm=np.ascontiguousarray(inp["mla_q_norm"][l].reshape(2, 128).T), kvnorm=np.ascontiguousarray(inp["mla_kv_norm"][l].reshape(128, 1)),
                posb=np.ascontiguousarray(np.broadcast_to(pos_b[None, :], (32, pos_b.shape[0]))).astype(np.int32), swc=swc, hgc=hgc)


def il13(w, F):
    a, g = w[:, :F].reshape(1024, F // 128, 128), w[:, F:].reshape(1024, F // 128, 128)
    return np.ascontiguousarray(np.stack([a, g], axis=2).reshape(1024, 2 * F))


def p2_shared_inputs(inp, l):
    w_in = inp["w_in"][l]
    wg = w_in[:, OFFS["gates"]:OFFS["gates"] + 4096]
    d = dict(wg=np.ascontiguousarray(wg.reshape(1024, 4, 8, 128).transpose(0, 2, 1, 3).reshape(1024, 4096)),
             wbr=np.ascontiguousarray(inp["w_branch"][l].reshape(1024, 1024)), wout=np.ascontiguousarray(inp["w_out"][l]),
             lng=np.ascontiguousarray(inp["ln_g"][l].reshape(3, 8, 128).transpose(2, 0, 1)),
             lnb=np.ascontiguousarray(inp["ln_b"][l].reshape(3, 8, 128).transpose(2, 0, 1)),
             wq=np.ascontiguousarray(inp["xa_wq"][l]), wkv=np.ascontiguousarray(inp["xa_wkv"][l]), wo=np.ascontiguousarray(inp["xa_wo"][l]))
    if l % 2 == 0:
        d.update(w13=il13(inp["ffn_w13"][l // 2], 2816), w2=np.ascontiguousarray(inp["ffn_w2"][l // 2]))
    else:
        sel = np.zeros((8, 8, 128), f32)
        for e in range(8):
            sel[e, e, :] = 1
        d.update(router=np.ascontiguousarray(inp["moe_router"][l // 2]), w13=np.stack([il13(inp["moe_w13"][l // 2][e], 3584) for e in range(8)]),
                 w2=np.ascontiguousarray(inp["moe_w2"][l // 2]), sel=sel, ident=np.eye(128, dtype=f32))
    return d


GROUPS = [[0, 1, 2, 3], [4, 5, 6, 7]]
_NPDT = {"float32": F32, "int32": I32, "bfloat16": BF16}


def core_inputs(inp, consts, c):
    b, q = c // 4, c % 4
    x = inp["x"]
    d = dict(xT0=np.ascontiguousarray(x[b].T), x32=np.ascontiguousarray(x[b, q * NTC:(q + 1) * NTC, :].T),
             memT=np.ascontiguousarray(inp["mem"][b].T))
    sel4 = np.zeros((128, 4), f32)
    sel4[:, q] = 1
    d["sel4"] = sel4
    d.update(consts)
    for l in range(2):
        p1 = p1_head_inputs(inp, l, b, q)
        if l == 1:
            p1.pop("posb")
        for k, v in p1.items():
            d[(f"l{l}_" + k) if k != "posb" else k] = v
    return d


def build_fused(sample, shared_shapes):
    nc = bass.Bass("TRN2", target_bir_lowering=False)
    S = S_FULL
    ext = {}
    for k, v in list(sample.items()) + list(shared_shapes.items()):
        ext[k] = nc.dram_tensor(k, list(v.shape), _NPDT[str(v.dtype)], kind="ExternalInput").ap()
    out = nc.dram_tensor("out", [1024, NTC], F32, kind="ExternalOutput").ap()
    ysrc = [[nc.dram_tensor(f"ysrc{l}_{i}", [256, 512], BF16) for i in range(16)] for l in range(2)]
    ygat = [[nc.dram_tensor(f"ygat{l}_{i}", [1024, 512], BF16) for i in range(16)] for l in range(2)]
    xsrc = [nc.dram_tensor(f"xsrc_{i}", [256, 512], BF16) for i in range(16)]
    xgat = [nc.dram_tensor(f"xgat_{i}", [1024, 512], BF16) for i in range(16)]
    x32mid = nc.dram_tensor("x32mid", [1024, NTC], F32).ap()
    blo = bucket_lo()
    cnames = ("masks", "tri", "ropec", "dist", "ident64", "rmask", "m64")

    with contextlib.ExitStack() as semst:
        def allgather(srcs, dsts, tag):
            cc = semst.enter_context(nc.semaphore(f"cc_{tag}"))
            with nc.Block() as block:
                @block.gpsimd
                def _(g):
                    for s_, d_ in zip(srcs, dsts):
                        g.collective_compute("AllGather", ALU.bypass, replica_groups=GROUPS, ins=[s_.ap().opt()], outs=[d_.ap().opt()]).then_inc(cc, 1)
                    g.wait_ge(cc, len(srcs))
            nc.all_engine_barrier()

        for l in range(2):
            P = Prog(nc)
            with contextlib.ExitStack() as st:
                cx = Ctx1(nc, P, st)
                dr = {k: ext[k] for k in cnames}
                dr["posb"] = ext["posb"]
                for k in ("wh", "wuq", "wukv", "qnorm", "kvnorm", "swc", "hgc"):
                    dr[k] = ext[f"l{l}_{k}"]
                dr["bucket_lo"] = blo
                if l == 0:
                    dr["xsrc"] = lambda tb: [(slice(0, 8), ext["xT0"].rearrange("(c p) t -> p c t", p=128)[:, :, tb * 512:(tb + 1) * 512])]
                else:
                    dr["xsrc"] = lambda tb: [(slice(2 * fc, 2 * fc + 2),
                                              xgat[fc * 4 + tb % 4].ap()[(tb // 4) * 256:(tb // 4 + 1) * 256, :].rearrange("(c p) t -> p c t", p=128))
                                             for fc in range(4)]
                dr["ydst"] = lambda br, qb, l=l: ysrc[l][qb].ap()[br * 64:(br + 1) * 64, :]
                phase1(cx, dr, S, l)
                P.finalize()
                P.emit(sem_stack=semst)
            nc.all_engine_barrier()
            allgather(ysrc[l], ygat[l], f"y{l}")
            P = Prog(nc)
            with contextlib.ExitStack() as st:
                cx = Ctx(nc, P, st)
                dr = dict(memT=ext["memT"], sel4=ext["sel4"])
                p2keys = ["wg", "wbr", "wout", "lng", "lnb", "wq", "wkv", "wo", "w13", "w2"] + (["router", "sel", "ident"] if l % 2 == 1 else [])
                for k in p2keys:
                    dr[k] = ext[f"l{l}_{k}"]
                dr["x32"] = ext["x32"] if l == 0 else x32mid
                dr["xo32"] = x32mid if l == 0 else out
                dr["ygather"] = lambda jq, lb, l=l: ygat[l][jq * 4 + lb].ap()
                if l == 0:
                    dr["xchunk"] = lambda fc, lb: xsrc[fc * 4 + lb].ap()
                phase2(cx, dr, l % 2 == 1, NTC)
                P.finalize()
                P.emit(sem_stack=semst)
            nc.all_engine_barrier()
            if l == 0:
                allgather(xsrc, xgat, "x")
    return nc


def kernel(**inp):
    inp = {k: np.asarray(v) for k, v in inp.items()}
    B, S, Dm = inp["x"].shape
    consts = p1_consts()
    cores = list(range(8))
    shared = {}
    for l in range(2):
        for k, v in p2_shared_inputs(inp, l).items():
            shared[f"l{l}_{k}"] = v
    in_maps = []
    for c in cores:
        d = core_inputs(inp, consts, c)
        if c == 0:
            nc = build_fused(d, shared)
        d.update(shared)
        in_maps.append(d)
    res = run_bass_kernel_spmd(nc, in_maps, core_ids=cores).results
    out = np.empty((B, S, Dm), np.float32)
    for c in cores:
        out[c // 4, (c % 4) * NTC:(c % 4 + 1) * NTC, :] = np.asarray(res[c]["out"]).T
    return out
```

```python
import math
import contextlib
import numpy as np
import ml_dtypes
import concourse.bass as bass
import concourse.mybir as mybir
from concourse.bass_utils import run_bass_kernel_spmd


F32 = mybir.dt.float32
BF16 = mybir.dt.bfloat16
I32 = mybir.dt.int32
AF = mybir.ActivationFunctionType
ALU = mybir.AluOpType
AX = mybir.AxisListType

ENGS = ("pe", "act", "dve", "pool", "sp")
SEM_CHUNK = 12000
RING = 8


class Op:
    __slots__ = ("eng", "fn", "reads", "writes", "is_dma", "deps", "pos", "gid",
                 "waits", "vc", "need_inc", "slot", "round", "cnt")

    def __init__(self, eng, fn, reads, writes, is_dma):
        self.eng = eng
        self.fn = fn
        self.reads = reads
        self.writes = writes
        self.is_dma = is_dma
        self.deps = ()
        self.waits = []
        self.vc = None
        self.need_inc = False
        self.slot = None
        self.round = None
        self.cnt = None


class Prog:
    def __init__(self, nc):
        self.nc = nc
        self.ops = []
        self.last_w = {}
        self.readers = {}

    def op(self, eng, fn, reads=(), writes=(), dma=False):
        o = Op(eng, fn, tuple(reads), tuple(writes), dma)
        o.gid = len(self.ops)
        deps = set()
        for k in o.reads:
            w = self.last_w.get(k)
            if w is not None:
                deps.add(w)
        for k in o.writes:
            w = self.last_w.get(k)
            if w is not None:
                deps.add(w)
            for r in self.readers.get(k, ()):
                deps.add(r)
        deps.discard(o.gid)
        o.deps = tuple(sorted(deps))
        for k in o.reads:
            self.readers.setdefault(k, []).append(o.gid)
        for k in o.writes:
            self.last_w[k] = o.gid
            self.readers[k] = []
        self.ops.append(o)
        return o

    def dma(self, eng, out, in_, reads=(), writes=(), **kw):
        return self.op(eng, lambda e: e.dma_start(out=out, in_=in_, **kw), reads, writes, dma=True)

    def I(self, eng, meth, *args, reads=(), writes=(), **kw):
        return self.op(eng, lambda e: getattr(e, meth)(*args, **kw), reads, writes)

    def finalize(self, final_wait_keys=()):
        nc = self.nc
        ops = self.ops
        streams = {e: [] for e in ENGS}
        for o in ops:
            o.pos = len(streams[o.eng])
            streams[o.eng].append(o)
        dma_count = {e: 0 for e in ENGS}
        for o in ops:
            if o.is_dma:
                i = dma_count[o.eng]
                dma_count[o.eng] += 1
                o.slot = (o.eng, i % RING)
                o.round = i // RING + 1
        know = {e: {} for e in ENGS}
        slot_last = {}

        def completion_key(d):
            return (d.slot, d.round) if d.is_dma else (d.eng, d.pos + 1)

        for o in ops:
            K = know[o.eng]
            need = {}
            if o.is_dma:
                prev = slot_last.get(o.slot)
                if prev is not None:
                    need[prev.slot] = (prev.round, prev)
                slot_last[o.slot] = o
            for di in o.deps:
                d = ops[di]
                if d.eng == o.eng and not d.is_dma and not o.is_dma:
                    if o.eng == "pe":
                        continue
                ck, cv = completion_key(d)
                if K.get(ck, 0) >= cv:
                    continue
                if ck not in need or need[ck][0] < cv:
                    need[ck] = (cv, d)
            items = sorted(need.items(), key=lambda kv: -kv[1][1].gid)
            for ck, (cv, d) in items:
                if K.get(ck, 0) >= cv:
                    continue
                o.waits.append(d)
                d.need_inc = True
                for k2, v2 in d.vc.items():
                    if K.get(k2, 0) < v2:
                        K[k2] = v2
            vc = dict(K)
            ck, cv = completion_key(o)
            vc[ck] = cv
            o.vc = vc
            if not o.is_dma and o.eng != "sp":
                pass
        finals = []
        for k in final_wait_keys:
            w = self.last_w.get(k)
            if w is not None:
                ops[w].need_inc = True
                finals.append(ops[w])
        sem_needed = {}
        for e in ENGS:
            c = 0
            for o in streams[e]:
                if o.is_dma:
                    o.need_inc = True
                    continue
                if o.need_inc:
                    c += 1
                    o.cnt = c
            sem_needed[e] = (c + SEM_CHUNK - 1) // SEM_CHUNK
        self.streams = streams
        self.finals = finals
        self.sem_needed = sem_needed
        self.dma_count = dma_count

    def emit(self, sem_stack=None):
        nc = self.nc
        streams = self.streams
        import contextlib
        with contextlib.ExitStack() as st:
            if sem_stack is not None:
                blk_stack, st = st, sem_stack
            else:
                blk_stack = st
            esems = {}
            for e in ENGS:
                esems[e] = [st.enter_context(nc.semaphore(f"s{id(self) % 100000}_{e}_{i}")) for i in range(self.sem_needed[e])]
            ssems = {}
            for e in ENGS:
                if self.dma_count[e] > 0:
                    for r in range(min(RING, self.dma_count[e])):
                        ssems[(e, r)] = st.enter_context(nc.semaphore(f"d{id(self) % 100000}_{e}_{r}"))
            block = blk_stack.enter_context(nc.Block())

            def wait_args(d):
                if d.is_dma:
                    return ssems[d.slot], 16 * d.round
                c = d.cnt - 1
                return esems[d.eng][c // SEM_CHUNK], (c % SEM_CHUNK) + 1

            def run(ename, eng):
                for o in streams[ename]:
                    for d in o.waits:
                        s, v = wait_args(d)
                        eng.wait_ge(s, v)
                    ins = o.fn(eng)
                    if o.need_inc:
                        if o.is_dma:
                            ins.then_inc(ssems[o.slot], 16)
                        else:
                            c = o.cnt - 1
                            ins.then_inc(esems[o.eng][c // SEM_CHUNK], 1)
                if ename == "sp":
                    last = {}
                    for o in self.ops:
                        if o.is_dma:
                            last[o.slot] = o.round
                    for slot, rnd in last.items():
                        eng.wait_ge(ssems[slot], 16 * rnd)

            @block.sync
            def _(eng):
                run("sp", eng)

            @block.tensor
            def _(eng):
                run("pe", eng)

            @block.scalar
            def _(eng):
                run("act", eng)

            @block.vector
            def _(eng):
                run("dve", eng)

            @block.gpsimd
            def _(eng):
                run("pool", eng)

    def stats(self):
        return {e: len(s) for e, s in self.streams.items()}


D = 1024
ALPHA = 4 ** 0.25
EPS = 1e-5
F_DENSE = 2816
F_EXP = 3584
NEXP = 8
TP = 1024
TB = 2


_UID = globals().get('_UID', [0])


class Ctx:
    def __init__(self, nc, P, st):
        self.nc, self.P, self.st = nc, P, st
        _UID[0] += 1
        self.uid = _UID[0]
        self.nbank = 0
        self.banks = [st.enter_context(nc.psum_tensor(f"u{_UID[0]}_ps{i}", [128, 512], F32)) for i in range(8)]
        self.wslot = 0

    def sb(self, name, shape, dt):
        return self.st.enter_context(self.nc.sbuf_tensor(f"u{self.uid}_sb_" + name, shape, dt))

    def bank(self):
        i = self.nbank % 8
        self.nbank += 1
        return self.banks[i], f"ps{i}"


def TS(tb):
    return slice(tb * 512, (tb + 1) * 512)


def phase2(cx, dr, is_moe, NTC):
    nc, P = cx.nc, cx.P
    I = P.I
    NPASS = NTC // TP
    sb = cx.sb
    r32 = sb("r32", [128, 8, TP], F32)
    xb = sb("xb", [128, 8, TP], BF16)
    big = sb("big", [128, 28, TP], BF16)
    gt = [sb(f"gt{n}", [128, 512], F32) for n in range(4)]
    rbv = lambda c: big[:, c // 2, (c % 2) * 512:(c % 2 + 1) * 512]
    sqv = lambda c: big[:, 4 + c // 2, (c % 2) * 512:(c % 2 + 1) * 512]
    rbk = lambda c: f"big{c // 2}_{c % 2}"
    sqk = lambda c: f"big{4 + c // 2}_{c % 2}"
    mean = sb("mean", [128, 512], F32)
    msq = sb("msq", [128, 512], F32)
    var = sb("var", [128, 512], F32)
    rstd = sb("rstd", [128, 512], F32)
    lnt = [sb(f"lnt{i}", [128, 512], F32) for i in range(2)]
    lng = sb("lng", [128, 3, 8], F32)
    lnb = sb("lnb", [128, 3, 8], F32)
    ones = sb("ones", [128, 128], BF16)
    memb = sb("memb", [128, 8, 256], BF16)
    KT = sb("KT", [64, 4, 256], BF16)
    Vx = sb("Vx", [128, 2, 256], BF16)
    PT = [sb(f"PT{i}", [128, 512], BF16) for i in range(2)]
    rden = sb("rden", [64, 512], F32)
    WSZ = 7168
    NW = 3
    wbuf = [sb(f"wbuf{i}", [128, WSZ], BF16) for i in range(NW)]
    sa = [sb(f"sa{i}", [128, 512], F32) for i in range(2)]
    R = lambda c, tb: f"r32_{c}_{tb}"
    X = lambda c, tb: f"xb_{c}_{tb}"
    B = lambda j, tb: f"big{j}_{tb}"

    I("dve", "memset", ones[:], 1.0, writes=["ones"])
    if "ygather" in dr:
        dr["sel4_sb"] = sb("sel4", [128, 4], F32)
        P.dma("sp", dr["sel4_sb"][:], dr["sel4"], writes=["sel4"])
    P.dma("sp", lng[:], dr["lng"], writes=["lng"])
    P.dma("sp", lnb[:], dr["lnb"], writes=["lnb"])

    def wload(src_ap, kc, ncols, parts=128):
        i = cx.wslot % NW
        cx.wslot += 1
        assert kc * ncols <= WSZ
        view = wbuf[i][0:parts, 0:kc * ncols].rearrange("p (c n) -> p c n", c=kc)
        P.dma("pool", view, src_ap, writes=[f"wbuf{i}"])
        return view, f"wbuf{i}"

    def layernorm(tb, li):
        ts = TS(tb)
        for c in range(8):
            I("pool", "tensor_copy", out=rbv(c), in_=r32[:, c, ts], reads=[R(c, tb)], writes=[rbk(c)])
            I("act", "activation", out=sqv(c), in_=r32[:, c, ts], func=AF.Square, reads=[R(c, tb)], writes=[sqk(c)])
        ps_s, ks = cx.bank()
        for c in range(8):
            I("pe", "matmul", ps_s[:], lhsT=ones[:], rhs=rbv(c), start=(c == 0), stop=(c == 7), reads=["ones", rbk(c)], writes=[ks])
        ps_q, kq = cx.bank()
        for c in range(8):
            I("pe", "matmul", ps_q[:], lhsT=ones[:], rhs=sqv(c), start=(c == 0), stop=(c == 7), reads=["ones", sqk(c)], writes=[kq])
        I("act", "activation", out=mean[:], in_=ps_s[:], func=AF.Copy, scale=1.0 / D, reads=[ks], writes=["mean"])
        I("pool", "tensor_tensor", out=msq[:], in0=mean[:], in1=mean[:], op=ALU.mult, reads=["mean"], writes=["msq"])
        I("dve", "scalar_tensor_tensor", out=var[:], in0=ps_q[:], scalar=1.0 / D, in1=msq[:], op0=ALU.mult, op1=ALU.subtract,
          reads=[kq, "msq"], writes=["var"])
        I("dve", "tensor_scalar", out=var[:], in0=var[:], scalar1=EPS, scalar2=None, op0=ALU.add, reads=["var"], writes=["var"])
        I("act", "activation", out=rstd[:], in_=var[:], func=AF.Ln, reads=["var"], writes=["rstd"])
        I("act", "activation", out=rstd[:], in_=rstd[:], func=AF.Exp, scale=-0.5, reads=["rstd"], writes=["rstd"])
        for c in range(8):
            t = lnt[c % 2]
            kt = f"lnt{c % 2}"
            I("dve", "tensor_tensor", out=t[:], in0=r32[:, c, ts], in1=mean[:], op=ALU.subtract, reads=[R(c, tb), "mean"], writes=[kt])
            I("pool", "tensor_tensor", out=t[:], in0=t[:], in1=rstd[:], op=ALU.mult, reads=[kt, "rstd"], writes=[kt])
            I("act", "activation", out=r32[:, c, ts], in_=t[:], func=AF.Identity, scale=lng[:, li, c:c + 1], bias=lnb[:, li, c:c + 1],
              reads=[kt, "lng", "lnb"], writes=[R(c, tb)])
            I("pool", "tensor_copy", out=xb[:, c, ts], in_=r32[:, c, ts], reads=[R(c, tb)], writes=[X(c, tb)])

    def resid_evac(ps, kps, m, tb):
        ts = TS(tb)
        I("dve", "scalar_tensor_tensor", out=r32[:, m, ts], in0=r32[:, m, ts], scalar=ALPHA, in1=ps[:], op0=ALU.mult, op1=ALU.add,
          reads=[kps, R(m, tb)], writes=[R(m, tb)])

    for ps_i in range(NPASS):
        t0 = ps_i * TP
        P.dma("sp", r32[:], dr["x32"].rearrange("(c p) t -> p c t", p=128)[:, :, t0:t0 + TP],
              writes=[R(c, tb) for c in range(8) for tb in range(TB)])
        for c in range(8):
            for tb in range(TB):
                I("pool", "tensor_copy", out=xb[:, c, TS(tb)], in_=r32[:, c, TS(tb)], reads=[R(c, tb)], writes=[X(c, tb)])
        if "ygather" not in dr:
            for n in range(4):
                P.dma("sp", big[:, 2 * n:2 * n + 2, :], dr["ybr"][n].rearrange("(c p) t -> p c t", p=128)[:, :, t0:t0 + TP],
                      writes=[B(2 * n + j, tb) for j in range(2) for tb in range(TB)])
        else:
            sel4 = dr["sel4_sb"]
            for tb in range(TB):
                for jq in range(4):
                    stg = big[:, 8 + 4 * (jq % 2):12 + 4 * (jq % 2), :].rearrange("p a (b t) -> p (a b) t", t=512)
                    kst = [B(8 + 4 * (jq % 2) + a, tb2) for a in range(4) for tb2 in range(TB)]
                    src = dr["ygather"](jq, ps_i * TB + tb)
                    for hh in range(2):
                        for c2 in range(2):
                            sv = src.rearrange("(c2 hh n d) t -> c2 hh d n t", c2=2, hh=2, n=4)[c2, hh]
                            dv = stg[hh * 64:(hh + 1) * 64].rearrange("p (n c2) t -> p c2 n t", c2=2)[:, c2]
                            P.dma("sp", dv, sv, writes=kst)
                    dst = big[:, 0:8, TS(tb)]
                    kd = [B(a, tb) for a in range(8)]
                    if jq == 0:
                        I("dve", "tensor_scalar", out=dst, in0=stg, scalar1=sel4[:, 0:1], scalar2=None, op0=ALU.mult, reads=kst + ["sel4"], writes=kd)
                    else:
                        I("dve", "scalar_tensor_tensor", out=dst, in0=stg, scalar=sel4[:, jq:jq + 1], in1=dst, op0=ALU.mult, op1=ALU.add,
                          reads=kst + kd + ["sel4"], writes=kd)
        for m in range(8):
            wg, kwg = wload(dr["wg"].rearrange("(c p) n -> p c n", p=128)[:, :, m * 512:(m + 1) * 512], 8, 512)
            wbr, kwbr = wload(dr["wbr"].rearrange("(c p) n -> p c n", p=128)[:, :, m * 128:(m + 1) * 128], 8, 128)
            for tb in range(TB):
                ts = TS(tb)
                for n in range(4):
                    ps, kps = cx.bank()
                    for c in range(8):
                        I("pe", "matmul", ps[:], lhsT=wg[:, c, n * 128:(n + 1) * 128], rhs=xb[:, c, ts], start=(c == 0), stop=(c == 7),
                          reads=[kwg, X(c, tb)], writes=[kps])
                    I("act", "activation", out=gt[n][:], in_=ps[:], func=AF.Sigmoid, reads=[kps], writes=[f"gt{n}"])
                for n in range(4):
                    ps, kps = cx.bank()
                    for c2 in range(2):
                        I("pe", "matmul", ps[:], lhsT=wbr[:, n * 2 + c2, :], rhs=big[:, n * 2 + c2, ts], start=(c2 == 0), stop=(c2 == 1),
                          reads=[kwbr, B(n * 2 + c2, tb)], writes=[kps])
                    I("dve", "tensor_tensor", out=gt[n][:], in0=gt[n][:], in1=ps[:], op=ALU.mult, reads=[kps, f"gt{n}"], writes=[f"gt{n}"])
                I("pool", "tensor_tensor", out=gt[0][:], in0=gt[0][:], in1=gt[1][:], op=ALU.add, reads=["gt0", "gt1"], writes=["gt0"])
                I("pool", "tensor_tensor", out=gt[2][:], in0=gt[2][:], in1=gt[3][:], op=ALU.add, reads=["gt2", "gt3"], writes=["gt2"])
                I("pool", "tensor_tensor", out=big[:, 8 + m, ts], in0=gt[0][:], in1=gt[2][:], op=ALU.add, reads=["gt0", "gt2"], writes=[B(8 + m, tb)])
        for m in range(8):
            wo_, kwo = wload(dr["wout"].rearrange("(c p) n -> p c n", p=128)[:, :, m * 128:(m + 1) * 128], 8, 128)
            for tb in range(TB):
                ps, kps = cx.bank()
                for c in range(8):
                    I("pe", "matmul", ps[:], lhsT=wo_[:, c, :], rhs=big[:, 8 + c, TS(tb)], start=(c == 0), stop=(c == 7),
                      reads=[kwo, B(8 + c, tb)], writes=[kps])
                resid_evac(ps, kps, m, tb)
        for tb in range(TB):
            layernorm(tb, 0)
        if ps_i == 0:
            P.dma("pool", memb[:], dr["memT"].rearrange("(c p) t -> p c t", p=128), writes=["memb"])
            wkv, kwkv = wload(dr["wkv"].rearrange("(c p) n -> p c n", p=128), 8, 512)
            for h in range(4):
                ps, kps = cx.bank()
                for c in range(8):
                    I("pe", "matmul", ps[0:64, 0:256], lhsT=wkv[:, c, h * 64:(h + 1) * 64], rhs=memb[:, c, :], start=(c == 0), stop=(c == 7),
                      reads=[kwkv, "memb"], writes=[kps])
                I("act", "activation", out=KT[:, h, :], in_=ps[0:64, 0:256], func=AF.Copy, reads=[kps], writes=["KT"])
            for mt in range(2):
                ps, kps = cx.bank()
                for c in range(8):
                    I("pe", "matmul", ps[:, 0:256], lhsT=memb[:, c, mt * 128:(mt + 1) * 128], rhs=wkv[:, c, 256:512], start=(c == 0), stop=(c == 7),
                      reads=[kwkv, "memb"], writes=[kps])
                I("act", "activation", out=Vx[:, mt, :], in_=ps[:, 0:256], func=AF.Copy, reads=[kps], writes=["Vx"])
        wq, kwq = wload(dr["wq"].rearrange("(c p) n -> p c n", p=128), 8, 256)
        for tb in range(TB):
            ts = TS(tb)
            for h in range(4):
                ps, kps = cx.bank()
                for c in range(8):
                    I("pe", "matmul", ps[0:64, :], lhsT=wq[:, c, h * 64:(h + 1) * 64], rhs=xb[:, c, ts], start=(c == 0), stop=(c == 7),
                      reads=[kwq, X(c, tb)], writes=[kps])
                I("act", "activation", out=big[0:64, 16 + h, ts], in_=ps[0:64, :], func=AF.Copy, scale=0.125, reads=[kps], writes=[B(16 + h, tb)])
            for h in range(4):
                for mt in range(2):
                    ps, kps = cx.bank()
                    I("pe", "matmul", ps[:], lhsT=KT[:, h, mt * 128:(mt + 1) * 128], rhs=big[0:64, 16 + h, ts], start=True, stop=True,
                      reads=["KT", B(16 + h, tb)], writes=[kps])
                    I("act", "activation", out=PT[mt][:], in_=ps[:], func=AF.Exp, reads=[kps], writes=[f"PT{mt}"])
                pso, kpo = cx.bank()
                for mt in range(2):
                    I("pe", "matmul", pso[0:64, :], lhsT=Vx[:, mt, h * 64:(h + 1) * 64], rhs=PT[mt][:], start=(mt == 0), stop=(mt == 1),
                      reads=["Vx", f"PT{mt}"], writes=[kpo])
                psd, kpd = cx.bank()
                for mt in range(2):
                    I("pe", "matmul", psd[0:64, :], lhsT=ones[:, 0:64], rhs=PT[mt][:], start=(mt == 0), stop=(mt == 1),
                      reads=["ones", f"PT{mt}"], writes=[kpd])
                I("dve", "reciprocal", out=rden[:], in_=psd[0:64, :], reads=[kpd], writes=["rden"])
                I("dve", "tensor_tensor", out=big[0:64, 20 + h, ts], in0=pso[0:64, :], in1=rden[:], op=ALU.mult,
                  reads=[kpo, "rden"], writes=[B(20 + h, tb)])
        wo2, kwo2 = wload(dr["wo"].rearrange("(h p) n -> p h n", p=64), 4, 1024, parts=64)
        for tb in range(TB):
            ts = TS(tb)
            for m in range(8):
                ps, kps = cx.bank()
                for h in range(4):
                    I("pe", "matmul", ps[:], lhsT=wo2[0:64, h, m * 128:(m + 1) * 128], rhs=big[0:64, 20 + h, ts], start=(h == 0), stop=(h == 3),
                      reads=[kwo2, B(20 + h, tb)], writes=[kps])
                resid_evac(ps, kps, m, tb)
        for tb in range(TB):
            layernorm(tb, 1)
        if not is_moe:
            ffn_expert(cx, WStream(wload, ffn_plan(dr["w13"], dr["w2"], F_DENSE)), F_DENSE, r32, xb, big, sa, None, True)
        else:
            moe(cx, dr, r32, xb, big, sa, wload)
        for tb in range(TB):
            layernorm(tb, 2)
        P.dma("sp", dr["xo32"].rearrange("(c p) t -> p c t", p=128)[:, :, t0:t0 + TP], r32[:],
              reads=[R(c, tb) for c in range(8) for tb in range(TB)], writes=["xo32"])
        if "xob" in dr:
            P.dma("sp", dr["xob"].rearrange("(c p) t -> p c t", p=128)[:, :, t0:t0 + TP], xb[:],
                  reads=[X(c, tb) for c in range(8) for tb in range(TB)], writes=["xob"])
        if "xchunk" in dr:
            for fc in range(4):
                for tb in range(TB):
                    P.dma("sp", dr["xchunk"](fc, ps_i * TB + tb).rearrange("(c p) t -> p c t", p=128), xb[:, 2 * fc:2 * fc + 2, TS(tb)],
                          reads=[X(c, tb) for c in (2 * fc, 2 * fc + 1)], writes=["xob"])


class WStream:
    def __init__(self, wload, plan, depth=2):
        self.wload, self.plan, self.depth = wload, plan, depth
        self.issued = []
        self.pos = 0

    def next(self):
        i = self.pos
        while len(self.issued) < min(len(self.plan), i + 1 + self.depth):
            self.issued.append(self.wload(*self.plan[len(self.issued)]))
        self.pos += 1
        return self.issued[i]


def ffn_plan(w13, w2, F):
    FT = F // 128
    plan = []
    w13v = w13.rearrange("(c p) n -> p c n", p=128)
    for f0 in range(0, FT, 2):
        nf = min(2, FT - f0)
        plan.append((w13v[:, :, f0 * 256:(f0 + nf) * 256], 8, nf * 256))
    w2v = w2.rearrange("(c p) n -> p c n", p=128)
    for m0 in range(0, 8, 2):
        plan.append((w2v[:, :, m0 * 128:(m0 + 2) * 128], FT, 256))
    return plan


def ffn_expert(cx, ws, F, r32, xb, big, sa, wbc, first_scale):
    P = cx.P
    I = P.I
    R = lambda c, tb: f"r32_{c}_{tb}"
    X = lambda c, tb: f"xb_{c}_{tb}"
    B = lambda j, tb: f"big{j}_{tb}"
    FT = F // 128
    cnt = 0
    for f0 in range(0, FT, 2):
        nf = min(2, FT - f0)
        wt, kw = ws.next()
        for fi in range(nf):
            f = f0 + fi
            for tb in range(TB):
                ts = TS(tb)
                psa, ka = cx.bank()
                for c in range(8):
                    I("pe", "matmul", psa[:], lhsT=wt[:, c, fi * 256:fi * 256 + 128], rhs=xb[:, c, ts], start=(c == 0), stop=(c == 7),
                      reads=[kw, X(c, tb)], writes=[ka])
                psg, kg = cx.bank()
                for c in range(8):
                    I("pe", "matmul", psg[:], lhsT=wt[:, c, fi * 256 + 128:fi * 256 + 256], rhs=xb[:, c, ts], start=(c == 0), stop=(c == 7),
                      reads=[kw, X(c, tb)], writes=[kg])
                s = sa[cnt % 2]
                ksa = f"sa{cnt % 2}"
                cnt += 1
                I("act", "activation", out=s[:], in_=psa[:], func=AF.Silu, reads=[ka], writes=[ksa])
                if wbc is None:
                    I("dve", "tensor_tensor", out=big[:, f, ts], in0=s[:], in1=psg[:], op=ALU.mult, reads=[kg, ksa], writes=[B(f, tb)])
                else:
                    I("dve", "tensor_tensor", out=s[:], in0=s[:], in1=psg[:], op=ALU.mult, reads=[kg, ksa], writes=[ksa])
                    I("dve", "tensor_tensor", out=big[:, f, ts], in0=s[:], in1=wbc[0][:, ts], op=ALU.mult, reads=[ksa, wbc[1]], writes=[B(f, tb)])
    for m0 in range(0, 8, 2):
        wt, kw = ws.next()
        for mi in range(2):
            m = m0 + mi
            for tb in range(TB):
                ts = TS(tb)
                ps, kps = cx.bank()
                for f in range(FT):
                    I("pe", "matmul", ps[:], lhsT=wt[:, f, mi * 128:(mi + 1) * 128], rhs=big[:, f, ts], start=(f == 0), stop=(f == FT - 1),
                      reads=[kw, B(f, tb)], writes=[kps])
                if first_scale:
                    I("dve", "scalar_tensor_tensor", out=r32[:, m, ts], in0=r32[:, m, ts], scalar=ALPHA, in1=ps[:], op0=ALU.mult, op1=ALU.add,
                      reads=[kps, R(m, tb)], writes=[R(m, tb)])
                else:
                    I("dve", "tensor_tensor", out=r32[:, m, ts], in0=r32[:, m, ts], in1=ps[:], op=ALU.add, reads=[kps, R(m, tb)], writes=[R(m, tb)])


def moe(cx, dr, r32, xb, big, sa, wload):
    P = cx.P
    I = P.I
    sb = cx.sb
    R = lambda c, tb: f"r32_{c}_{tb}"
    NTT = TP // 128
    if not hasattr(cx, "moe_tiles"):
        cx.moe_tiles = dict(
            rt=sb("rt32", [128, 8, 8], F32), lg=sb("lg", [128, NTT, 8], F32), top=sb("top8", [128, 8], F32),
            w0=sb("w0", [128, 1], F32), w1=sb("w1", [128, 1], F32), dw=sb("dw", [128, 1], F32), dm=sb("dm", [128, 1], F32),
            eq=sb("eq", [128, 8], F32), ge=sb("ge", [128, 8], F32), wtok=sb("wtok", [128, NTT, 8], F32),
            wT=sb("wT", [8, TP], F32), sel=sb("sel", [8, 8, 128], F32), ident=sb("ident", [128, 128], F32),
            wbc=[sb(f"wbc{i}", [128, TP], F32) for i in range(2)])
        t = cx.moe_tiles
        P.dma("sp", t["rt"][:], dr["router"].rearrange("(c p) e -> p c e", p=128), writes=["rt32"])
        P.dma("sp", t["sel"][:], dr["sel"], writes=["sel"])
        P.dma("sp", t["ident"][:], dr["ident"], writes=["ident"])
    t = cx.moe_tiles
    rt, lg, top, w0, w1, dw, dm, eq, ge, wtok, wT, sel, ident, wbc = (t[k] for k in
        ("rt", "lg", "top", "w0", "w1", "dw", "dm", "eq", "ge", "wtok", "wT", "sel", "ident", "wbc"))
    psr, kr = cx.bank()
    for tt in range(NTT):
        for c in range(8):
            I("pe", "matmul", psr[:, tt * 8:(tt + 1) * 8], lhsT=r32[:, c, tt * 128:(tt + 1) * 128], rhs=rt[:, c, :], start=(c == 0), stop=(c == 7),
              reads=["rt32", R(c, tt // 4)], writes=[kr])
    I("dve", "tensor_copy", out=lg[:].rearrange("p a b -> p (a b)"), in_=psr[:, 0:NTT * 8], reads=[kr], writes=["lg"])
    psts = [cx.bank() for _ in range(TB)]
    for tt in range(NTT):
        pst, kt = psts[tt // 4]
        I("dve", "max", out=top[:], in_=lg[:, tt, :], reads=["lg"], writes=["top8"])
        I("dve", "tensor_tensor", out=dm[:], in0=top[:, 0:1], in1=top[:, 1:2], op=ALU.subtract, reads=["top8"], writes=["dm"])
        I("act", "activation", out=w0[:], in_=dm[:], func=AF.Sigmoid, reads=["dm"], writes=["w0"])
        I("dve", "tensor_scalar", out=w1[:], in0=w0[:], scalar1=-1.0, scalar2=1.0, op0=ALU.mult, op1=ALU.add, reads=["w0"], writes=["w1"])
        I("dve", "tensor_tensor", out=dw[:], in0=w0[:], in1=w1[:], op=ALU.subtract, reads=["w0", "w1"], writes=["dw"])
        I("dve", "tensor_scalar", out=eq[:], in0=lg[:, tt, :], scalar1=top[:, 0:1], scalar2=None, op0=ALU.is_equal, reads=["lg", "top8"], writes=["eq"])
        I("dve", "tensor_scalar", out=ge[:], in0=lg[:, tt, :], scalar1=top[:, 1:2], scalar2=None, op0=ALU.is_ge, reads=["lg", "top8"], writes=["ge"])
        I("dve", "tensor_scalar", out=eq[:], in0=eq[:], scalar1=dw[:, 0:1], scalar2=w1[:, 0:1], op0=ALU.mult, op1=ALU.add,
          reads=["eq", "dw", "w1"], writes=["eq"])
        I("dve", "tensor_tensor", out=wtok[:, tt, :], in0=eq[:], in1=ge[:], op=ALU.mult, reads=["eq", "ge"], writes=["wtok"])
        I("pe", "transpose", out=pst[0:8, (tt % 4) * 128:(tt % 4 + 1) * 128], in_=wtok[:, tt, :], identity=ident[:], reads=["wtok", "ident"], writes=[kt])
    for tb in range(TB):
        I("dve", "tensor_copy", out=wT[:, TS(tb)], in_=psts[tb][0][0:8, :], reads=[psts[tb][1]], writes=["wT"])
    for c in range(8):
        for tb in range(TB):
            I("pool", "tensor_scalar", out=r32[:, c, TS(tb)], in0=r32[:, c, TS(tb)], scalar1=ALPHA, scalar2=None, op0=ALU.mult,
              reads=[R(c, tb)], writes=[R(c, tb)])
    plan = []
    for ex in range(NEXP):
        plan += ffn_plan(dr["w13"][ex], dr["w2"][ex], F_EXP)
    ws = WStream(wload, plan)
    for ex in range(NEXP):
        wb_ = wbc[ex % 2]
        kwb = f"wbc{ex % 2}"
        for tb in range(TB):
            ps, kps = cx.bank()
            I("pe", "matmul", ps[:], lhsT=sel[:, ex, :], rhs=wT[:, TS(tb)], start=True, stop=True, reads=["sel", "wT"], writes=[kps])
            I("act", "activation", out=wb_[:, TS(tb)], in_=ps[:], func=AF.Copy, reads=[kps], writes=[kwb])
        ffn_expert(cx, ws, F_EXP, r32, xb, big, sa, (wb_, kwb), False)


EPS = 1e-5
C_CQ, C_CKV, C_KR, C_KRS = 0, 256, 384, 416
C_SWQ, C_SWK, C_SWV = 448, 512, 576
C_HQ, C_HF, C_HG, C_HI = 640, 704, 768, 832
C_SBQ, C_SBK, C_SBV = 896, 960, 1024
NCOL = 1088


_UID = globals().get('_UID', [0])


class Ctx1:
    def __init__(self, nc, P, st):
        self.nc, self.P, self.st = nc, P, st
        _UID[0] += 1
        self.uid = _UID[0]
        self.banks = [st.enter_context(nc.psum_tensor(f"u{_UID[0]}_ps{i}", [128, 512], F32)) for i in range(8)]
        self.free = list(range(8))
        self.rr = 0

    def sb(self, name, shape, dt):
        return self.st.enter_context(self.nc.sbuf_tensor(f"u{self.uid}_sb_" + name, shape, dt))

    def bank(self):
        i = self.free[self.rr % len(self.free)]
        self.rr += 1
        return self.banks[i], f"ps{i}"

    def hold(self):
        i = self.free.pop(self.rr % len(self.free))
        return self.banks[i], f"ps{i}", i

    def release(self, i):
        self.free.append(i)
        self.free.sort()


def phase1(cx, dr, S, layer, do=("mla", "swa", "hgrn", "sb")):
    nc, P = cx.nc, cx.P
    I = P.I
    sb = cx.sb
    NB = S // 512
    W = sb("W", [128, 8, NCOL], BF16)
    P.dma("pool", W[:], dr["wh"].rearrange("(c p) n -> p c n", p=128), writes=["W"])
    xbuf = [sb(f"xbuf{i}", [128, 8, 512], BF16) for i in range(2)]
    ones = sb("ones", [128, 128], BF16)
    I("dve", "memset", ones[:], 1.0, writes=["ones"])
    masks = sb("masks", [128, 8, 512], BF16)
    P.dma("sp", masks[:], dr["masks"], writes=["masks"])
    cx.xcnt = 0

    cx.xpending = {}

    def xload(tb):
        def issue(t):
            i = cx.xcnt % 2
            cx.xcnt += 1
            for csl, src in dr["xsrc"](t):
                P.dma("pool", xbuf[i][:, csl, :], src, writes=[f"xbuf{i}"])
            return xbuf[i], f"xbuf{i}"
        if tb not in cx.xpending:
            cx.xpending[tb] = issue(tb)
        r = cx.xpending.pop(tb)
        if tb + 1 < NB and (tb + 1) not in cx.xpending:
            cx.xpending[tb + 1] = issue(tb + 1)
        return r

    def proj_fm(xb_, kx, col0, M, out_ps, kps, tslice=slice(0, 512)):
        for c in range(8):
            I("pe", "matmul", out_ps, lhsT=W[:, c, col0:col0 + M], rhs=xb_[:, c, tslice], start=(c == 0), stop=(c == 7), reads=["W", kx], writes=[kps])

    def proj_tm(xb_, kx, col0, N, out_ps, kps, tok0, ntok):
        for c in range(8):
            I("pe", "matmul", out_ps, lhsT=xb_[:, c, tok0:tok0 + ntok], rhs=W[:, c, col0:col0 + N], start=(c == 0), stop=(c == 7), reads=["W", kx], writes=[kps])

    QT = sb("QT", [128, S], BF16)
    KT = sb("KT", [128, S], BF16)
    Vm = sb("Vm", [128, S // 128, 64], BF16)
    PTn = 4
    PT = [sb(f"PT{i}", [128, 512], BF16) for i in range(PTn)]
    rden = sb("rden", [64, 512], F32)
    yout = [sb(f"yout{i}", [64, 512], BF16) for i in range(2)]
    tA = [sb(f"tA{i}", [128, 512], F32) for i in range(4)]
    tB = [sb(f"tB{i}", [128, 512], BF16) for i in range(4)]
    cx.ycnt = 0

    def ystore(branch, qb, src_ps, kps, scale_ap, kscale):
        i = cx.ycnt % 2
        cx.ycnt += 1
        I("dve", "tensor_tensor", out=yout[i][:], in0=src_ps, in1=scale_ap, op=ALU.mult, reads=[kps, kscale], writes=[f"yout{i}"])
        P.dma("sp", dr["ydst"](branch, qb), yout[i][:], reads=[f"yout{i}"], writes=["ybr"])

    if "mla" in do:
        wuq = sb("wuq", [128, 2, 128], BF16)
        wukv = sb("wukv", [128, 128], BF16)
        P.dma("pool", wuq[:], dr["wuq"].rearrange("(c p) n -> p c n", p=128), writes=["wuq"])
        P.dma("pool", wukv[:], dr["wukv"], writes=["wukv"])
        qn = sb("qn", [128, 2], F32)
        kvn = sb("kvn", [128, 1], F32)
        rc = sb("ropec", [128, 2], F32)
        P.dma("sp", qn[:], dr["qnorm"], writes=["qn"])
        P.dma("sp", kvn[:], dr["kvnorm"], writes=["kvn"])
        P.dma("sp", rc[:], dr["ropec"], writes=["ropec"])
        posi = sb("posi", [128, 512], I32)
        cqn = sb("cqn", [128, 2, 512], BF16)
        ckvn = sb("ckvn", [128, 512], BF16)
        RS = slice(64, 96)
        for tb in range(NB):
            ts = slice(tb * 512, (tb + 1) * 512)
            xb_, kx = xload(tb)
            P.dma("sp", posi[RS, :], dr["posb"][:, ts], writes=["posi"])
            tt, tf = tA[0], tA[1]
            I("dve", "tensor_copy", out=tt[RS, :], in_=posi[RS, :], reads=["posi"], writes=["tA0"])
            I("dve", "tensor_scalar", out=tt[RS, :], in0=tt[RS, :], scalar1=rc[RS, 0:1], scalar2=None, op0=ALU.mult, reads=["tA0", "ropec"], writes=["tA0"])
            sincos = []
            for which, off in (("sin", 0.0), ("cos", 0.25)):
                dst = tA[2] if which == "sin" else tA[3]
                kd = "tA2" if which == "sin" else "tA3"
                I("dve", "tensor_scalar", out=dst[RS, :], in0=tt[RS, :], scalar1=off, scalar2=None, op0=ALU.add, reads=["tA0"], writes=[kd])
                I("dve", "tensor_copy", out=posi[RS, :], in_=dst[RS, :], reads=[kd], writes=["posi"])
                I("dve", "tensor_copy", out=tf[RS, :], in_=posi[RS, :], reads=["posi"], writes=["tA1"])
                I("dve", "tensor_tensor", out=dst[RS, :], in0=dst[RS, :], in1=tf[RS, :], op=ALU.subtract, reads=[kd, "tA1"], writes=[kd])
                I("dve", "tensor_scalar", out=tf[RS, :], in0=dst[RS, :], scalar1=0.5, scalar2=None, op0=ALU.is_gt, reads=[kd], writes=["tA1"])
                I("dve", "tensor_tensor", out=dst[RS, :], in0=dst[RS, :], in1=tf[RS, :], op=ALU.subtract, reads=[kd, "tA1"], writes=[kd])
                I("dve", "tensor_scalar", out=tf[RS, :], in0=dst[RS, :], scalar1=-0.5, scalar2=None, op0=ALU.is_lt, reads=[kd], writes=["tA1"])
                I("dve", "tensor_tensor", out=dst[RS, :], in0=dst[RS, :], in1=tf[RS, :], op=ALU.add, reads=[kd, "tA1"], writes=[kd])
                I("act", "activation", out=dst[RS, :], in_=dst[RS, :], func=AF.Sin, scale=2.0 * math.pi, reads=[kd], writes=[kd])
            sin_t, cos_t = tA[2], tA[3]
            pcq = [cx.bank() for _ in range(2)]
            for j in range(2):
                proj_fm(xb_, kx, C_CQ + j * 128, 128, pcq[j][0][:], pcq[j][1])
                I("act", "activation", out=tB[2 + j][:], in_=pcq[j][0][:], func=AF.Square, reads=[pcq[j][1]], writes=[f"tB{2 + j}"])
            pss, kss = cx.bank()
            for j in range(2):
                I("pe", "matmul", pss[:], lhsT=ones[:], rhs=tB[2 + j][:], start=(j == 0), stop=(j == 1), reads=["ones", f"tB{2 + j}"], writes=[kss])
            rq = tA[1]
            I("act", "activation", out=rq[:], in_=pss[:], func=AF.Ln, scale=1.0 / 256, bias=EPS, reads=[kss], writes=["tA1"])
            I("act", "activation", out=rq[:], in_=rq[:], func=AF.Exp, scale=-0.5, reads=["tA1"], writes=["tA1"])
            for j in range(2):
                I("dve", "scalar_tensor_tensor", out=cqn[:, j, :], in0=pcq[j][0][:], scalar=qn[:, j:j + 1], in1=rq[:], op0=ALU.mult, op1=ALU.mult,
                  reads=[pcq[j][1], "qn", "tA1"], writes=[f"cqn{j}"])
            pq, kq = cx.bank()
            for j in range(2):
                I("pe", "matmul", pq[0:96, :], lhsT=wuq[:, j, 0:96], rhs=cqn[:, j, :], start=(j == 0), stop=(j == 1), reads=["wuq", f"cqn{j}"], writes=[kq])
            pq2, kq2 = cx.bank()
            for j in range(2):
                I("pe", "matmul", pq2[64:96, :], lhsT=wuq[:, j, 96:128], rhs=cqn[:, j, :], start=(j == 0), stop=(j == 1), reads=["wuq", f"cqn{j}"], writes=[kq2])
            I("act", "activation", out=QT[0:64, ts], in_=pq[0:64, :], func=AF.Copy, reads=[kq], writes=[f"QT{tb}"])
            I("dve", "tensor_tensor", out=tt[RS, :], in0=pq[RS, :], in1=cos_t[RS, :], op=ALU.mult, reads=[kq, "tA3"], writes=["tA0"])
            I("dve", "scalar_tensor_tensor", out=tf[RS, :], in0=pq2[RS, :], scalar=rc[RS, 1:2], in1=sin_t[RS, :], op0=ALU.mult, op1=ALU.mult,
              reads=[kq2, "ropec", "tA2"], writes=["tA1"])
            I("pool", "tensor_tensor", out=QT[RS, ts], in0=tt[RS, :], in1=tf[RS, :], op=ALU.add, reads=["tA0", "tA1"], writes=[f"QT{tb}"])
            pkv, kkv = cx.bank()
            proj_fm(xb_, kx, C_CKV, 128, pkv[:], kkv)
            I("act", "activation", out=tB[2][:], in_=pkv[:], func=AF.Square, reads=[kkv], writes=["tB2"])
            pss2, kss2 = cx.bank()
            I("pe", "matmul", pss2[:], lhsT=ones[:], rhs=tB[2][:], start=True, stop=True, reads=["ones", "tB2"], writes=[kss2])
            I("act", "activation", out=rq[:], in_=pss2[:], func=AF.Ln, scale=1.0 / 128, bias=EPS, reads=[kss2], writes=["tA1"])
            I("act", "activation", out=rq[:], in_=rq[:], func=AF.Exp, scale=-0.5, reads=["tA1"], writes=["tA1"])
            I("dve", "scalar_tensor_tensor", out=ckvn[:], in0=pkv[:], scalar=kvn[:, 0:1], in1=rq[:], op0=ALU.mult, op1=ALU.mult,
              reads=[kkv, "kvn", "tA1"], writes=["ckvn"])
            pk, kk = cx.bank()
            I("pe", "matmul", pk[0:64, :], lhsT=wukv[:, 0:64], rhs=ckvn[:], start=True, stop=True, reads=["wukv", "ckvn"], writes=[kk])
            I("act", "activation", out=KT[0:64, ts], in_=pk[0:64, :], func=AF.Copy, reads=[kk], writes=[f"KT{tb}"])
            pkr, kkr = cx.bank()
            proj_fm(xb_, kx, C_KR, 32, pkr[RS, :], kkr)
            pkr2, kkr2 = cx.bank()
            proj_fm(xb_, kx, C_KRS, 32, pkr2[RS, :], kkr2)
            I("dve", "tensor_tensor", out=tt[RS, :], in0=pkr[RS, :], in1=cos_t[RS, :], op=ALU.mult, reads=[kkr, "tA3"], writes=["tA0"])
            I("dve", "scalar_tensor_tensor", out=tf[RS, :], in0=pkr2[RS, :], scalar=rc[RS, 1:2], in1=sin_t[RS, :], op0=ALU.mult, op1=ALU.mult,
              reads=[kkr2, "ropec", "tA2"], writes=["tA1"])
            I("pool", "tensor_tensor", out=KT[RS, ts], in0=tt[RS, :], in1=tf[RS, :], op=ALU.add, reads=["tA0", "tA1"], writes=[f"KT{tb}"])
            pv, kv_ = cx.bank()
            for sbk in range(4):
                I("pe", "matmul", pv[:, sbk * 64:(sbk + 1) * 64], lhsT=ckvn[:, sbk * 128:(sbk + 1) * 128], rhs=wukv[:, 64:128], start=True, stop=True,
                  reads=["wukv", "ckvn"], writes=[kv_])
            I("act", "activation", out=Vm[:, tb * 4:(tb + 1) * 4, :], in_=pv[:, 0:256].rearrange("p (a b) -> p a b", a=4), func=AF.Copy,
              reads=[kv_], writes=[f"Vm{tb}"])
        scale = 96 ** -0.5
        for qb in range(NB):
            pso, kpo, io = cx.hold()
            psd, kpd, id_ = cx.hold()
            qs = slice(qb * 512, (qb + 1) * 512)
            nt = 4 * (qb + 1)

            def stA(t):
                ps, kps = cx.bank()
                I("pe", "matmul", ps[:], lhsT=KT[0:96, t * 128:(t + 1) * 128], rhs=QT[0:96, qs], start=True, stop=True,
                  reads=[f"KT{t // 4}", f"QT{qb}"], writes=[kps])
                p = PT[t % PTn]
                I("act", "activation", out=p[:], in_=ps[:], func=AF.Exp, scale=scale, reads=[kps], writes=[f"PT{t % PTn}"])
                if t >= 4 * qb:
                    I("pool", "tensor_tensor", out=p[:], in0=p[:], in1=masks[:, t - 4 * qb, :], op=ALU.mult, reads=[f"PT{t % PTn}", "masks"], writes=[f"PT{t % PTn}"])

            def stB(t):
                p = PT[t % PTn]
                I("pe", "matmul", pso[0:64, :], lhsT=Vm[:, t, :], rhs=p[:], start=(t == 0), stop=(t == nt - 1), reads=[f"Vm{t // 4}", f"PT{t % PTn}"], writes=[kpo])
                I("pe", "matmul", psd[0:64, :], lhsT=ones[:, 0:64], rhs=p[:], start=(t == 0), stop=(t == nt - 1), reads=["ones", f"PT{t % PTn}"], writes=[kpd])

            for s_ in range(nt + 2):
                if s_ < nt:
                    stA(s_)
                if 0 <= s_ - 2 < nt:
                    stB(s_ - 2)
            I("dve", "reciprocal", out=rden[:], in_=psd[0:64, :], reads=[kpd], writes=["rden"])
            ystore(0, qb, pso[0:64, :], kpo, rden[:], "rden")
            cx.release(io)
            cx.release(id_)

    if "sb" in do:
        tri = sb("tri", [128, 2, 128], BF16)
        P.dma("sp", tri[:], dr["tri"], writes=["tri"])
        for tb in range(NB):
            ts = slice(tb * 512, (tb + 1) * 512)
            xb_, kx = xload(tb)
            pq, kq = cx.bank()
            proj_fm(xb_, kx, C_SBQ, 64, pq[0:64, :], kq)
            I("act", "activation", out=QT[0:64, ts], in_=pq[0:64, :], func=AF.Copy, scale=0.125, reads=[kq], writes=[f"QT{tb}"])
            pk, kk = cx.bank()
            proj_fm(xb_, kx, C_SBK, 64, pk[0:64, :], kk)
            I("act", "activation", out=KT[0:64, ts], in_=pk[0:64, :], func=AF.Copy, reads=[kk], writes=[f"KT{tb}"])
            pv, kv_ = cx.bank()
            for sbk in range(4):
                proj_tm(xb_, kx, C_SBV, 64, pv[:, sbk * 64:(sbk + 1) * 64], kv_, sbk * 128, 128)
            I("act", "activation", out=Vm[:, tb * 4:(tb + 1) * 4, :], in_=pv[:, 0:256].rearrange("p (a b) -> p a b", a=4), func=AF.Copy,
              reads=[kv_], writes=[f"Vm{tb}"])
        S32 = sb("S32", [128, 512], F32)
        carry = [sb(f"carry{i}", [128, 512], BF16) for i in range(3)]
        spb = [sb(f"spb{i}", [128, 512], BF16) for i in range(3)]
        AT = [sb(f"AT{i}", [128, 512], BF16) for i in range(3)]
        for qb in range(NB):
            pso, kpo, io = cx.hold()
            qs = slice(qb * 512, (qb + 1) * 512)
            nt = 4 * (qb + 1)
            zb = {}

            def kb_of(t):
                return 4 * qb + 3 - t

            def stA(t):
                kb = kb_of(t)
                ps, kps = cx.bank()
                zb[t] = (ps, kps)
                I("pe", "matmul", ps[:], lhsT=KT[0:64, kb * 128:(kb + 1) * 128], rhs=QT[0:64, qs], start=True, stop=True, skip_group_check=True,
                  reads=[f"KT{kb // 4}", f"QT{qb}"], writes=[kps])
                e1 = tA[t % 2]
                I("act", "activation", out=e1[:], in_=ps[:], func=AF.Exp, reads=[kps], writes=[f"tA{t % 2}"])
                sp_ = spb[t % 3]
                I("act", "activation", out=sp_[:], in_=e1[:], func=AF.Ln, bias=1.0, reads=[f"tA{t % 2}"], writes=[f"spb{t % 3}"])
                if t < 4:
                    I("pool", "tensor_tensor", out=sp_[:], in0=sp_[:], in1=masks[:, 4 + (3 - t), :], op=ALU.mult, reads=[f"spb{t % 3}", "masks"], writes=[f"spb{t % 3}"])
                if t == 0:
                    I("dve", "tensor_copy", out=S32[:], in_=sp_[:], reads=[f"spb{t % 3}"], writes=["S32"])
                else:
                    I("dve", "tensor_tensor", out=S32[:], in0=S32[:], in1=sp_[:], op=ALU.add, reads=["S32", f"spb{t % 3}"], writes=["S32"])
                if t + 1 < nt:
                    I("pool", "tensor_copy", out=carry[(t + 1) % 3][:], in_=S32[:], reads=["S32"], writes=[f"carry{(t + 1) % 3}"])

            def stB(t):
                ps, kps = zb[t]
                I("pe", "matmul", ps[:], lhsT=tri[:, 0, :], rhs=spb[t % 3][:], start=False, stop=(t == 0), skip_group_check=True,
                  reads=["tri", f"spb{t % 3}"], writes=[kps])
                if t > 0:
                    I("pe", "matmul", ps[:], lhsT=tri[:, 1, :], rhs=carry[t % 3][:], start=False, stop=True, skip_group_check=True,
                      reads=["tri", f"carry{t % 3}"], writes=[kps])
                a = AT[t % 3]
                I("act", "activation", out=a[:], in_=ps[:], func=AF.Exp, reads=[kps], writes=[f"AT{t % 3}"])
                if t < 4:
                    I("pool", "tensor_tensor", out=a[:], in0=a[:], in1=masks[:, 4 + (3 - t), :], op=ALU.mult, reads=[f"AT{t % 3}", "masks"], writes=[f"AT{t % 3}"])

            def stC(t):
                kb = kb_of(t)
                I("pe", "matmul", pso[0:64, :], lhsT=Vm[:, kb, :], rhs=AT[t % 3][:], start=(t == 0), stop=(t == nt - 1), reads=[f"Vm{kb // 4}", f"AT{t % 3}"], writes=[kpo])

            for s_ in range(nt + 2):
                if s_ < nt:
                    stA(s_)
                if 0 <= s_ - 1 < nt:
                    stB(s_ - 1)
                if 0 <= s_ - 2 < nt:
                    stC(s_ - 2)
            i = cx.ycnt % 2
            cx.ycnt += 1
            I("act", "activation", out=yout[i][:], in_=pso[0:64, :], func=AF.Copy, reads=[kpo], writes=[f"yout{i}"])
            P.dma("sp", dr["ydst"](3, qb), yout[i][:], reads=[f"yout{i}"], writes=["ybr"])
            cx.release(io)

    if "swa" in do:
        swc = sb("swc", [128, 34], F32)
        P.dma("sp", swc[:], dr["swc"], writes=["swc"])
        dcur = sb("dcur", [128, 128], F32)
        dprev = sb("dprev", [128, 128], F32)
        P.dma("sp", dcur[:], dr["dist"][0], writes=["dcur"])
        P.dma("sp", dprev[:], dr["dist"][1], writes=["dprev"])
        diff = sb("swdiff", [128, 31], F32)
        I("dve", "tensor_tensor", out=diff[:], in0=swc[:, 1:32], in1=swc[:, 0:31], op=ALU.subtract, reads=["swc"], writes=["swdiff"])
        esink = sb("esink", [128, 1], F32)
        I("act", "activation", out=esink[:], in_=swc[:, 32:33], func=AF.Exp, reads=["swc"], writes=["esink"])
        EB = [sb(f"EB{i}", [128, 512], F32) for i in range(3)]
        acc = tA[0]
        stp = tA[1]
        los = dr["bucket_lo"]
        for which, dt_, kd in ((0, dcur, "dcur"), (1, dprev, "dprev")):
            I("dve", "tensor_scalar", out=acc[:, 0:128], in0=dt_[:], scalar1=0.0, scalar2=swc[:, 0:1], op0=ALU.mult, op1=ALU.add, reads=[kd, "swc"], writes=["tA0"])
            for b in range(1, 32):
                I("dve", "tensor_scalar", out=stp[:, 0:128], in0=dt_[:], scalar1=float(los[b - 1]), scalar2=diff[:, b - 1:b], op0=ALU.is_ge, op1=ALU.mult,
                  reads=[kd, "swdiff"], writes=["tA1"])
                I("dve", "tensor_tensor", out=acc[:, 0:128], in0=acc[:, 0:128], in1=stp[:, 0:128], op=ALU.add, reads=["tA0", "tA1"], writes=["tA0"])
            I("act", "activation", out=acc[:, 0:128], in_=acc[:, 0:128], func=AF.Exp, reads=["tA0"], writes=["tA0"])
            if which == 0:
                I("dve", "tensor_scalar", out=stp[:, 0:128], in0=dt_[:], scalar1=0.0, scalar2=None, op0=ALU.is_ge, reads=[kd], writes=["tA1"])
            else:
                I("dve", "tensor_scalar", out=stp[:, 0:128], in0=dt_[:], scalar1=127.0, scalar2=None, op0=ALU.is_le, reads=[kd], writes=["tA1"])
            for r in range(4):
                I("dve", "tensor_tensor", out=EB[which][:, r * 128:(r + 1) * 128], in0=acc[:, 0:128], in1=stp[:, 0:128], op=ALU.mult,
                  reads=["tA0", "tA1"], writes=[f"EB{which}"])
        I("pool", "tensor_copy", out=EB[2][:], in_=EB[1][:], reads=["EB1"], writes=["EB2"])
        I("pool", "memset", EB[2][:, 0:128], 0.0, reads=[], writes=["EB2"])
        for tb in range(NB):
            ts = slice(tb * 512, (tb + 1) * 512)
            xb_, kx = xload(tb)
            pq, kq = cx.bank()
            proj_fm(xb_, kx, C_SWQ, 64, pq[0:64, :], kq)
            I("act", "activation", out=QT[0:64, ts], in_=pq[0:64, :], func=AF.Copy, scale=0.125, reads=[kq], writes=[f"QT{tb}"])
            pk, kk = cx.bank()
            proj_fm(xb_, kx, C_SWK, 64, pk[0:64, :], kk)
            I("act", "activation", out=KT[0:64, ts], in_=pk[0:64, :], func=AF.Copy, reads=[kk], writes=[f"KT{tb}"])
            pv, kv_ = cx.bank()
            for sbk in range(4):
                proj_tm(xb_, kx, C_SWV, 64, pv[:, sbk * 64:(sbk + 1) * 64], kv_, sbk * 128, 128)
            I("act", "activation", out=Vm[:, tb * 4:(tb + 1) * 4, :], in_=pv[:, 0:256].rearrange("p (a b) -> p a b", a=4), func=AF.Copy,
              reads=[kv_], writes=[f"Vm{tb}"])
        for g in range(NB):
            qs = slice(g * 512, (g + 1) * 512)
            pc, kc = cx.bank()
            pp, kp = cx.bank()
            for j in range(4):
                blk = 4 * g + j
                I("pe", "matmul", pc[:, j * 128:(j + 1) * 128], lhsT=KT[0:64, blk * 128:(blk + 1) * 128], rhs=QT[0:64, blk * 128:(blk + 1) * 128],
                  start=True, stop=True, reads=[f"KT{g}", f"QT{g}"], writes=[kc])
                pb = max(blk - 1, 0)
                I("pe", "matmul", pp[:, j * 128:(j + 1) * 128], lhsT=KT[0:64, pb * 128:(pb + 1) * 128], rhs=QT[0:64, blk * 128:(blk + 1) * 128],
                  start=True, stop=True, reads=[f"KT{pb // 4}", f"QT{g}"], writes=[kp])
            ec, ep = tA[2], tA[3]
            I("act", "activation", out=ec[:], in_=pc[:], func=AF.Exp, reads=[kc], writes=["tA2"])
            I("act", "activation", out=ep[:], in_=pp[:], func=AF.Exp, reads=[kp], writes=["tA3"])
            pcb, ppb = tB[0], tB[1]
            I("dve", "tensor_tensor", out=pcb[:], in0=ec[:], in1=EB[0][:], op=ALU.mult, reads=["tA2", "EB0"], writes=["tB0"])
            ebp = 2 if g == 0 else 1
            I("pool", "tensor_tensor", out=ppb[:], in0=ep[:], in1=EB[ebp][:], op=ALU.mult, reads=["tA3", f"EB{ebp}"], writes=["tB1"])
            pso, kpo = cx.bank()
            psd, kpd = cx.bank()
            for j in range(4):
                blk = 4 * g + j
                pb = max(blk - 1, 0)
                cs = slice(j * 128, (j + 1) * 128)
                I("pe", "matmul", pso[0:64, cs], lhsT=Vm[:, blk, :], rhs=pcb[:, cs], start=True, stop=False, reads=[f"Vm{g}", "tB0"], writes=[kpo])
                I("pe", "matmul", pso[0:64, cs], lhsT=Vm[:, pb, :], rhs=ppb[:, cs], start=False, stop=True, reads=[f"Vm{pb // 4}", "tB1"], writes=[kpo])
                I("pe", "matmul", psd[0:64, cs], lhsT=ones[:, 0:64], rhs=pcb[:, cs], start=True, stop=False, reads=["ones", "tB0"], writes=[kpd])
                I("pe", "matmul", psd[0:64, cs], lhsT=ones[:, 0:64], rhs=ppb[:, cs], start=False, stop=True, reads=["ones", "tB1"], writes=[kpd])
            I("dve", "tensor_scalar", out=rden[:], in0=psd[0:64, :], scalar1=esink[0:64, 0:1], scalar2=None, op0=ALU.add, reads=[kpd, "esink"], writes=["rden"])
            I("dve", "reciprocal", out=rden[:], in_=rden[:], reads=["rden"], writes=["rden"])
            ystore(1, g, pso[0:64, :], kpo, rden[:], "rden")

    if "hgrn" in do:
        hgrn(cx, dr, S, layer, W, xload, proj_fm, proj_tm, ones, tA, tB, yout)


def hgrn(cx, dr, S, layer, W, xload, proj_fm, proj_tm, ones, tA, tB, yout):
    import os
    HG = int(os.environ.get("HG_STOP", "99"))
    nc, P = cx.nc, cx.P
    I = P.I
    sb = cx.sb
    SEG = min(2048, S)
    NSEG = S // SEG
    NBS = SEG // 512
    NCS = SEG // 32
    hc = sb("hgc", [64, 4], F32)
    P.dma("sp", hc[:], dr["hgc"], writes=["hgc"])
    ident = sb("identh", [64, 64], F32)
    P.dma("sp", ident[:], dr["ident64"], writes=["identh"])
    rmask = sb("rmask", [64, 512], F32)
    P.dma("sp", rmask[:], dr["rmask"], writes=["rmask"])
    m64 = sb("m64", [64, 512], F32)
    P.dma("sp", m64[:], dr["m64"], writes=["m64"])
    lb = sb("lb", [64, 1], F32)
    oml = sb("oml", [64, 1], F32)
    e2 = sb("e2", [64, 2], F32)
    I("act", "activation", out=e2[:], in_=hc[:, 0:2], func=AF.Exp, reads=["hgc"], writes=["e2"])
    I("dve", "tensor_tensor", out=lb[:], in0=e2[:, 0:1], in1=e2[:, 1:2], op=ALU.add, reads=["e2"], writes=["lb"])
    I("dve", "reciprocal", out=lb[:], in_=lb[:], reads=["lb"], writes=["lb"])
    I("dve", "tensor_tensor", out=lb[:], in0=lb[:], in1=e2[:, 1:2], op=ALU.mult, reads=["lb", "e2"], writes=["lb"])
    I("dve", "tensor_scalar", out=lb[:], in0=lb[:], scalar1=float(layer), scalar2=None, op0=ALU.mult, reads=["lb"], writes=["lb"])
    I("dve", "tensor_scalar", out=oml[:], in0=lb[:], scalar1=-1.0, scalar2=1.0, op0=ALU.mult, op1=ALU.add, reads=["lb"], writes=["oml"])
    QD = sb("QD", [64, SEG], BF16)
    KD = sb("KD", [64, SEG], BF16)
    SG = sb("SG", [64, SEG], BF16)
    KEt = sb("KEt", [64, 2, SEG // 64, 64], BF16)
    I("pool", "memset", KEt[:], 0.0, writes=[f"KEt{bi}" for bi in range(NBS)])
    Vt = sb("Vt", [64, SEG // 64, 64], BF16)
    Us = sb("Us", [64, NCS, 64], BF16)
    Ds = sb("Ds", [64, NCS], F32)
    Sst = sb("Sst", [64, NCS + 1, 64], F32)
    Sb = sb("Sb", [64, NCS, 64], BF16)
    attm = sb("attm", [64, 512], BF16)
    hq, hf, hl, hk, hb, he = (sb(f"h{n}", [64, 512], F32) for n in ("q", "f", "l", "k", "b", "e"))
    ke = sb("hke", [64, 512], F32)
    rs = sb("hrs", [64, 512], F32)
    sqb = sb("hsq", [64, 512], BF16)
    I("dve", "memset", Sst[:, 0, :], 0.0, writes=["Sst"])
    for sg in range(NSEG):
        for bi in range(NBS):
            tb = sg * NBS + bi
            ls = slice(bi * 512, (bi + 1) * 512)
            xb_, kx = xload(tb)
            pq, kq = cx.bank()
            proj_fm(xb_, kx, C_HQ, 64, pq[0:64, :], kq)
            pf, kf_ = cx.bank()
            proj_fm(xb_, kx, C_HF, 64, pf[0:64, :], kf_)
            pg, kg = cx.bank()
            proj_fm(xb_, kx, C_HG, 64, pg[0:64, :], kg)
            pv, kv_ = cx.bank()
            for pc_ in range(8):
                proj_tm(xb_, kx, C_HI, 64, pv[0:64, pc_ * 64:(pc_ + 1) * 64], kv_, pc_ * 64, 64)
            I("act", "activation", out=Vt[:, bi * 8:(bi + 1) * 8, :], in_=pv[0:64, :].rearrange("p (a b) -> p a b", a=8), func=AF.Copy, reads=[kv_], writes=[f"Vt{bi}"])
            I("act", "activation", out=hq[:], in_=pq[0:64, :], func=AF.Silu, reads=[kq], writes=["hq"])
            I("act", "activation", out=SG[:, ls], in_=pg[0:64, :], func=AF.Silu, reads=[kg], writes=[f"SG{bi}"])
            I("act", "activation", out=hf[:], in_=pf[0:64, :], func=AF.Sigmoid, reads=[kf_], writes=["hf"])
            I("dve", "tensor_scalar", out=hf[:], in0=hf[:], scalar1=oml[:, 0:1], scalar2=lb[:, 0:1], op0=ALU.mult, op1=ALU.add, reads=["hf", "oml", "lb"], writes=["hf"])
            I("act", "activation", out=hl[:], in_=hf[:], func=AF.Ln, reads=["hf"], writes=["hl"])
            I("pool", "tensor_scalar", out=hk[:], in0=hf[:], scalar1=-1.0, scalar2=1.0, op0=ALU.mult, op1=ALU.add, reads=["hf"], writes=["hk"])
            if HG < 1:
                continue
            I("dve", "tensor_tensor_scan", out=hb[:], data0=rmask[:], data1=hl[:], initial=0.0, op0=ALU.mult, op1=ALU.add, reads=["rmask", "hl"], writes=["hb"])
            I("act", "activation", out=he[:], in_=hb[:], func=AF.Exp, reads=["hb"], writes=["he"])
            I("dve", "tensor_tensor", out=QD[:, ls], in0=hq[:], in1=he[:], op=ALU.mult, reads=["hq", "he"], writes=[f"QD{bi}"])
            I("act", "activation", out=he[:], in_=hb[:], func=AF.Exp, scale=-1.0, reads=["hb"], writes=["he"])
            I("pool", "tensor_tensor", out=ke[:], in0=hk[:], in1=he[:], op=ALU.mult, reads=["hk", "he"], writes=["hke"])
            I("pool", "tensor_copy", out=KD[:, ls], in_=ke[:], reads=["hke"], writes=[f"KD{bi}"])
            if HG < 2:
                continue
            I("act", "activation", out=Ds[:, bi * 16:(bi + 1) * 16], in_=hb[:].rearrange("p (c t) -> p c t", t=32)[:, :, 31], func=AF.Exp,
              reads=["hb"], writes=[f"Ds{bi}"])
            for c in range(16):
                I("pool", "tensor_scalar", out=ke[:, c * 32:(c + 1) * 32], in0=ke[:, c * 32:(c + 1) * 32], scalar1=Ds[:, bi * 16 + c:bi * 16 + c + 1], scalar2=None,
                  op0=ALU.mult, reads=["hke", f"Ds{bi}"], writes=["hke"])
            if HG < 3:
                continue
            pt, kt = cx.bank()
            for pc_ in range(8):
                I("pe", "transpose", out=pt[0:64, pc_ * 64:(pc_ + 1) * 64], in_=ke[:, pc_ * 64:(pc_ + 1) * 64], identity=ident[:], reads=["hke", "identh"], writes=[kt])
            I("dve", "tensor_copy", out=KEt[0:32, 0, bi * 8:(bi + 1) * 8, :], in_=pt[0:32, :].rearrange("p (a b) -> p a b", a=8), reads=[kt], writes=[f"KEt{bi}"])
            I("dve", "tensor_copy", out=KEt[32:64, 1, bi * 8:(bi + 1) * 8, :], in_=pt[32:64, :].rearrange("p (a b) -> p a b", a=8), reads=[kt], writes=[f"KEt{bi}"])
            if HG < 4:
                continue
            pu = [cx.bank() for _ in range(2)]
            for c in range(16):
                pc_, half = c // 2, c % 2
                hs = slice(half * 32, (half + 1) * 32)
                pb_, kb_ = pu[c // 8]
                I("pe", "matmul", pb_[0:64, (c % 8) * 64:(c % 8 + 1) * 64], lhsT=KEt[:, half, bi * 8 + pc_, :], rhs=Vt[:, bi * 8 + pc_, :], start=True, stop=True,
                  reads=[f"KEt{bi}", f"Vt{bi}"], writes=[kb_])
            for k2 in range(2):
                I("act", "activation", out=Us[:, bi * 16 + k2 * 8:bi * 16 + k2 * 8 + 8, :], in_=pu[k2][0][0:64, :].rearrange("p (a b) -> p a b", a=8),
                  func=AF.Copy, reads=[pu[k2][1]], writes=[f"Us{bi}"])
        if HG < 5:
            continue
        allU = [f"Us{bi}" for bi in range(NBS)]
        allD = [f"Ds{bi}" for bi in range(NBS)]
        for v in range(64):
            I("dve", "tensor_tensor_scan", out=Sst[:, 1:NCS + 1, v], data0=Ds[:, 0:NCS], data1=Us[:, :, v], initial=Sst[:, 0, v:v + 1],
              op0=ALU.mult, op1=ALU.add, reads=allU + allD + ["Sst"], writes=["Sst"])
        I("pool", "tensor_copy", out=Sb[:], in_=Sst[:, 0:NCS, :], reads=["Sst"], writes=["Sb"])
        if HG < 6:
            continue
        for bi in range(NBS):
            tb = sg * NBS + bi
            ls = slice(bi * 512, (bi + 1) * 512)
            pa, ka = cx.bank()
            for pc_ in range(8):
                cs = slice(bi * 512 + pc_ * 64, bi * 512 + (pc_ + 1) * 64)
                I("pe", "matmul", pa[0:64, pc_ * 64:(pc_ + 1) * 64], lhsT=KD[:, cs], rhs=QD[:, cs], start=True, stop=True, reads=[f"KD{bi}", f"QD{bi}"], writes=[ka])
            I("dve", "tensor_tensor", out=attm[:], in0=pa[0:64, :], in1=m64[:], op=ALU.mult, reads=[ka, "m64"], writes=["attm"])
            po, ko = cx.bank()
            for pc_ in range(8):
                for half in range(2):
                    c = bi * 16 + pc_ * 2 + half
                    oc = slice(pc_ * 64 + half * 32, pc_ * 64 + half * 32 + 32)
                    c32 = slice(bi * 512 + pc_ * 64 + half * 32, bi * 512 + pc_ * 64 + half * 32 + 32)
                    I("pe", "matmul", po[0:64, oc], lhsT=Vt[:, bi * 8 + pc_, :], rhs=attm[:, oc], start=True, stop=False, reads=[f"Vt{bi}", "attm"], writes=[ko])
                    I("pe", "matmul", po[0:64, oc], lhsT=Sb[:, c, :], rhs=QD[:, c32], start=False, stop=True, reads=["Sb", f"QD{bi}"], writes=[ko])
            I("act", "activation", out=sqb[:], in_=po[0:64, :], func=AF.Square, reads=[ko], writes=["hsq"])
            pn, kn = cx.bank()
            I("pe", "matmul", pn[0:64, :], lhsT=ones[0:64, 0:64], rhs=sqb[:], start=True, stop=True, reads=["ones", "hsq"], writes=[kn])
            I("act", "activation", out=rs[:], in_=pn[0:64, :], func=AF.Ln, scale=1.0 / 64, bias=EPS, reads=[kn], writes=["hrs"])
            I("act", "activation", out=rs[:], in_=rs[:], func=AF.Exp, scale=-0.5, reads=["hrs"], writes=["hrs"])
            I("dve", "tensor_tensor", out=rs[:], in0=po[0:64, :], in1=rs[:], op=ALU.mult, reads=[ko, "hrs"], writes=["hrs"])
            i = cx.ycnt % 2
            cx.ycnt += 1
            I("dve", "scalar_tensor_tensor", out=yout[i][:], in0=rs[:], scalar=hc[:, 2:3], in1=SG[:, ls], op0=ALU.mult, op1=ALU.mult,
              reads=["hrs", "hgc", f"SG{bi}"], writes=[f"yout{i}"])
            P.dma("sp", dr["ydst"](2, tb), yout[i][:], reads=[f"yout{i}"], writes=["ybr"])
        if sg + 1 < NSEG:
            I("dve", "tensor_copy", out=Sst[:, 0, :], in_=Sst[:, NCS, :], reads=["Sst"], writes=["Sst"])


S_FULL = 8192
NTC = 2048
f32 = np.float32

IN_SPLITS = (('mla_cq', 256), ('mla_ckv', 128), ('mla_kr', 32), ('swa_q', 256), ('swa_k', 128), ('swa_v', 128),
             ('hgrn_q', 256), ('hgrn_f', 256), ('hgrn_i', 256), ('hgrn_g', 256), ('sb_q', 256), ('sb_k', 256), ('sb_v', 256), ('gates', 4096))
OFFS = {}
_o = 0
for _n, _w in IN_SPLITS:
    OFFS[_n] = _o
    _o += _w


def p1_consts():
    c = {}
    k = np.arange(128)[:, None]
    q = np.arange(512)[None, :]
    m = np.zeros((128, 8, 512), f32)
    for j in range(4):
        m[:, j, :] = (j * 128 + k) <= q
        m[:, 4 + j, :] = (j * 128 + k) < q
    c["masks"] = m.astype(ml_dtypes.bfloat16)
    tri = np.zeros((128, 2, 128), f32)
    jj = np.arange(128)[:, None]
    kk = np.arange(128)[None, :]
    tri[:, 0, :] = -(jj >= kk).astype(f32)
    tri[:, 1, :] = -1
    c["tri"] = tri.astype(ml_dtypes.bfloat16)
    inv = (10000.0 ** (-np.arange(16, dtype=f32) / 16)).astype(f32)
    rc = np.zeros((128, 2), f32)
    rc[64:96, 0] = np.concatenate([inv, inv]) / f32(2 * np.pi)
    rc[64:80, 1] = -1
    rc[80:96, 1] = 1
    c["ropec"] = rc
    kq = np.arange(128)
    c["dist"] = np.stack([(kq[None, :] - kq[:, None]).astype(f32), (128 + kq[None, :] - kq[:, None]).astype(f32)])
    c["ident64"] = np.eye(64, dtype=f32)
    rm = np.ones((64, 512), f32)
    rm[:, ::32] = 0
    c["rmask"] = rm
    s_ = np.arange(64)[:, None]
    t_ = np.arange(64)[None, :]
    c["m64"] = np.tile(((s_ // 32 == t_ // 32) & (s_ <= t_)).astype(f32), (1, 8))
    return c


def bucket_lo():
    n = np.arange(128)
    nf = np.maximum(n, 1).astype(f32)
    large = 16 + (np.log(nf / f32(16)) / f32(np.log(128 / 16)) * f32(16)).astype(np.int32)
    bucket = np.where(n < 16, n, np.clip(large, 0, 31))
    return [int(np.min(np.nonzero(bucket >= b)[0])) if np.any(bucket >= b) else 100000 for b in range(1, 32)]


def p1_head_inputs(inp, l, b, h):
    w_in = inp["w_in"][l]

    def cols(name, a, b_):
        return w_in[:, OFFS[name] + a: OFFS[name] + b_]
    kr = cols("mla_kr", 0, 32)
    krs = np.concatenate([kr[:, 16:32], kr[:, 0:16]], axis=1)
    g = h // 2
    hs = slice(h * 64, h * 64 + 64)
    wh = np.concatenate([cols("mla_cq", 0, 256), cols("mla_ckv", 0, 128), kr, krs,
                         cols("swa_q", h * 64, h * 64 + 64), cols("swa_k", g * 64, g * 64 + 64), cols("swa_v", g * 64, g * 64 + 64),
                         cols("hgrn_q", h * 64, h * 64 + 64), cols("hgrn_f", h * 64, h * 64 + 64), cols("hgrn_g", h * 64, h * 64 + 64),
                         cols("hgrn_i", h * 64, h * 64 + 64),
                         cols("sb_q", h * 64, h * 64 + 64), cols("sb_k", h * 64, h * 64 + 64), cols("sb_v", h * 64, h * 64 + 64)], axis=1)
    uq = inp["mla_w_uq"][l][:, h * 96:(h + 1) * 96]
    wuq = np.concatenate([uq, uq[:, 80:96], uq[:, 64:80]], axis=1)
    wukv = inp["mla_w_ukv"][l][:, h * 128:(h + 1) * 128]
    swc = np.zeros((128, 34), f32)
    swc[:, 0:32] = inp["rel_bias_table"][:, h][None, :]
    swc[:, 32] = inp["swa_sinks"][l][h]
    hgc = np.zeros((64, 4), f32)
    hgc[:, 0] = inp["hgrn_lb_logits"][0, hs]
    hgc[:, 1] = inp["hgrn_lb_logits"][1, hs]
    hgc[:, 2] = inp["hgrn_norm"][l][hs]
    pos_b = inp["positions"][b]
    return dict(wh=np.ascontiguousarray(wh), wuq=np.ascontiguousarray(wuq), wukv=np.ascontiguousarray(wukv),
                qnorm=np.ascontiguousarray(inp["mla_q_norm"][l].reshape(2, 128).T), kvnorm=np.ascontiguousarray(inp["mla_kv_norm"][l].reshape(128, 1)),
                posb=np.ascontiguousarray(np.broadcast_to(pos_b[None, :], (32, pos_b.shape[0]))).astype(np.int32), swc=swc, hgc=hgc)


def il13(w, F):
    a, g = w[:, :F].reshape(1024, F // 128, 128), w[:, F:].reshape(1024, F // 128, 128)
    return np.ascontiguousarray(np.stack([a, g], axis=2).reshape(1024, 2 * F))


def p2_shared_inputs(inp, l):
    w_in = inp["w_in"][l]
    wg = w_in[:, OFFS["gates"]:OFFS["gates"] + 4096]
    d = dict(wg=np.ascontiguousarray(wg.reshape(1024, 4, 8, 128).transpose(0, 2, 1, 3).reshape(1024, 4096)),
             wbr=np.ascontiguousarray(inp["w_branch"][l].reshape(1024, 1024)), wout=np.ascontiguousarray(inp["w_out"][l]),
             lng=np.ascontiguousarray(inp["ln_g"][l].reshape(3, 8, 128).transpose(2, 0, 1)),
             lnb=np.ascontiguousarray(inp["ln_b"][l].reshape(3, 8, 128).transpose(2, 0, 1)),
             wq=np.ascontiguousarray(inp["xa_wq"][l]), wkv=np.ascontiguousarray(inp["xa_wkv"][l]), wo=np.ascontiguousarray(inp["xa_wo"][l]))
    if l % 2 == 0:
        d.update(w13=il13(inp["ffn_w13"][l // 2], 2816), w2=np.ascontiguousarray(inp["ffn_w2"][l // 2]))
    else:
        sel = np.zeros((8, 8, 128), f32)
        for e in range(8):
            sel[e, e, :] = 1
        d.update(router=np.ascontiguousarray(inp["moe_router"][l // 2]), w13=np.stack([il13(inp["moe_w13"][l // 2][e], 3584) for e in range(8)]),
                 w2=np.ascontiguousarray(inp["moe_w2"][l // 2]), sel=sel, ident=np.eye(128, dtype=f32))
    return d


GROUPS = [[0, 1, 2, 3], [4, 5, 6, 7]]
_NPDT = {"float32": F32, "int32": I32, "bfloat16": BF16}


def core_inputs(inp, consts, c):
    b, q = c // 4, c % 4
    x = inp["x"]
    d = dict(xT0=np.ascontiguousarray(x[b].T), x32=np.ascontiguousarray(x[b, q * NTC:(q + 1) * NTC, :].T),
             memT=np.ascontiguousarray(inp["mem"][b].T))
    sel4 = np.zeros((128, 4), f32)
    sel4[:, q] = 1
    d["sel4"] = sel4
    d.update(consts)
    for l in range(2):
        p1 = p1_head_inputs(inp, l, b, q)
        if l == 1:
            p1.pop("posb")
        for k, v in p1.items():
            d[(f"l{l}_" + k) if k != "posb" else k] = v
    return d


def build_fused(sample, shared_shapes):
    nc = bass.Bass("TRN2", target_bir_lowering=False)
    S = S_FULL
    ext = {}
    for k, v in list(sample.items()) + list(shared_shapes.items()):
        ext[k] = nc.dram_tensor(k, list(v.shape), _NPDT[str(v.dtype)], kind="ExternalInput").ap()
    out = nc.dram_tensor("out", [1024, NTC], F32, kind="ExternalOutput").ap()
    ysrc = [[nc.dram_tensor(f"ysrc{l}_{i}", [256, 512], BF16) for i in range(16)] for l in range(2)]
    ygat = [[nc.dram_tensor(f"ygat{l}_{i}", [1024, 512], BF16) for i in range(16)] for l in range(2)]
    xsrc = [nc.dram_tensor(f"xsrc_{i}", [256, 512], BF16) for i in range(16)]
    xgat = [nc.dram_tensor(f"xgat_{i}", [1024, 512], BF16) for i in range(16)]
    x32mid = nc.dram_tensor("x32mid", [1024, NTC], F32).ap()
    blo = bucket_lo()
    cnames = ("masks", "tri", "ropec", "dist", "ident64", "rmask", "m64")

    with contextlib.ExitStack() as semst:
        def allgather(srcs, dsts, tag):
            cc = semst.enter_context(nc.semaphore(f"cc_{tag}"))
            with nc.Block() as block:
                @block.gpsimd
                def _(g):
                    for s_, d_ in zip(srcs, dsts):
                        g.collective_compute("AllGather", ALU.bypass, replica_groups=GROUPS, ins=[s_.ap().opt()], outs=[d_.ap().opt()]).then_inc(cc, 1)
                    g.wait_ge(cc, len(srcs))
            nc.all_engine_barrier()

        for l in range(2):
            P = Prog(nc)
            with contextlib.ExitStack() as st:
                cx = Ctx1(nc, P, st)
                dr = {k: ext[k] for k in cnames}
                dr["posb"] = ext["posb"]
                for k in ("wh", "wuq", "wukv", "qnorm", "kvnorm", "swc", "hgc"):
                    dr[k] = ext[f"l{l}_{k}"]
                dr["bucket_lo"] = blo
                if l == 0:
                    dr["xsrc"] = lambda tb: [(slice(0, 8), ext["xT0"].rearrange("(c p) t -> p c t", p=128)[:, :, tb * 512:(tb + 1) * 512])]
                else:
                    dr["xsrc"] = lambda tb: [(slice(2 * fc, 2 * fc + 2),
                                              xgat[fc * 4 + tb % 4].ap()[(tb // 4) * 256:(tb // 4 + 1) * 256, :].rearrange("(c p) t -> p c t", p=128))
                                             for fc in range(4)]
                dr["ydst"] = lambda br, qb, l=l: ysrc[l][qb].ap()[br * 64:(br + 1) * 64, :]
                phase1(cx, dr, S, l)
                P.finalize()
                P.emit(sem_stack=semst)
            nc.all_engine_barrier()
            allgather(ysrc[l], ygat[l], f"y{l}")
            P = Prog(nc)
            with contextlib.ExitStack() as st:
                cx = Ctx(nc, P, st)
                dr = dict(memT=ext["memT"], sel4=ext["sel4"])
                p2keys = ["wg", "wbr", "wout", "lng", "lnb", "wq", "wkv", "wo", "w13", "w2"] + (["router", "sel", "ident"] if l % 2 == 1 else [])
                for k in p2keys:
                    dr[k] = ext[f"l{l}_{k}"]
                dr["x32"] = ext["x32"] if l == 0 else x32mid
                dr["xo32"] = x32mid if l == 0 else out
                dr["ygather"] = lambda jq, lb, l=l: ygat[l][jq * 4 + lb].ap()
                if l == 0:
                    dr["xchunk"] = lambda fc, lb: xsrc[fc * 4 + lb].ap()
                phase2(cx, dr, l % 2 == 1, NTC)
                P.finalize()
                P.emit(sem_stack=semst)
            nc.all_engine_barrier()
            if l == 0:
                allgather(xsrc, xgat, "x")
    return nc


def kernel(**inp):
    inp = {k: np.asarray(v) for k, v in inp.items()}
    B, S, Dm = inp["x"].shape
    consts = p1_consts()
    cores = list(range(8))
    shared = {}
    for l in range(2):
        for k, v in p2_shared_inputs(inp, l).items():
            shared[f"l{l}_{k}"] = v
    in_maps = []
    for c in cores:
        d = core_inputs(inp, consts, c)
        if c == 0:
            nc = build_fused(d, shared)
        d.update(shared)
        in_maps.append(d)
    res = run_bass_kernel_spmd(nc, in_maps, core_ids=cores).results
    out = np.empty((B, S, Dm), np.float32)
    for c in cores:
        out[c // 4, (c % 4) * NTC:(c % 4 + 1) * NTC, :] = np.asarray(res[c]["out"]).T
    return out
```

```python
import math
import contextlib
import numpy as np
import ml_dtypes
import concourse.bass as bass
import concourse.mybir as mybir
from concourse.bass_utils import run_bass_kernel_spmd


F32 = mybir.dt.float32
BF16 = mybir.dt.bfloat16
I32 = mybir.dt.int32
AF = mybir.ActivationFunctionType
ALU = mybir.AluOpType
AX = mybir.AxisListType

ENGS = ("pe", "act", "dve", "pool", "sp")
SEM_CHUNK = 12000
RING = 8


class Op:
    __slots__ = ("eng", "fn", "reads", "writes", "is_dma", "deps", "pos", "gid",
                 "waits", "vc", "need_inc", "slot", "round", "cnt")

    def __init__(self, eng, fn, reads, writes, is_dma):
        self.eng = eng
        self.fn = fn
        self.reads = reads
        self.writes = writes
        self.is_dma = is_dma
        self.deps = ()
        self.waits = []
        self.vc = None
        self.need_inc = False
        self.slot = None
        self.round = None
        self.cnt = None


class Prog:
    def __init__(self, nc):
        self.nc = nc
        self.ops = []
        self.last_w = {}
        self.readers = {}

    def op(self, eng, fn, reads=(), writes=(), dma=False):
        o = Op(eng, fn, tuple(reads), tuple(writes), dma)
        o.gid = len(self.ops)
        deps = set()
        for k in o.reads:
            w = self.last_w.get(k)
            if w is not None:
                deps.add(w)
        for k in o.writes:
            w = self.last_w.get(k)
            if w is not None:
                deps.add(w)
            for r in self.readers.get(k, ()):
                deps.add(r)
        deps.discard(o.gid)
        o.deps = tuple(sorted(deps))
        for k in o.reads:
            self.readers.setdefault(k, []).append(o.gid)
        for k in o.writes:
            self.last_w[k] = o.gid
            self.readers[k] = []
        self.ops.append(o)
        return o

    def dma(self, eng, out, in_, reads=(), writes=(), **kw):
        return self.op(eng, lambda e: e.dma_start(out=out, in_=in_, **kw), reads, writes, dma=True)

    def I(self, eng, meth, *args, reads=(), writes=(), **kw):
        return self.op(eng, lambda e: getattr(e, meth)(*args, **kw), reads, writes)

    def finalize(self, final_wait_keys=()):
        nc = self.nc
        ops = self.ops
        streams = {e: [] for e in ENGS}
        for o in ops:
            o.pos = len(streams[o.eng])
            streams[o.eng].append(o)
        dma_count = {e: 0 for e in ENGS}
        for o in ops:
            if o.is_dma:
                i = dma_count[o.eng]
                dma_count[o.eng] += 1
                o.slot = (o.eng, i % RING)
                o.round = i // RING + 1
        know = {e: {} for e in ENGS}
        slot_last = {}

        def completion_key(d):
            return (d.slot, d.round) if d.is_dma else (d.eng, d.pos + 1)

        for o in ops:
            K = know[o.eng]
            need = {}
            if o.is_dma:
                prev = slot_last.get(o.slot)
                if prev is not None:
                    need[prev.slot] = (prev.round, prev)
                slot_last[o.slot] = o
            for di in o.deps:
                d = ops[di]
                if d.eng == o.eng and not d.is_dma and not o.is_dma:
                    if o.eng == "pe":
                        continue
                ck, cv = completion_key(d)
                if K.get(ck, 0) >= cv:
                    continue
                if ck not in need or need[ck][0] < cv:
                    need[ck] = (cv, d)
            items = sorted(need.items(), key=lambda kv: -kv[1][1].gid)
            for ck, (cv, d) in items:
                if K.get(ck, 0) >= cv:
                    continue
                o.waits.append(d)
                d.need_inc = True
                for k2, v2 in d.vc.items():
                    if K.get(k2, 0) < v2:
                        K[k2] = v2
            vc = dict(K)
            ck, cv = completion_key(o)
            vc[ck] = cv
            o.vc = vc
            if not o.is_dma and o.eng != "sp":
                pass
        finals = []
        for k in final_wait_keys:
            w = self.last_w.get(k)
            if w is not None:
                ops[w].need_inc = True
                finals.append(ops[w])
        sem_needed = {}
        for e in ENGS:
            c = 0
            for o in streams[e]:
                if o.is_dma:
                    o.need_inc = True
                    continue
                if o.need_inc:
                    c += 1
                    o.cnt = c
            sem_needed[e] = (c + SEM_CHUNK - 1) // SEM_CHUNK
        self.streams = streams
        self.finals = finals
        self.sem_needed = sem_needed
        self.dma_count = dma_count

    def emit(self, sem_stack=None):
        nc = self.nc
        streams = self.streams
        import contextlib
        with contextlib.ExitStack() as st:
            if sem_stack is not None:
                blk_stack, st = st, sem_stack
            else:
                blk_stack = st
            esems = {}
            for e in ENGS:
                esems[e] = [st.enter_context(nc.semaphore(f"s{id(self) % 100000}_{e}_{i}")) for i in range(self.sem_needed[e])]
            ssems = {}
            for e in ENGS:
                if self.dma_count[e] > 0:
                    for r in range(min(RING, self.dma_count[e])):
                        ssems[(e, r)] = st.enter_context(nc.semaphore(f"d{id(self) % 100000}_{e}_{r}"))
            block = blk_stack.enter_context(nc.Block())

            def wait_args(d):
                if d.is_dma:
                    return ssems[d.slot], 16 * d.round
                c = d.cnt - 1
                return esems[d.eng][c // SEM_CHUNK], (c % SEM_CHUNK) + 1

            def run(ename, eng):
                for o in streams[ename]:
                    for d in o.waits:
                        s, v = wait_args(d)
                        eng.wait_ge(s, v)
                    ins = o.fn(eng)
                    if o.need_inc:
                        if o.is_dma:
                            ins.then_inc(ssems[o.slot], 16)
                        else:
                            c = o.cnt - 1
                            ins.then_inc(esems[o.eng][c // SEM_CHUNK], 1)
                if ename == "sp":
                    last = {}
                    for o in self.ops:
                        if o.is_dma:
                            last[o.slot] = o.round
                    for slot, rnd in last.items():
                        eng.wait_ge(ssems[slot], 16 * rnd)

            @block.sync
            def _(eng):
                run("sp", eng)

            @block.tensor
            def _(eng):
                run("pe", eng)

            @block.scalar
            def _(eng):
                run("act", eng)

            @block.vector
            def _(eng):
                run("dve", eng)

            @block.gpsimd
            def _(eng):
                run("pool", eng)

    def stats(self):
        return {e: len(s) for e, s in self.streams.items()}


D = 1024
ALPHA = 4 ** 0.25
EPS = 1e-5
F_DENSE = 2816
F_EXP = 3584
NEXP = 8
TP = 1024
TB = 2


_UID = globals().get('_UID', [0])


class Ctx:
    def __init__(self, nc, P, st):
        self.nc, self.P, self.st = nc, P, st
        _UID[0] += 1
        self.uid = _UID[0]
        self.nbank = 0
        self.banks = [st.enter_context(nc.psum_tensor(f"u{_UID[0]}_ps{i}", [128, 512], F32)) for i in range(8)]
        self.wslot = 0

    def sb(self, name, shape, dt):
        return self.st.enter_context(self.nc.sbuf_tensor(f"u{self.uid}_sb_" + name, shape, dt))

    def bank(self):
        i = self.nbank % 8
        self.nbank += 1
        return self.banks[i], f"ps{i}"


def TS(tb):
    return slice(tb * 512, (tb + 1) * 512)


def phase2(cx, dr, is_moe, NTC):
    nc, P = cx.nc, cx.P
    I = P.I
    NPASS = NTC // TP
    sb = cx.sb
    r32 = sb("r32", [128, 8, TP], F32)
    xb = sb("xb", [128, 8, TP], BF16)
    big = sb("big", [128, 28, TP], BF16)
    gt = [sb(f"gt{n}", [128, 512], F32) for n in range(4)]
    rbv = lambda c: big[:, c // 2, (c % 2) * 512:(c % 2 + 1) * 512]
    sqv = lambda c: big[:, 4 + c // 2, (c % 2) * 512:(c % 2 + 1) * 512]
    rbk = lambda c: f"big{c // 2}_{c % 2}"
    sqk = lambda c: f"big{4 + c // 2}_{c % 2}"
    mean = sb("mean", [128, 512], F32)
    msq = sb("msq", [128, 512], F32)
    var = sb("var", [128, 512], F32)
    rstd = sb("rstd", [128, 512], F32)
    lnt = [sb(f"lnt{i}", [128, 512], F32) for i in range(2)]
    lng = sb("lng", [128, 3, 8], F32)
    lnb = sb("lnb", [128, 3, 8], F32)
    ones = sb("ones", [128, 128], BF16)
    memb = sb("memb", [128, 8, 256], BF16)
    KT = sb("KT", [64, 4, 256], BF16)
    Vx = sb("Vx", [128, 2, 256], BF16)
    PT = [sb(f"PT{i}", [128, 512], BF16) for i in range(2)]
    rden = sb("rden", [64, 512], F32)
    WSZ = 7168
    NW = 3
    wbuf = [sb(f"wbuf{i}", [128, WSZ], BF16) for i in range(NW)]
    sa = [sb(f"sa{i}", [128, 512], F32) for i in range(2)]
    R = lambda c, tb: f"r32_{c}_{tb}"
    X = lambda c, tb: f"xb_{c}_{tb}"
    B = lambda j, tb: f"big{j}_{tb}"

    I("dve", "memset", ones[:], 1.0, writes=["ones"])
    if "ygather" in dr:
        dr["sel4_sb"] = sb("sel4", [128, 4], F32)
        P.dma("sp", dr["sel4_sb"][:], dr["sel4"], writes=["sel4"])
    P.dma("sp", lng[:], dr["lng"], writes=["lng"])
    P.dma("sp", lnb[:], dr["lnb"], writes=["lnb"])

    def wload(src_ap, kc, ncols, parts=128):
        i = cx.wslot % NW
        cx.wslot += 1
        assert kc * ncols <= WSZ
        view = wbuf[i][0:parts, 0:kc * ncols].rearrange("p (c n) -> p c n", c=kc)
        P.dma("pool", view, src_ap, writes=[f"wbuf{i}"])
        return view, f"wbuf{i}"

    def layernorm(tb, li):
        ts = TS(tb)
        for c in range(8):
            I("pool", "tensor_copy", out=rbv(c), in_=r32[:, c, ts], reads=[R(c, tb)], writes=[rbk(c)])
            I("act", "activation", out=sqv(c), in_=r32[:, c, ts], func=AF.Square, reads=[R(c, tb)], writes=[sqk(c)])
        ps_s, ks = cx.bank()
        for c in range(8):
            I("pe", "matmul", ps_s[:], lhsT=ones[:], rhs=rbv(c), start=(c == 0), stop=(c == 7), reads=["ones", rbk(c)], writes=[ks])
        ps_q, kq = cx.bank()
        for c in range(8):
            I("pe", "matmul", ps_q[:], lhsT=ones[:], rhs=sqv(c), start=(c == 0), stop=(c == 7), reads=["ones", sqk(c)], writes=[kq])
        I("act", "activation", out=mean[:], in_=ps_s[:], func=AF.Copy, scale=1.0 / D, reads=[ks], writes=["mean"])
        I("pool", "tensor_tensor", out=msq[:], in0=mean[:], in1=mean[:], op=ALU.mult, reads=["mean"], writes=["msq"])
        I("dve", "scalar_tensor_tensor", out=var[:], in0=ps_q[:], scalar=1.0 / D, in1=msq[:], op0=ALU.mult, op1=ALU.subtract,
          reads=[kq, "msq"], writes=["var"])
        I("dve", "tensor_scalar", out=var[:], in0=var[:], scalar1=EPS, scalar2=None, op0=ALU.add, reads=["var"], writes=["var"])
        I("act", "activation", out=rstd[:], in_=var[:], func=AF.Ln, reads=["var"], writes=["rstd"])
        I("act", "activation", out=rstd[:], in_=rstd[:], func=AF.Exp, scale=-0.5, reads=["rstd"], writes=["rstd"])
        for c in range(8):
            t = lnt[c % 2]
            kt = f"lnt{c % 2}"
            I("dve", "tensor_tensor", out=t[:], in0=r32[:, c, ts], in1=mean[:], op=ALU.subtract, reads=[R(c, tb), "mean"], writes=[kt])
            I("pool", "tensor_tensor", out=t[:], in0=t[:], in1=rstd[:], op=ALU.mult, reads=[kt, "rstd"], writes=[kt])
            I("act", "activation", out=r32[:, c, ts], in_=t[:], func=AF.Identity, scale=lng[:, li, c:c + 1], bias=lnb[:, li, c:c + 1],
              reads=[kt, "lng", "lnb"], writes=[R(c, tb)])
            I("pool", "tensor_copy", out=xb[:, c, ts], in_=r32[:, c, ts], reads=[R(c, tb)], writes=[X(c, tb)])

    def resid_evac(ps, kps, m, tb):
        ts = TS(tb)
        I("dve", "scalar_tensor_tensor", out=r32[:, m, ts], in0=r32[:, m, ts], scalar=ALPHA, in1=ps[:], op0=ALU.mult, op1=ALU.add,
          reads=[kps, R(m, tb)], writes=[R(m, tb)])

    for ps_i in range(NPASS):
        t0 = ps_i * TP
        P.dma("sp", r32[:], dr["x32"].rearrange("(c p) t -> p c t", p=128)[:, :, t0:t0 + TP],
              writes=[R(c, tb) for c in range(8) for tb in range(TB)])
        for c in range(8):
            for tb in range(TB):
                I("pool", "tensor_copy", out=xb[:, c, TS(tb)], in_=r32[:, c, TS(tb)], reads=[R(c, tb)], writes=[X(c, tb)])
        if "ygather" not in dr:
            for n in range(4):
                P.dma("sp", big[:, 2 * n:2 * n + 2, :], dr["ybr"][n].rearrange("(c p) t -> p c t", p=128)[:, :, t0:t0 + TP],
                      writes=[B(2 * n + j, tb) for j in range(2) for tb in range(TB)])
        else:
            sel4 = dr["sel4_sb"]
            for tb in range(TB):
                for jq in range(4):
                    stg = big[:, 8 + 4 * (jq % 2):12 + 4 * (jq % 2), :].rearrange("p a (b t) -> p (a b) t", t=512)
                    kst = [B(8 + 4 * (jq % 2) + a, tb2) for a in range(4) for tb2 in range(TB)]
                    src = dr["ygather"](jq, ps_i * TB + tb)
                    for hh in range(2):
                        for c2 in range(2):
                            sv = src.rearrange("(c2 hh n d) t -> c2 hh d n t", c2=2, hh=2, n=4)[c2, hh]
                            dv = stg[hh * 64:(hh + 1) * 64].rearrange("p (n c2) t -> p c2 n t", c2=2)[:, c2]
                            P.dma("sp", dv, sv, writes=kst)
                    dst = big[:, 0:8, TS(tb)]
                    kd = [B(a, tb) for a in range(8)]
                    if jq == 0:
                        I("dve", "tensor_scalar", out=dst, in0=stg, scalar1=sel4[:, 0:1], scalar2=None, op0=ALU.mult, reads=kst + ["sel4"], writes=kd)
                    else:
                        I("dve", "scalar_tensor_tensor", out=dst, in0=stg, scalar=sel4[:, jq:jq + 1], in1=dst, op0=ALU.mult, op1=ALU.add,
                          reads=kst + kd + ["sel4"], writes=kd)
        planA = []
        for m in range(8):
            planA.append((dr["wg"].rearrange("(c p) n -> p c n", p=128)[:, :, m * 512:(m + 1) * 512], 8, 512))
            planA.append((dr["wbr"].rearrange("(c p) n -> p c n", p=128)[:, :, m * 128:(m + 1) * 128], 8, 128))
        for m in range(8):
            planA.append((dr["wout"].rearrange("(c p) n -> p c n", p=128)[:, :, m * 128:(m + 1) * 128], 8, 128))
        wsA = WStream(wload, planA, depth=1)
        for m in range(8):
            wg, kwg = wsA.next()
            wbr, kwbr = wsA.next()
            for tb in range(TB):
                ts = TS(tb)
                for n in range(4):
                    ps, kps = cx.bank()
                    for c in range(8):
                        I("pe", "matmul", ps[:], lhsT=wg[:, c, n * 128:(n + 1) * 128], rhs=xb[:, c, ts], start=(c == 0), stop=(c == 7),
                          reads=[kwg, X(c, tb)], writes=[kps])
                    I("act", "activation", out=gt[n][:], in_=ps[:], func=AF.Sigmoid, reads=[kps], writes=[f"gt{n}"])
                for n in range(4):
                    ps, kps = cx.bank()
                    for c2 in range(2):
                        I("pe", "matmul", ps[:], lhsT=wbr[:, n * 2 + c2, :], rhs=big[:, n * 2 + c2, ts], start=(c2 == 0), stop=(c2 == 1),
                          reads=[kwbr, B(n * 2 + c2, tb)], writes=[kps])
                    I("dve", "tensor_tensor", out=gt[n][:], in0=gt[n][:], in1=ps[:], op=ALU.mult, reads=[kps, f"gt{n}"], writes=[f"gt{n}"])
                I("pool", "tensor_tensor", out=gt[0][:], in0=gt[0][:], in1=gt[1][:], op=ALU.add, reads=["gt0", "gt1"], writes=["gt0"])
                I("pool", "tensor_tensor", out=gt[2][:], in0=gt[2][:], in1=gt[3][:], op=ALU.add, reads=["gt2", "gt3"], writes=["gt2"])
                I("pool", "tensor_tensor", out=big[:, 8 + m, ts], in0=gt[0][:], in1=gt[2][:], op=ALU.add, reads=["gt0", "gt2"], writes=[B(8 + m, tb)])
        for m in range(8):
            wo_, kwo = wsA.next()
            for tb in range(TB):
                ps, kps = cx.bank()
                for c in range(8):
                    I("pe", "matmul", ps[:], lhsT=wo_[:, c, :], rhs=big[:, 8 + c, TS(tb)], start=(c == 0), stop=(c == 7),
                      reads=[kwo, B(8 + c, tb)], writes=[kps])
                resid_evac(ps, kps, m, tb)
        for tb in range(TB):
            layernorm(tb, 0)
        if ps_i == 0:
            P.dma("pool", memb[:], dr["memT"].rearrange("(c p) t -> p c t", p=128), writes=["memb"])
            wkv, kwkv = wload(dr["wkv"].rearrange("(c p) n -> p c n", p=128), 8, 512)
            for h in range(4):
                ps, kps = cx.bank()
                for c in range(8):
                    I("pe", "matmul", ps[0:64, 0:256], lhsT=wkv[:, c, h * 64:(h + 1) * 64], rhs=memb[:, c, :], start=(c == 0), stop=(c == 7),
                      reads=[kwkv, "memb"], writes=[kps])
                I("act", "activation", out=KT[:, h, :], in_=ps[0:64, 0:256], func=AF.Copy, reads=[kps], writes=["KT"])
            for mt in range(2):
                ps, kps = cx.bank()
                for c in range(8):
                    I("pe", "matmul", ps[:, 0:256], lhsT=memb[:, c, mt * 128:(mt + 1) * 128], rhs=wkv[:, c, 256:512], start=(c == 0), stop=(c == 7),
                      reads=[kwkv, "memb"], writes=[kps])
                I("act", "activation", out=Vx[:, mt, :], in_=ps[:, 0:256], func=AF.Copy, reads=[kps], writes=["Vx"])
        wq, kwq = wload(dr["wq"].rearrange("(c p) n -> p c n", p=128), 8, 256)
        for tb in range(TB):
            ts = TS(tb)
            for h in range(4):
                ps, kps = cx.bank()
                for c in range(8):
                    I("pe", "matmul", ps[0:64, :], lhsT=wq[:, c, h * 64:(h + 1) * 64], rhs=xb[:, c, ts], start=(c == 0), stop=(c == 7),
                      reads=[kwq, X(c, tb)], writes=[kps])
                I("act", "activation", out=big[0:64, 16 + h, ts], in_=ps[0:64, :], func=AF.Copy, scale=0.125, reads=[kps], writes=[B(16 + h, tb)])
            for h in range(4):
                for mt in range(2):
                    ps, kps = cx.bank()
                    I("pe", "matmul", ps[:], lhsT=KT[:, h, mt * 128:(mt + 1) * 128], rhs=big[0:64, 16 + h, ts], start=True, stop=True,
                      reads=["KT", B(16 + h, tb)], writes=[kps])
                    I("act", "activation", out=PT[mt][:], in_=ps[:], func=AF.Exp, reads=[kps], writes=[f"PT{mt}"])
                pso, kpo = cx.bank()
                for mt in range(2):
                    I("pe", "matmul", pso[0:64, :], lhsT=Vx[:, mt, h * 64:(h + 1) * 64], rhs=PT[mt][:], start=(mt == 0), stop=(mt == 1),
                      reads=["Vx", f"PT{mt}"], writes=[kpo])
                psd, kpd = cx.bank()
                for mt in range(2):
                    I("pe", "matmul", psd[0:64, :], lhsT=ones[:, 0:64], rhs=PT[mt][:], start=(mt == 0), stop=(mt == 1),
                      reads=["ones", f"PT{mt}"], writes=[kpd])
                I("dve", "reciprocal", out=rden[:], in_=psd[0:64, :], reads=[kpd], writes=["rden"])
                I("dve", "tensor_tensor", out=big[0:64, 20 + h, ts], in0=pso[0:64, :], in1=rden[:], op=ALU.mult,
                  reads=[kpo, "rden"], writes=[B(20 + h, tb)])
        wo2, kwo2 = wload(dr["wo"].rearrange("(h p) n -> p h n", p=64), 4, 1024, parts=64)
        for tb in range(TB):
            ts = TS(tb)
            for m in range(8):
                ps, kps = cx.bank()
                for h in range(4):
                    I("pe", "matmul", ps[:], lhsT=wo2[0:64, h, m * 128:(m + 1) * 128], rhs=big[0:64, 20 + h, ts], start=(h == 0), stop=(h == 3),
                      reads=[kwo2, B(20 + h, tb)], writes=[kps])
                resid_evac(ps, kps, m, tb)
        for tb in range(TB):
            layernorm(tb, 1)
        if not is_moe:
            ffn_expert(cx, WStream(wload, ffn_plan(dr["w13"], dr["w2"], F_DENSE)), F_DENSE, r32, xb, big, sa, None, True)
        else:
            moe(cx, dr, r32, xb, big, sa, wload)
        for tb in range(TB):
            layernorm(tb, 2)
        P.dma("sp", dr["xo32"].rearrange("(c p) t -> p c t", p=128)[:, :, t0:t0 + TP], r32[:],
              reads=[R(c, tb) for c in range(8) for tb in range(TB)], writes=["xo32"])
        if "xob" in dr:
            P.dma("sp", dr["xob"].rearrange("(c p) t -> p c t", p=128)[:, :, t0:t0 + TP], xb[:],
                  reads=[X(c, tb) for c in range(8) for tb in range(TB)], writes=["xob"])
        if "xchunk" in dr:
            for fc in range(4):
                for tb in range(TB):
                    P.dma("sp", dr["xchunk"](fc, ps_i * TB + tb).rearrange("(c p) t -> p c t", p=128), xb[:, 2 * fc:2 * fc + 2, TS(tb)],
                          reads=[X(c, tb) for c in (2 * fc, 2 * fc + 1)], writes=["xob"])


class WStream:
    def __init__(self, wload, plan, depth=2):
        self.wload, self.plan, self.depth = wload, plan, depth
        self.issued = []
        self.pos = 0

    def next(self):
        i = self.pos
        while len(self.issued) < min(len(self.plan), i + 1 + self.depth):
            self.issued.append(self.wload(*self.plan[len(self.issued)]))
        self.pos += 1
        return self.issued[i]


def ffn_plan(w13, w2, F):
    FT = F // 128
    plan = []
    w13v = w13.rearrange("(c p) n -> p c n", p=128)
    for f0 in range(0, FT, 2):
        nf = min(2, FT - f0)
        plan.append((w13v[:, :, f0 * 256:(f0 + nf) * 256], 8, nf * 256))
    w2v = w2.rearrange("(c p) n -> p c n", p=128)
    for m0 in range(0, 8, 2):
        plan.append((w2v[:, :, m0 * 128:(m0 + 2) * 128], FT, 256))
    return plan


def ffn_expert(cx, ws, F, r32, xb, big, sa, wbc, first_scale):
    P = cx.P
    I = P.I
    R = lambda c, tb: f"r32_{c}_{tb}"
    X = lambda c, tb: f"xb_{c}_{tb}"
    B = lambda j, tb: f"big{j}_{tb}"
    FT = F // 128
    cnt = 0
    for f0 in range(0, FT, 2):
        nf = min(2, FT - f0)
        wt, kw = ws.next()
        for fi in range(nf):
            f = f0 + fi
            for tb in range(TB):
                ts = TS(tb)
                psa, ka = cx.bank()
                for c in range(8):
                    I("pe", "matmul", psa[:], lhsT=wt[:, c, fi * 256:fi * 256 + 128], rhs=xb[:, c, ts], start=(c == 0), stop=(c == 7),
                      reads=[kw, X(c, tb)], writes=[ka])
                psg, kg = cx.bank()
                for c in range(8):
                    I("pe", "matmul", psg[:], lhsT=wt[:, c, fi * 256 + 128:fi * 256 + 256], rhs=xb[:, c, ts], start=(c == 0), stop=(c == 7),
                      reads=[kw, X(c, tb)], writes=[kg])
                s = sa[cnt % 2]
                ksa = f"sa{cnt % 2}"
                cnt += 1
                I("act", "activation", out=s[:], in_=psa[:], func=AF.Silu, reads=[ka], writes=[ksa])
                if wbc is None:
                    I("dve", "tensor_tensor", out=big[:, f, ts], in0=s[:], in1=psg[:], op=ALU.mult, reads=[kg, ksa], writes=[B(f, tb)])
                else:
                    I("dve", "tensor_tensor", out=s[:], in0=s[:], in1=psg[:], op=ALU.mult, reads=[kg, ksa], writes=[ksa])
                    I("dve", "tensor_tensor", out=big[:, f, ts], in0=s[:], in1=wbc[0][:, ts], op=ALU.mult, reads=[ksa, wbc[1]], writes=[B(f, tb)])
    for m0 in range(0, 8, 2):
        wt, kw = ws.next()
        for mi in range(2):
            m = m0 + mi
            for tb in range(TB):
                ts = TS(tb)
                ps, kps = cx.bank()
                for f in range(FT):
                    I("pe", "matmul", ps[:], lhsT=wt[:, f, mi * 128:(mi + 1) * 128], rhs=big[:, f, ts], start=(f == 0), stop=(f == FT - 1),
                      reads=[kw, B(f, tb)], writes=[kps])
                if first_scale:
                    I("dve", "scalar_tensor_tensor", out=r32[:, m, ts], in0=r32[:, m, ts], scalar=ALPHA, in1=ps[:], op0=ALU.mult, op1=ALU.add,
                      reads=[kps, R(m, tb)], writes=[R(m, tb)])
                else:
                    I("dve", "tensor_tensor", out=r32[:, m, ts], in0=r32[:, m, ts], in1=ps[:], op=ALU.add, reads=[kps, R(m, tb)], writes=[R(m, tb)])


def moe(cx, dr, r32, xb, big, sa, wload):
    P = cx.P
    I = P.I
    sb = cx.sb
    R = lambda c, tb: f"r32_{c}_{tb}"
    NTT = TP // 128
    if not hasattr(cx, "moe_tiles"):
        cx.moe_tiles = dict(
            rt=sb("rt32", [128, 8, 8], F32), lg=sb("lg", [128, NTT, 8], F32), top=sb("top8", [128, 8], F32),
            w0=sb("w0", [128, 1], F32), w1=sb("w1", [128, 1], F32), dw=sb("dw", [128, 1], F32), dm=sb("dm", [128, 1], F32),
            eq=sb("eq", [128, 8], F32), ge=sb("ge", [128, 8], F32), wtok=sb("wtok", [128, NTT, 8], F32),
            wT=sb("wT", [8, TP], F32), sel=sb("sel", [8, 8, 128], F32), ident=sb("ident", [128, 128], F32),
            wbc=[sb(f"wbc{i}", [128, TP], F32) for i in range(2)])
        t = cx.moe_tiles
        P.dma("sp", t["rt"][:], dr["router"].rearrange("(c p) e -> p c e", p=128), writes=["rt32"])
        P.dma("sp", t["sel"][:], dr["sel"], writes=["sel"])
        P.dma("sp", t["ident"][:], dr["ident"], writes=["ident"])
    t = cx.moe_tiles
    rt, lg, top, w0, w1, dw, dm, eq, ge, wtok, wT, sel, ident, wbc = (t[k] for k in
        ("rt", "lg", "top", "w0", "w1", "dw", "dm", "eq", "ge", "wtok", "wT", "sel", "ident", "wbc"))
    psr, kr = cx.bank()
    for tt in range(NTT):
        for c in range(8):
            I("pe", "matmul", psr[:, tt * 8:(tt + 1) * 8], lhsT=r32[:, c, tt * 128:(tt + 1) * 128], rhs=rt[:, c, :], start=(c == 0), stop=(c == 7),
              reads=["rt32", R(c, tt // 4)], writes=[kr])
    I("dve", "tensor_copy", out=lg[:].rearrange("p a b -> p (a b)"), in_=psr[:, 0:NTT * 8], reads=[kr], writes=["lg"])
    psts = [cx.bank() for _ in range(TB)]
    for tt in range(NTT):
        pst, kt = psts[tt // 4]
        I("dve", "max", out=top[:], in_=lg[:, tt, :], reads=["lg"], writes=["top8"])
        I("dve", "tensor_tensor", out=dm[:], in0=top[:, 0:1], in1=top[:, 1:2], op=ALU.subtract, reads=["top8"], writes=["dm"])
        I("act", "activation", out=w0[:], in_=dm[:], func=AF.Sigmoid, reads=["dm"], writes=["w0"])
        I("dve", "tensor_scalar", out=w1[:], in0=w0[:], scalar1=-1.0, scalar2=1.0, op0=ALU.mult, op1=ALU.add, reads=["w0"], writes=["w1"])
        I("dve", "tensor_tensor", out=dw[:], in0=w0[:], in1=w1[:], op=ALU.subtract, reads=["w0", "w1"], writes=["dw"])
        I("dve", "tensor_scalar", out=eq[:], in0=lg[:, tt, :], scalar1=top[:, 0:1], scalar2=None, op0=ALU.is_equal, reads=["lg", "top8"], writes=["eq"])
        I("dve", "tensor_scalar", out=ge[:], in0=lg[:, tt, :], scalar1=top[:, 1:2], scalar2=None, op0=ALU.is_ge, reads=["lg", "top8"], writes=["ge"])
        I("dve", "tensor_scalar", out=eq[:], in0=eq[:], scalar1=dw[:, 0:1], scalar2=w1[:, 0:1], op0=ALU.mult, op1=ALU.add,
          reads=["eq", "dw", "w1"], writes=["eq"])
        I("dve", "tensor_tensor", out=wtok[:, tt, :], in0=eq[:], in1=ge[:], op=ALU.mult, reads=["eq", "ge"], writes=["wtok"])
        I("pe", "transpose", out=pst[0:8, (tt % 4) * 128:(tt % 4 + 1) * 128], in_=wtok[:, tt, :], identity=ident[:], reads=["wtok", "ident"], writes=[kt])
    for tb in range(TB):
        I("dve", "tensor_copy", out=wT[:, TS(tb)], in_=psts[tb][0][0:8, :], reads=[psts[tb][1]], writes=["wT"])
    for c in range(8):
        for tb in range(TB):
            I("pool", "tensor_scalar", out=r32[:, c, TS(tb)], in0=r32[:, c, TS(tb)], scalar1=ALPHA, scalar2=None, op0=ALU.mult,
              reads=[R(c, tb)], writes=[R(c, tb)])
    plan = []
    for ex in range(NEXP):
        plan += ffn_plan(dr["w13"][ex], dr["w2"][ex], F_EXP)
    ws = WStream(wload, plan)
    for ex in range(NEXP):
        wb_ = wbc[ex % 2]
        kwb = f"wbc{ex % 2}"
        for tb in range(TB):
            ps, kps = cx.bank()
            I("pe", "matmul", ps[:], lhsT=sel[:, ex, :], rhs=wT[:, TS(tb)], start=True, stop=True, reads=["sel", "wT"], writes=[kps])
            I("act", "activation", out=wb_[:, TS(tb)], in_=ps[:], func=AF.Copy, reads=[kps], writes=[kwb])
        ffn_expert(cx, ws, F_EXP, r32, xb, big, sa, (wb_, kwb), False)


EPS = 1e-5
C_CQ, C_CKV, C_KR, C_KRS = 0, 256, 384, 416
C_SWQ, C_SWK, C_SWV = 448, 512, 576
C_HQ, C_HF, C_HG, C_HI = 640, 704, 768, 832
C_SBQ, C_SBK, C_SBV = 896, 960, 1024
NCOL = 1088


_UID = globals().get('_UID', [0])


class Ctx1:
    def __init__(self, nc, P, st):
        self.nc, self.P, self.st = nc, P, st
        _UID[0] += 1
        self.uid = _UID[0]
        self.banks = [st.enter_context(nc.psum_tensor(f"u{_UID[0]}_ps{i}", [128, 512], F32)) for i in range(8)]
        self.free = list(range(8))
        self.rr = 0

    def sb(self, name, shape, dt):
        return self.st.enter_context(self.nc.sbuf_tensor(f"u{self.uid}_sb_" + name, shape, dt))

    def bank(self):
        i = self.free[self.rr % len(self.free)]
        self.rr += 1
        return self.banks[i], f"ps{i}"

    def hold(self):
        i = self.free.pop(self.rr % len(self.free))
        return self.banks[i], f"ps{i}", i

    def release(self, i):
        self.free.append(i)
        self.free.sort()


def phase1(cx, dr, S, layer, do=("mla", "swa", "hgrn", "sb")):
    nc, P = cx.nc, cx.P
    I = P.I
    sb = cx.sb
    NB = S // 512
    W = sb("W", [128, 8, NCOL], BF16)
    P.dma("pool", W[:], dr["wh"].rearrange("(c p) n -> p c n", p=128), writes=["W"])
    xbuf = [sb(f"xbuf{i}", [128, 8, 512], BF16) for i in range(2)]
    ones = sb("ones", [128, 128], BF16)
    I("dve", "memset", ones[:], 1.0, writes=["ones"])
    masks = sb("masks", [128, 8, 512], BF16)
    P.dma("sp", masks[:], dr["masks"], writes=["masks"])
    cx.xcnt = 0

    cx.xpending = {}

    def xload(tb):
        def issue(t):
            i = cx.xcnt % 2
            cx.xcnt += 1
            for csl, src in dr["xsrc"](t):
                P.dma("pool", xbuf[i][:, csl, :], src, writes=[f"xbuf{i}"])
            return xbuf[i], f"xbuf{i}"
        if tb not in cx.xpending:
            cx.xpending[tb] = issue(tb)
        r = cx.xpending.pop(tb)
        if tb + 1 < NB and (tb + 1) not in cx.xpending:
            cx.xpending[tb + 1] = issue(tb + 1)
        return r

    def proj_fm(xb_, kx, col0, M, out_ps, kps, tslice=slice(0, 512)):
        for c in range(8):
            I("pe", "matmul", out_ps, lhsT=W[:, c, col0:col0 + M], rhs=xb_[:, c, tslice], start=(c == 0), stop=(c == 7), reads=["W", kx], writes=[kps])

    def proj_tm(xb_, kx, col0, N, out_ps, kps, tok0, ntok):
        for c in range(8):
            I("pe", "matmul", out_ps, lhsT=xb_[:, c, tok0:tok0 + ntok], rhs=W[:, c, col0:col0 + N], start=(c == 0), stop=(c == 7), reads=["W", kx], writes=[kps])

    QT = sb("QT", [128, S], BF16)
    KT = sb("KT", [128, S], BF16)
    Vm = sb("Vm", [128, S // 128, 64], BF16)
    PTn = 4
    PT = [sb(f"PT{i}", [128, 512], BF16) for i in range(PTn)]
    rden = sb("rden", [64, 512], F32)
    yout = [sb(f"yout{i}", [64, 512], BF16) for i in range(2)]
    tA = [sb(f"tA{i}", [128, 512], F32) for i in range(4)]
    tB = [sb(f"tB{i}", [128, 512], BF16) for i in range(4)]
    cx.ycnt = 0

    def ystore(branch, qb, src_ps, kps, scale_ap, kscale):
        i = cx.ycnt % 2
        cx.ycnt += 1
        I("dve", "tensor_tensor", out=yout[i][:], in0=src_ps, in1=scale_ap, op=ALU.mult, reads=[kps, kscale], writes=[f"yout{i}"])
        P.dma("sp", dr["ydst"](branch, qb), yout[i][:], reads=[f"yout{i}"], writes=["ybr"])

    if "mla" in do:
        wuq = sb("wuq", [128, 2, 128], BF16)
        wukv = sb("wukv", [128, 128], BF16)
        P.dma("pool", wuq[:], dr["wuq"].rearrange("(c p) n -> p c n", p=128), writes=["wuq"])
        P.dma("pool", wukv[:], dr["wukv"], writes=["wukv"])
        qn = sb("qn", [128, 2], F32)
        kvn = sb("kvn", [128, 1], F32)
        rc = sb("ropec", [128, 2], F32)
        P.dma("sp", qn[:], dr["qnorm"], writes=["qn"])
        P.dma("sp", kvn[:], dr["kvnorm"], writes=["kvn"])
        P.dma("sp", rc[:], dr["ropec"], writes=["ropec"])
        posi = sb("posi", [128, 512], I32)
        cqn = sb("cqn", [128, 2, 512], BF16)
        ckvn = sb("ckvn", [128, 512], BF16)
        RS = slice(64, 96)
        for tb in range(NB):
            ts = slice(tb * 512, (tb + 1) * 512)
            xb_, kx = xload(tb)
            P.dma("sp", posi[RS, :], dr["posb"][:, ts], writes=["posi"])
            tt, tf = tA[0], tA[1]
            I("dve", "tensor_copy", out=tt[RS, :], in_=posi[RS, :], reads=["posi"], writes=["tA0"])
            I("dve", "tensor_scalar", out=tt[RS, :], in0=tt[RS, :], scalar1=rc[RS, 0:1], scalar2=None, op0=ALU.mult, reads=["tA0", "ropec"], writes=["tA0"])
            sincos = []
            for which, off in (("sin", 0.0), ("cos", 0.25)):
                dst = tA[2] if which == "sin" else tA[3]
                kd = "tA2" if which == "sin" else "tA3"
                I("dve", "tensor_scalar", out=dst[RS, :], in0=tt[RS, :], scalar1=off, scalar2=None, op0=ALU.add, reads=["tA0"], writes=[kd])
                I("dve", "tensor_copy", out=posi[RS, :], in_=dst[RS, :], reads=[kd], writes=["posi"])
                I("dve", "tensor_copy", out=tf[RS, :], in_=posi[RS, :], reads=["posi"], writes=["tA1"])
                I("dve", "tensor_tensor", out=dst[RS, :], in0=dst[RS, :], in1=tf[RS, :], op=ALU.subtract, reads=[kd, "tA1"], writes=[kd])
                I("dve", "tensor_scalar", out=tf[RS, :], in0=dst[RS, :], scalar1=0.5, scalar2=None, op0=ALU.is_gt, reads=[kd], writes=["tA1"])
                I("dve", "tensor_tensor", out=dst[RS, :], in0=dst[RS, :], in1=tf[RS, :], op=ALU.subtract, reads=[kd, "tA1"], writes=[kd])
                I("dve", "tensor_scalar", out=tf[RS, :], in0=dst[RS, :], scalar1=-0.5, scalar2=None, op0=ALU.is_lt, reads=[kd], writes=["tA1"])
                I("dve", "tensor_tensor", out=dst[RS, :], in0=dst[RS, :], in1=tf[RS, :], op=ALU.add, reads=[kd, "tA1"], writes=[kd])
                I("act", "activation", out=dst[RS, :], in_=dst[RS, :], func=AF.Sin, scale=2.0 * math.pi, reads=[kd], writes=[kd])
            sin_t, cos_t = tA[2], tA[3]
            pcq = [cx.bank() for _ in range(2)]
            for j in range(2):
                proj_fm(xb_, kx, C_CQ + j * 128, 128, pcq[j][0][:], pcq[j][1])
                I("act", "activation", out=tB[2 + j][:], in_=pcq[j][0][:], func=AF.Square, reads=[pcq[j][1]], writes=[f"tB{2 + j}"])
            pss, kss = cx.bank()
            for j in range(2):
                I("pe", "matmul", pss[:], lhsT=ones[:], rhs=tB[2 + j][:], start=(j == 0), stop=(j == 1), reads=["ones", f"tB{2 + j}"], writes=[kss])
            rq = tA[1]
            I("act", "activation", out=rq[:], in_=pss[:], func=AF.Ln, scale=1.0 / 256, bias=EPS, reads=[kss], writes=["tA1"])
            I("act", "activation", out=rq[:], in_=rq[:], func=AF.Exp, scale=-0.5, reads=["tA1"], writes=["tA1"])
            for j in range(2):
                I("dve", "scalar_tensor_tensor", out=cqn[:, j, :], in0=pcq[j][0][:], scalar=qn[:, j:j + 1], in1=rq[:], op0=ALU.mult, op1=ALU.mult,
                  reads=[pcq[j][1], "qn", "tA1"], writes=[f"cqn{j}"])
            pq, kq = cx.bank()
            for j in range(2):
                I("pe", "matmul", pq[0:96, :], lhsT=wuq[:, j, 0:96], rhs=cqn[:, j, :], start=(j == 0), stop=(j == 1), reads=["wuq", f"cqn{j}"], writes=[kq])
            pq2, kq2 = cx.bank()
            for j in range(2):
                I("pe", "matmul", pq2[64:96, :], lhsT=wuq[:, j, 96:128], rhs=cqn[:, j, :], start=(j == 0), stop=(j == 1), reads=["wuq", f"cqn{j}"], writes=[kq2])
            I("act", "activation", out=QT[0:64, ts], in_=pq[0:64, :], func=AF.Copy, reads=[kq], writes=[f"QT{tb}"])
            I("dve", "tensor_tensor", out=tt[RS, :], in0=pq[RS, :], in1=cos_t[RS, :], op=ALU.mult, reads=[kq, "tA3"], writes=["tA0"])
            I("dve", "scalar_tensor_tensor", out=tf[RS, :], in0=pq2[RS, :], scalar=rc[RS, 1:2], in1=sin_t[RS, :], op0=ALU.mult, op1=ALU.mult,
              reads=[kq2, "ropec", "tA2"], writes=["tA1"])
            I("pool", "tensor_tensor", out=QT[RS, ts], in0=tt[RS, :], in1=tf[RS, :], op=ALU.add, reads=["tA0", "tA1"], writes=[f"QT{tb}"])
            pkv, kkv = cx.bank()
            proj_fm(xb_, kx, C_CKV, 128, pkv[:], kkv)
            I("act", "activation", out=tB[2][:], in_=pkv[:], func=AF.Square, reads=[kkv], writes=["tB2"])
            pss2, kss2 = cx.bank()
            I("pe", "matmul", pss2[:], lhsT=ones[:], rhs=tB[2][:], start=True, stop=True, reads=["ones", "tB2"], writes=[kss2])
            I("act", "activation", out=rq[:], in_=pss2[:], func=AF.Ln, scale=1.0 / 128, bias=EPS, reads=[kss2], writes=["tA1"])
            I("act", "activation", out=rq[:], in_=rq[:], func=AF.Exp, scale=-0.5, reads=["tA1"], writes=["tA1"])
            I("dve", "scalar_tensor_tensor", out=ckvn[:], in0=pkv[:], scalar=kvn[:, 0:1], in1=rq[:], op0=ALU.mult, op1=ALU.mult,
              reads=[kkv, "kvn", "tA1"], writes=["ckvn"])
            pk, kk = cx.bank()
            I("pe", "matmul", pk[0:64, :], lhsT=wukv[:, 0:64], rhs=ckvn[:], start=True, stop=True, reads=["wukv", "ckvn"], writes=[kk])
            I("act", "activation", out=KT[0:64, ts], in_=pk[0:64, :], func=AF.Copy, reads=[kk], writes=[f"KT{tb}"])
            pkr, kkr = cx.bank()
            proj_fm(xb_, kx, C_KR, 32, pkr[RS, :], kkr)
            pkr2, kkr2 = cx.bank()
            proj_fm(xb_, kx, C_KRS, 32, pkr2[RS, :], kkr2)
            I("dve", "tensor_tensor", out=tt[RS, :], in0=pkr[RS, :], in1=cos_t[RS, :], op=ALU.mult, reads=[kkr, "tA3"], writes=["tA0"])
            I("dve", "scalar_tensor_tensor", out=tf[RS, :], in0=pkr2[RS, :], scalar=rc[RS, 1:2], in1=sin_t[RS, :], op0=ALU.mult, op1=ALU.mult,
              reads=[kkr2, "ropec", "tA2"], writes=["tA1"])
            I("pool", "tensor_tensor", out=KT[RS, ts], in0=tt[RS, :], in1=tf[RS, :], op=ALU.add, reads=["tA0", "tA1"], writes=[f"KT{tb}"])
            pv, kv_ = cx.bank()
            for sbk in range(4):
                I("pe", "matmul", pv[:, sbk * 64:(sbk + 1) * 64], lhsT=ckvn[:, sbk * 128:(sbk + 1) * 128], rhs=wukv[:, 64:128], start=True, stop=True,
                  reads=["wukv", "ckvn"], writes=[kv_])
            I("act", "activation", out=Vm[:, tb * 4:(tb + 1) * 4, :], in_=pv[:, 0:256].rearrange("p (a b) -> p a b", a=4), func=AF.Copy,
              reads=[kv_], writes=[f"Vm{tb}"])
        scale = 96 ** -0.5
        for qb in range(NB):
            pso, kpo, io = cx.hold()
            psd, kpd, id_ = cx.hold()
            qs = slice(qb * 512, (qb + 1) * 512)
            nt = 4 * (qb + 1)

            def stA(t):
                ps, kps = cx.bank()
                I("pe", "matmul", ps[:], lhsT=KT[0:96, t * 128:(t + 1) * 128], rhs=QT[0:96, qs], start=True, stop=True,
                  reads=[f"KT{t // 4}", f"QT{qb}"], writes=[kps])
                p = PT[t % PTn]
                I("act", "activation", out=p[:], in_=ps[:], func=AF.Exp, scale=scale, reads=[kps], writes=[f"PT{t % PTn}"])
                if t >= 4 * qb:
                    I("pool", "tensor_tensor", out=p[:], in0=p[:], in1=masks[:, t - 4 * qb, :], op=ALU.mult, reads=[f"PT{t % PTn}", "masks"], writes=[f"PT{t % PTn}"])

            def stB(t):
                p = PT[t % PTn]
                I("pe", "matmul", pso[0:64, :], lhsT=Vm[:, t, :], rhs=p[:], start=(t == 0), stop=(t == nt - 1), reads=[f"Vm{t // 4}", f"PT{t % PTn}"], writes=[kpo])
                I("pe", "matmul", psd[0:64, :], lhsT=ones[:, 0:64], rhs=p[:], start=(t == 0), stop=(t == nt - 1), reads=["ones", f"PT{t % PTn}"], writes=[kpd])

            for s_ in range(nt + 2):
                if s_ < nt:
                    stA(s_)
                if 0 <= s_ - 2 < nt:
                    stB(s_ - 2)
            I("dve", "reciprocal", out=rden[:], in_=psd[0:64, :], reads=[kpd], writes=["rden"])
            ystore(0, qb, pso[0:64, :], kpo, rden[:], "rden")
            cx.release(io)
            cx.release(id_)

    if "sb" in do:
        tri = sb("tri", [128, 2, 128], BF16)
        P.dma("sp", tri[:], dr["tri"], writes=["tri"])
        for tb in range(NB):
            ts = slice(tb * 512, (tb + 1) * 512)
            xb_, kx = xload(tb)
            pq, kq = cx.bank()
            proj_fm(xb_, kx, C_SBQ, 64, pq[0:64, :], kq)
            I("act", "activation", out=QT[0:64, ts], in_=pq[0:64, :], func=AF.Copy, scale=0.125, reads=[kq], writes=[f"QT{tb}"])
            pk, kk = cx.bank()
            proj_fm(xb_, kx, C_SBK, 64, pk[0:64, :], kk)
            I("act", "activation", out=KT[0:64, ts], in_=pk[0:64, :], func=AF.Copy, reads=[kk], writes=[f"KT{tb}"])
            pv, kv_ = cx.bank()
            for sbk in range(4):
                proj_tm(xb_, kx, C_SBV, 64, pv[:, sbk * 64:(sbk + 1) * 64], kv_, sbk * 128, 128)
            I("act", "activation", out=Vm[:, tb * 4:(tb + 1) * 4, :], in_=pv[:, 0:256].rearrange("p (a b) -> p a b", a=4), func=AF.Copy,
              reads=[kv_], writes=[f"Vm{tb}"])
        S32 = sb("S32", [128, 512], F32)
        carry = [sb(f"carry{i}", [128, 512], BF16) for i in range(3)]
        spb = [sb(f"spb{i}", [128, 512], BF16) for i in range(3)]
        AT = [sb(f"AT{i}", [128, 512], BF16) for i in range(3)]
        for qb in range(NB):
            pso, kpo, io = cx.hold()
            qs = slice(qb * 512, (qb + 1) * 512)
            nt = 4 * (qb + 1)
            zb = {}

            def kb_of(t):
                return 4 * qb + 3 - t

            def stA(t):
                kb = kb_of(t)
                ps, kps = cx.bank()
                zb[t] = (ps, kps)
                I("pe", "matmul", ps[:], lhsT=KT[0:64, kb * 128:(kb + 1) * 128], rhs=QT[0:64, qs], start=True, stop=True, skip_group_check=True,
                  reads=[f"KT{kb // 4}", f"QT{qb}"], writes=[kps])
                e1 = tA[t % 2]
                I("act", "activation", out=e1[:], in_=ps[:], func=AF.Exp, reads=[kps], writes=[f"tA{t % 2}"])
                sp_ = spb[t % 3]
                I("act", "activation", out=sp_[:], in_=e1[:], func=AF.Ln, bias=1.0, reads=[f"tA{t % 2}"], writes=[f"spb{t % 3}"])
                if t < 4:
                    I("pool", "tensor_tensor", out=sp_[:], in0=sp_[:], in1=masks[:, 4 + (3 - t), :], op=ALU.mult, reads=[f"spb{t % 3}", "masks"], writes=[f"spb{t % 3}"])
                if t == 0:
                    I("dve", "tensor_copy", out=S32[:], in_=sp_[:], reads=[f"spb{t % 3}"], writes=["S32"])
                else:
                    I("dve", "tensor_tensor", out=S32[:], in0=S32[:], in1=sp_[:], op=ALU.add, reads=["S32", f"spb{t % 3}"], writes=["S32"])
                if t + 1 < nt:
                    I("pool", "tensor_copy", out=carry[(t + 1) % 3][:], in_=S32[:], reads=["S32"], writes=[f"carry{(t + 1) % 3}"])

            def stB(t):
                ps, kps = zb[t]
                I("pe", "matmul", ps[:], lhsT=tri[:, 0, :], rhs=spb[t % 3][:], start=False, stop=(t == 0), skip_group_check=True,
                  reads=["tri", f"spb{t % 3}"], writes=[kps])
                if t > 0:
                    I("pe", "matmul", ps[:], lhsT=tri[:, 1, :], rhs=carry[t % 3][:], start=False, stop=True, skip_group_check=True,
                      reads=["tri", f"carry{t % 3}"], writes=[kps])
                a = AT[t % 3]
                I("act", "activation", out=a[:], in_=ps[:], func=AF.Exp, reads=[kps], writes=[f"AT{t % 3}"])
                if t < 4:
                    I("pool", "tensor_tensor", out=a[:], in0=a[:], in1=masks[:, 4 + (3 - t), :], op=ALU.mult, reads=[f"AT{t % 3}", "masks"], writes=[f"AT{t % 3}"])

            def stC(t):
                kb = kb_of(t)
                I("pe", "matmul", pso[0:64, :], lhsT=Vm[:, kb, :], rhs=AT[t % 3][:], start=(t == 0), stop=(t == nt - 1), reads=[f"Vm{kb // 4}", f"AT{t % 3}"], writes=[kpo])

            for s_ in range(nt + 2):
                if s_ < nt:
                    stA(s_)
                if 0 <= s_ - 1 < nt:
                    stB(s_ - 1)
                if 0 <= s_ - 2 < nt:
                    stC(s_ - 2)
            i = cx.ycnt % 2
            cx.ycnt += 1
            I("act", "activation", out=yout[i][:], in_=pso[0:64, :], func=AF.Copy, reads=[kpo], writes=[f"yout{i}"])
            P.dma("sp", dr["ydst"](3, qb), yout[i][:], reads=[f"yout{i}"], writes=["ybr"])
            cx.release(io)

    if "swa" in do:
        swc = sb("swc", [128, 34], F32)
        P.dma("sp", swc[:], dr["swc"], writes=["swc"])
        dcur = sb("dcur", [128, 128], F32)
        dprev = sb("dprev", [128, 128], F32)
        P.dma("sp", dcur[:], dr["dist"][0], writes=["dcur"])
        P.dma("sp", dprev[:], dr["dist"][1], writes=["dprev"])
        diff = sb("swdiff", [128, 31], F32)
        I("dve", "tensor_tensor", out=diff[:], in0=swc[:, 1:32], in1=swc[:, 0:31], op=ALU.subtract, reads=["swc"], writes=["swdiff"])
        esink = sb("esink", [128, 1], F32)
        I("act", "activation", out=esink[:], in_=swc[:, 32:33], func=AF.Exp, reads=["swc"], writes=["esink"])
        EB = [sb(f"EB{i}", [128, 512], F32) for i in range(3)]
        acc = tA[0]
        stp = tA[1]
        los = dr["bucket_lo"]
        for which, dt_, kd in ((0, dcur, "dcur"), (1, dprev, "dprev")):
            I("dve", "tensor_scalar", out=acc[:, 0:128], in0=dt_[:], scalar1=0.0, scalar2=swc[:, 0:1], op0=ALU.mult, op1=ALU.add, reads=[kd, "swc"], writes=["tA0"])
            for b in range(1, 32):
                I("dve", "tensor_scalar", out=stp[:, 0:128], in0=dt_[:], scalar1=float(los[b - 1]), scalar2=diff[:, b - 1:b], op0=ALU.is_ge, op1=ALU.mult,
                  reads=[kd, "swdiff"], writes=["tA1"])
                I("dve", "tensor_tensor", out=acc[:, 0:128], in0=acc[:, 0:128], in1=stp[:, 0:128], op=ALU.add, reads=["tA0", "tA1"], writes=["tA0"])
            I("act", "activation", out=acc[:, 0:128], in_=acc[:, 0:128], func=AF.Exp, reads=["tA0"], writes=["tA0"])
            if which == 0:
                I("dve", "tensor_scalar", out=stp[:, 0:128], in0=dt_[:], scalar1=0.0, scalar2=None, op0=ALU.is_ge, reads=[kd], writes=["tA1"])
            else:
                I("dve", "tensor_scalar", out=stp[:, 0:128], in0=dt_[:], scalar1=127.0, scalar2=None, op0=ALU.is_le, reads=[kd], writes=["tA1"])
            for r in range(4):
                I("dve", "tensor_tensor", out=EB[which][:, r * 128:(r + 1) * 128], in0=acc[:, 0:128], in1=stp[:, 0:128], op=ALU.mult,
                  reads=["tA0", "tA1"], writes=[f"EB{which}"])
        I("pool", "tensor_copy", out=EB[2][:], in_=EB[1][:], reads=["EB1"], writes=["EB2"])
        I("pool", "memset", EB[2][:, 0:128], 0.0, reads=[], writes=["EB2"])
        for tb in range(NB):
            ts = slice(tb * 512, (tb + 1) * 512)
            xb_, kx = xload(tb)
            pq, kq = cx.bank()
            proj_fm(xb_, kx, C_SWQ, 64, pq[0:64, :], kq)
            I("act", "activation", out=QT[0:64, ts], in_=pq[0:64, :], func=AF.Copy, scale=0.125, reads=[kq], writes=[f"QT{tb}"])
            pk, kk = cx.bank()
            proj_fm(xb_, kx, C_SWK, 64, pk[0:64, :], kk)
            I("act", "activation", out=KT[0:64, ts], in_=pk[0:64, :], func=AF.Copy, reads=[kk], writes=[f"KT{tb}"])
            pv, kv_ = cx.bank()
            for sbk in range(4):
                proj_tm(xb_, kx, C_SWV, 64, pv[:, sbk * 64:(sbk + 1) * 64], kv_, sbk * 128, 128)
            I("act", "activation", out=Vm[:, tb * 4:(tb + 1) * 4, :], in_=pv[:, 0:256].rearrange("p (a b) -> p a b", a=4), func=AF.Copy,
              reads=[kv_], writes=[f"Vm{tb}"])
        for g in range(NB):
            qs = slice(g * 512, (g + 1) * 512)
            pc, kc = cx.bank()
            pp, kp = cx.bank()
            for j in range(4):
                blk = 4 * g + j
                I("pe", "matmul", pc[:, j * 128:(j + 1) * 128], lhsT=KT[0:64, blk * 128:(blk + 1) * 128], rhs=QT[0:64, blk * 128:(blk + 1) * 128],
                  start=True, stop=True, reads=[f"KT{g}", f"QT{g}"], writes=[kc])
                pb = max(blk - 1, 0)
                I("pe", "matmul", pp[:, j * 128:(j + 1) * 128], lhsT=KT[0:64, pb * 128:(pb + 1) * 128], rhs=QT[0:64, blk * 128:(blk + 1) * 128],
                  start=True, stop=True, reads=[f"KT{pb // 4}", f"QT{g}"], writes=[kp])
            ec, ep = tA[2], tA[3]
            I("act", "activation", out=ec[:], in_=pc[:], func=AF.Exp, reads=[kc], writes=["tA2"])
            I("act", "activation", out=ep[:], in_=pp[:], func=AF.Exp, reads=[kp], writes=["tA3"])
            pcb, ppb = tB[0], tB[1]
            I("dve", "tensor_tensor", out=pcb[:], in0=ec[:], in1=EB[0][:], op=ALU.mult, reads=["tA2", "EB0"], writes=["tB0"])
            ebp = 2 if g == 0 else 1
            I("pool", "tensor_tensor", out=ppb[:], in0=ep[:], in1=EB[ebp][:], op=ALU.mult, reads=["tA3", f"EB{ebp}"], writes=["tB1"])
            pso, kpo = cx.bank()
            psd, kpd = cx.bank()
            for j in range(4):
                blk = 4 * g + j
                pb = max(blk - 1, 0)
                cs = slice(j * 128, (j + 1) * 128)
                I("pe", "matmul", pso[0:64, cs], lhsT=Vm[:, blk, :], rhs=pcb[:, cs], start=True, stop=False, reads=[f"Vm{g}", "tB0"], writes=[kpo])
                I("pe", "matmul", pso[0:64, cs], lhsT=Vm[:, pb, :], rhs=ppb[:, cs], start=False, stop=True, reads=[f"Vm{pb // 4}", "tB1"], writes=[kpo])
                I("pe", "matmul", psd[0:64, cs], lhsT=ones[:, 0:64], rhs=pcb[:, cs], start=True, stop=False, reads=["ones", "tB0"], writes=[kpd])
                I("pe", "matmul", psd[0:64, cs], lhsT=ones[:, 0:64], rhs=ppb[:, cs], start=False, stop=True, reads=["ones", "tB1"], writes=[kpd])
            I("dve", "tensor_scalar", out=rden[:], in0=psd[0:64, :], scalar1=esink[0:64, 0:1], scalar2=None, op0=ALU.add, reads=[kpd, "esink"], writes=["rden"])
            I("dve", "reciprocal", out=rden[:], in_=rden[:], reads=["rden"], writes=["rden"])
            ystore(1, g, pso[0:64, :], kpo, rden[:], "rden")

    if "hgrn" in do:
        hgrn(cx, dr, S, layer, W, xload, proj_fm, proj_tm, ones, tA, tB, yout)


def hgrn(cx, dr, S, layer, W, xload, proj_fm, proj_tm, ones, tA, tB, yout):
    import os
    HG = int(os.environ.get("HG_STOP", "99"))
    nc, P = cx.nc, cx.P
    I = P.I
    sb = cx.sb
    SEG = min(1024, S)
    NSEG = S // SEG
    NBS = SEG // 512
    NCS = SEG // 32
    hc = sb("hgc", [64, 4], F32)
    P.dma("sp", hc[:], dr["hgc"], writes=["hgc"])
    ident = sb("identh", [64, 64], F32)
    P.dma("sp", ident[:], dr["ident64"], writes=["identh"])
    rmask = sb("rmask", [64, 512], F32)
    P.dma("sp", rmask[:], dr["rmask"], writes=["rmask"])
    m64 = sb("m64", [64, 512], F32)
    P.dma("sp", m64[:], dr["m64"], writes=["m64"])
    lb = sb("lb", [64, 1], F32)
    oml = sb("oml", [64, 1], F32)
    e2 = sb("e2", [64, 2], F32)
    I("act", "activation", out=e2[:], in_=hc[:, 0:2], func=AF.Exp, reads=["hgc"], writes=["e2"])
    I("dve", "tensor_tensor", out=lb[:], in0=e2[:, 0:1], in1=e2[:, 1:2], op=ALU.add, reads=["e2"], writes=["lb"])
    I("dve", "reciprocal", out=lb[:], in_=lb[:], reads=["lb"], writes=["lb"])
    I("dve", "tensor_tensor", out=lb[:], in0=lb[:], in1=e2[:, 1:2], op=ALU.mult, reads=["lb", "e2"], writes=["lb"])
    I("dve", "tensor_scalar", out=lb[:], in0=lb[:], scalar1=float(layer), scalar2=None, op0=ALU.mult, reads=["lb"], writes=["lb"])
    I("dve", "tensor_scalar", out=oml[:], in0=lb[:], scalar1=-1.0, scalar2=1.0, op0=ALU.mult, op1=ALU.add, reads=["lb"], writes=["oml"])
    QD = sb("QD", [64, SEG], BF16)
    KD = sb("KD", [64, SEG], BF16)
    SG = sb("SG", [64, SEG], BF16)
    KEt = sb("KEt", [64, 2, SEG // 64, 64], BF16)
    I("pool", "memset", KEt[:], 0.0, writes=[f"KEt{bi}" for bi in range(NBS)])
    Vt = sb("Vt", [64, SEG // 64, 64], BF16)
    Us = sb("Us", [64, NCS, 64], BF16)
    Ds = sb("Ds", [64, NCS], F32)
    Sst = sb("Sst", [64, NCS + 1, 64], F32)
    Sb = sb("Sb", [64, NCS, 64], BF16)
    attm = sb("attm", [64, 512], BF16)
    hbufs = [[sb(f"h{n}{p}", [64, 512], F32) for n in ("q", "f", "l", "k", "b", "e", "ke")] for p in range(2)]
    rs = sb("hrs", [64, 512], F32)
    sqb = sb("hsq", [64, 512], BF16)
    I("dve", "memset", Sst[:, 0, :], 0.0, writes=["Sst"])
    for sg in range(NSEG):
        for bi in range(NBS):
            tb = sg * NBS + bi
            ls = slice(bi * 512, (bi + 1) * 512)
            xb_, kx = xload(tb)
            hq, hf, hl, hk, hb, he, ke = hbufs[tb % 2]
            Kp = lambda n, p=tb % 2: f"{n}{p}"
            pq, kq = cx.bank()
            proj_fm(xb_, kx, C_HQ, 64, pq[0:64, :], kq)
            pf, kf_ = cx.bank()
            proj_fm(xb_, kx, C_HF, 64, pf[0:64, :], kf_)
            pg, kg = cx.bank()
            proj_fm(xb_, kx, C_HG, 64, pg[0:64, :], kg)
            pv, kv_ = cx.bank()
            for pc_ in range(8):
                proj_tm(xb_, kx, C_HI, 64, pv[0:64, pc_ * 64:(pc_ + 1) * 64], kv_, pc_ * 64, 64)
            I("act", "activation", out=Vt[:, bi * 8:(bi + 1) * 8, :], in_=pv[0:64, :].rearrange("p (a b) -> p a b", a=8), func=AF.Copy, reads=[kv_], writes=[f"Vt{bi}"])
            I("act", "activation", out=hq[:], in_=pq[0:64, :], func=AF.Silu, reads=[kq], writes=[Kp("hq")])
            I("act", "activation", out=SG[:, ls], in_=pg[0:64, :], func=AF.Silu, reads=[kg], writes=[f"SG{bi}"])
            I("act", "activation", out=hf[:], in_=pf[0:64, :], func=AF.Sigmoid, reads=[kf_], writes=[Kp("hf")])
            I("dve", "tensor_scalar", out=hf[:], in0=hf[:], scalar1=oml[:, 0:1], scalar2=lb[:, 0:1], op0=ALU.mult, op1=ALU.add, reads=[Kp("hf"), "oml", "lb"], writes=[Kp("hf")])
            I("act", "activation", out=hl[:], in_=hf[:], func=AF.Ln, reads=[Kp("hf")], writes=[Kp("hl")])
            I("pool", "tensor_scalar", out=hk[:], in0=hf[:], scalar1=-1.0, scalar2=1.0, op0=ALU.mult, op1=ALU.add, reads=[Kp("hf")], writes=[Kp("hk")])
            if HG < 1:
                continue
            I("dve", "tensor_tensor_scan", out=hb[:], data0=rmask[:], data1=hl[:], initial=0.0, op0=ALU.mult, op1=ALU.add, reads=["rmask", Kp("hl")], writes=[Kp("hb")])
            I("act", "activation", out=he[:], in_=hb[:], func=AF.Exp, reads=[Kp("hb")], writes=[Kp("he")])
            I("dve", "tensor_tensor", out=QD[:, ls], in0=hq[:], in1=he[:], op=ALU.mult, reads=[Kp("hq"), Kp("he")], writes=[f"QD{bi}"])
            I("act", "activation", out=he[:], in_=hb[:], func=AF.Exp, scale=-1.0, reads=[Kp("hb")], writes=[Kp("he")])
            I("pool", "tensor_tensor", out=ke[:], in0=hk[:], in1=he[:], op=ALU.mult, reads=[Kp("hk"), Kp("he")], writes=[Kp("hke")])
            I("pool", "tensor_copy", out=KD[:, ls], in_=ke[:], reads=[Kp("hke")], writes=[f"KD{bi}"])
            if HG < 2:
                continue
            I("act", "activation", out=Ds[:, bi * 16:(bi + 1) * 16], in_=hb[:].rearrange("p (c t) -> p c t", t=32)[:, :, 31], func=AF.Exp,
              reads=[Kp("hb")], writes=[f"Ds{bi}"])
            for c in range(16):
                I("pool", "tensor_scalar", out=ke[:, c * 32:(c + 1) * 32], in0=ke[:, c * 32:(c + 1) * 32], scalar1=Ds[:, bi * 16 + c:bi * 16 + c + 1], scalar2=None,
                  op0=ALU.mult, reads=[Kp("hke"), f"Ds{bi}"], writes=[Kp("hke")])
            if HG < 3:
                continue
            pt, kt = cx.bank()
            for pc_ in range(8):
                I("pe", "transpose", out=pt[0:64, pc_ * 64:(pc_ + 1) * 64], in_=ke[:, pc_ * 64:(pc_ + 1) * 64], identity=ident[:], reads=[Kp("hke"), "identh"], writes=[kt])
            I("dve", "tensor_copy", out=KEt[0:32, 0, bi * 8:(bi + 1) * 8, :], in_=pt[0:32, :].rearrange("p (a b) -> p a b", a=8), reads=[kt], writes=[f"KEt{bi}"])
            I("dve", "tensor_copy", out=KEt[32:64, 1, bi * 8:(bi + 1) * 8, :], in_=pt[32:64, :].rearrange("p (a b) -> p a b", a=8), reads=[kt], writes=[f"KEt{bi}"])
            if HG < 4:
                continue
            pu = [cx.bank() for _ in range(2)]
            for c in range(16):
                pc_, half = c // 2, c % 2
                hs = slice(half * 32, (half + 1) * 32)
                pb_, kb_ = pu[c // 8]
                I("pe", "matmul", pb_[0:64, (c % 8) * 64:(c % 8 + 1) * 64], lhsT=KEt[:, half, bi * 8 + pc_, :], rhs=Vt[:, bi * 8 + pc_, :], start=True, stop=True,
                  reads=[f"KEt{bi}", f"Vt{bi}"], writes=[kb_])
            for k2 in range(2):
                I("act", "activation", out=Us[:, bi * 16 + k2 * 8:bi * 16 + k2 * 8 + 8, :], in_=pu[k2][0][0:64, :].rearrange("p (a b) -> p a b", a=8),
                  func=AF.Copy, reads=[pu[k2][1]], writes=[f"Us{bi}"])
        if HG < 5:
            continue
        allU = [f"Us{bi}" for bi in range(NBS)]
        allD = [f"Ds{bi}" for bi in range(NBS)]
        for v in range(64):
            I("dve", "tensor_tensor_scan", out=Sst[:, 1:NCS + 1, v], data0=Ds[:, 0:NCS], data1=Us[:, :, v], initial=Sst[:, 0, v:v + 1],
              op0=ALU.mult, op1=ALU.add, reads=allU + allD + ["Sst"], writes=["Sst"])
        I("pool", "tensor_copy", out=Sb[:], in_=Sst[:, 0:NCS, :], reads=["Sst"], writes=["Sb"])
        if HG < 6:
            continue
        for bi in range(NBS):
            tb = sg * NBS + bi
            ls = slice(bi * 512, (bi + 1) * 512)
            pa, ka = cx.bank()
            for pc_ in range(8):
                cs = slice(bi * 512 + pc_ * 64, bi * 512 + (pc_ + 1) * 64)
                I("pe", "matmul", pa[0:64, pc_ * 64:(pc_ + 1) * 64], lhsT=KD[:, cs], rhs=QD[:, cs], start=True, stop=True, reads=[f"KD{bi}", f"QD{bi}"], writes=[ka])
            I("dve", "tensor_tensor", out=attm[:], in0=pa[0:64, :], in1=m64[:], op=ALU.mult, reads=[ka, "m64"], writes=["attm"])
            po, ko = cx.bank()
            for pc_ in range(8):
                for half in range(2):
                    c = bi * 16 + pc_ * 2 + half
                    oc = slice(pc_ * 64 + half * 32, pc_ * 64 + half * 32 + 32)
                    c32 = slice(bi * 512 + pc_ * 64 + half * 32, bi * 512 + pc_ * 64 + half * 32 + 32)
                    I("pe", "matmul", po[0:64, oc], lhsT=Vt[:, bi * 8 + pc_, :], rhs=attm[:, oc], start=True, stop=False, reads=[f"Vt{bi}", "attm"], writes=[ko])
                    I("pe", "matmul", po[0:64, oc], lhsT=Sb[:, c, :], rhs=QD[:, c32], start=False, stop=True, reads=["Sb", f"QD{bi}"], writes=[ko])
            I("act", "activation", out=sqb[:], in_=po[0:64, :], func=AF.Square, reads=[ko], writes=["hsq"])
            pn, kn = cx.bank()
            I("pe", "matmul", pn[0:64, :], lhsT=ones[0:64, 0:64], rhs=sqb[:], start=True, stop=True, reads=["ones", "hsq"], writes=[kn])
            I("act", "activation", out=rs[:], in_=pn[0:64, :], func=AF.Ln, scale=1.0 / 64, bias=EPS, reads=[kn], writes=["hrs"])
            I("act", "activation", out=rs[:], in_=rs[:], func=AF.Exp, scale=-0.5, reads=["hrs"], writes=["hrs"])
            I("dve", "tensor_tensor", out=rs[:], in0=po[0:64, :], in1=rs[:], op=ALU.mult, reads=[ko, "hrs"], writes=["hrs"])
            i = cx.ycnt % 2
            cx.ycnt += 1
            I("dve", "scalar_tensor_tensor", out=yout[i][:], in0=rs[:], scalar=hc[:, 2:3], in1=SG[:, ls], op0=ALU.mult, op1=ALU.mult,
              reads=["hrs", "hgc", f"SG{bi}"], writes=[f"yout{i}"])
            P.dma("sp", dr["ydst"](2, tb), yout[i][:], reads=[f"yout{i}"], writes=["ybr"])
        if sg + 1 < NSEG:
            I("dve", "tensor_copy", out=Sst[:, 0, :], in_=Sst[:, NCS, :], reads=["Sst"], writes=["Sst"])


S_FULL = 8192
NTC = 2048
f32 = np.float32

IN_SPLITS = (('mla_cq', 256), ('mla_ckv', 128), ('mla_kr', 32), ('swa_q', 256), ('swa_k', 128), ('swa_v', 128),
             ('hgrn_q', 256), ('hgrn_f', 256), ('hgrn_i', 256), ('hgrn_g', 256), ('sb_q', 256), ('sb_k', 256), ('sb_v', 256), ('gates', 4096))
OFFS = {}
_o = 0
for _n, _w in IN_SPLITS:
    OFFS[_n] = _o
    _o += _w


def p1_consts():
    c = {}
    k = np.arange(128)[:, None]
    q = np.arange(512)[None, :]
    m = np.zeros((128, 8, 512), f32)
    for j in range(4):
        m[:, j, :] = (j * 128 + k) <= q
        m[:, 4 + j, :] = (j * 128 + k) < q
    c["masks"] = m.astype(ml_dtypes.bfloat16)
    tri = np.zeros((128, 2, 128), f32)
    jj = np.arange(128)[:, None]
    kk = np.arange(128)[None, :]
    tri[:, 0, :] = -(jj >= kk).astype(f32)
    tri[:, 1, :] = -1
    c["tri"] = tri.astype(ml_dtypes.bfloat16)
    inv = (10000.0 ** (-np.arange(16, dtype=f32) / 16)).astype(f32)
    rc = np.zeros((128, 2), f32)
    rc[64:96, 0] = np.concatenate([inv, inv]) / f32(2 * np.pi)
    rc[64:80, 1] = -1
    rc[80:96, 1] = 1
    c["ropec"] = rc
    kq = np.arange(128)
    c["dist"] = np.stack([(kq[None, :] - kq[:, None]).astype(f32), (128 + kq[None, :] - kq[:, None]).astype(f32)])
    c["ident64"] = np.eye(64, dtype=f32)
    rm = np.ones((64, 512), f32)
    rm[:, ::32] = 0
    c["rmask"] = rm
    s_ = np.arange(64)[:, None]
    t_ = np.arange(64)[None, :]
    c["m64"] = np.tile(((s_ // 32 == t_ // 32) & (s_ <= t_)).astype(f32), (1, 8))
    return c


def bucket_lo():
    n = np.arange(128)
    nf = np.maximum(n, 1).astype(f32)
    large = 16 + (np.log(nf / f32(16)) / f32(np.log(128 / 16)) * f32(16)).astype(np.int32)
    bucket = np.where(n < 16, n, np.clip(large, 0, 31))
    return [int(np.min(np.nonzero(bucket >= b)[0])) if np.any(bucket >= b) else 100000 for b in range(1, 32)]


def p1_head_inputs(inp, l, b, h):
    w_in = inp["w_in"][l]

    def cols(name, a, b_):
        return w_in[:, OFFS[name] + a: OFFS[name] + b_]
    kr = cols("mla_kr", 0, 32)
    krs = np.concatenate([kr[:, 16:32], kr[:, 0:16]], axis=1)
    g = h // 2
    hs = slice(h * 64, h * 64 + 64)
    wh = np.concatenate([cols("mla_cq", 0, 256), cols("mla_ckv", 0, 128), kr, krs,
                         cols("swa_q", h * 64, h * 64 + 64), cols("swa_k", g * 64, g * 64 + 64), cols("swa_v", g * 64, g * 64 + 64),
                         cols("hgrn_q", h * 64, h * 64 + 64), cols("hgrn_f", h * 64, h * 64 + 64), cols("hgrn_g", h * 64, h * 64 + 64),
                         cols("hgrn_i", h * 64, h * 64 + 64),
                         cols("sb_q", h * 64, h * 64 + 64), cols("sb_k", h * 64, h * 64 + 64), cols("sb_v", h * 64, h * 64 + 64)], axis=1)
    uq = inp["mla_w_uq"][l][:, h * 96:(h + 1) * 96]
    wuq = np.concatenate([uq, uq[:, 80:96], uq[:, 64:80]], axis=1)
    wukv = inp["mla_w_ukv"][l][:, h * 128:(h + 1) * 128]
    swc = np.zeros((128, 34), f32)
    swc[:, 0:32] = inp["rel_bias_table"][:, h][None, :]
    swc[:, 32] = inp["swa_sinks"][l][h]
    hgc = np.zeros((64, 4), f32)
    hgc[:, 0] = inp["hgrn_lb_logits"][0, hs]
    hgc[:, 1] = inp["hgrn_lb_logits"][1, hs]
    hgc[:, 2] = inp["hgrn_norm"][l][hs]
    pos_b = inp["positions"][b]
    return dict(wh=np.ascontiguousarray(wh), wuq=np.ascontiguousarray(wuq), wukv=np.ascontiguousarray(wukv),
                qnorm=np.ascontiguousarray(inp["mla_q_norm"][l].reshape(2, 128).T), kvnorm=np.ascontiguousarray(inp["mla_kv_norm"][l].reshape(128, 1)),
                posb=np.ascontiguousarray(np.broadcast_to(pos_b[None, :], (32, pos_b.shape[0]))).astype(np.int32), swc=swc, hgc=hgc)


def il13(w, F):
    a, g = w[:, :F].reshape(1024, F // 128, 128), w[:, F:].reshape(1024, F // 128, 128)
    return np.ascontiguousarray(np.stack([a, g], axis=2).reshape(1024, 2 * F))


def p2_shared_inputs(inp, l):
    w_in = inp["w_in"][l]
    wg = w_in[:, OFFS["gates"]:OFFS["gates"] + 4096]
    d = dict(wg=np.ascontiguousarray(wg.reshape(1024, 4, 8, 128).transpose(0, 2, 1, 3).reshape(1024, 4096)),
             wbr=np.ascontiguousarray(inp["w_branch"][l].reshape(1024, 1024)), wout=np.ascontiguousarray(inp["w_out"][l]),
             lng=np.ascontiguousarray(inp["ln_g"][l].reshape(3, 8, 128).transpose(2, 0, 1)),
             lnb=np.ascontiguousarray(inp["ln_b"][l].reshape(3, 8, 128).transpose(2, 0, 1)),
             wq=np.ascontiguousarray(inp["xa_wq"][l]), wkv=np.ascontiguousarray(inp["xa_wkv"][l]), wo=np.ascontiguousarray(inp["xa_wo"][l]))
    if l % 2 == 0:
        d.update(w13=il13(inp["ffn_w13"][l // 2], 2816), w2=np.ascontiguousarray(inp["ffn_w2"][l // 2]))
    else:
        sel = np.zeros((8, 8, 128), f32)
        for e in range(8):
            sel[e, e, :] = 1
        d.update(router=np.ascontiguousarray(inp["moe_router"][l // 2]), w13=np.stack([il13(inp["moe_w13"][l // 2][e], 3584) for e in range(8)]),
                 w2=np.ascontiguousarray(inp["moe_w2"][l // 2]), sel=sel, ident=np.eye(128, dtype=f32))
    return d


GROUPS = [[0, 1, 2, 3], [4, 5, 6, 7]]
_NPDT = {"float32": F32, "int32": I32, "bfloat16": BF16}


def core_inputs(inp, consts, c):
    b, q = c // 4, c % 4
    x = inp["x"]
    d = dict(xT0=np.ascontiguousarray(x[b].T), x32=np.ascontiguousarray(x[b, q * NTC:(q + 1) * NTC, :].T),
             memT=np.ascontiguousarray(inp["mem"][b].T))
    sel4 = np.zeros((128, 4), f32)
    sel4[:, q] = 1
    d["sel4"] = sel4
    d.update(consts)
    for l in range(2):
        p1 = p1_head_inputs(inp, l, b, q)
        if l == 1:
            p1.pop("posb")
        for k, v in p1.items():
            d[(f"l{l}_" + k) if k != "posb" else k] = v
    return d


def build_fused(sample, shared_shapes):
    nc = bass.Bass("TRN2", target_bir_lowering=False)
    S = S_FULL
    ext = {}
    for k, v in list(sample.items()) + list(shared_shapes.items()):
        ext[k] = nc.dram_tensor(k, list(v.shape), _NPDT[str(v.dtype)], kind="ExternalInput").ap()
    out = nc.dram_tensor("out", [1024, NTC], F32, kind="ExternalOutput").ap()
    ysrc = [[nc.dram_tensor(f"ysrc{l}_{i}", [256, 512], BF16) for i in range(16)] for l in range(2)]
    ygat = [[nc.dram_tensor(f"ygat{l}_{i}", [1024, 512], BF16) for i in range(16)] for l in range(2)]
    xsrc = [nc.dram_tensor(f"xsrc_{i}", [256, 512], BF16) for i in range(16)]
    xgat = [nc.dram_tensor(f"xgat_{i}", [1024, 512], BF16) for i in range(16)]
    x32mid = nc.dram_tensor("x32mid", [1024, NTC], F32).ap()
    blo = bucket_lo()
    cnames = ("masks", "tri", "ropec", "dist", "ident64", "rmask", "m64")

    with contextlib.ExitStack() as semst:
        def allgather(srcs, dsts, tag):
            cc = semst.enter_context(nc.semaphore(f"cc_{tag}"))
            with nc.Block() as block:
                @block.gpsimd
                def _(g):
                    for s_, d_ in zip(srcs, dsts):
                        g.collective_compute("AllGather", ALU.bypass, replica_groups=GROUPS, ins=[s_.ap().opt()], outs=[d_.ap().opt()]).then_inc(cc, 1)
                    g.wait_ge(cc, len(srcs))
            nc.all_engine_barrier()

        for l in range(2):
            P = Prog(nc)
            with contextlib.ExitStack() as st:
                cx = Ctx1(nc, P, st)
                dr = {k: ext[k] for k in cnames}
                dr["posb"] = ext["posb"]
                for k in ("wh", "wuq", "wukv", "qnorm", "kvnorm", "swc", "hgc"):
                    dr[k] = ext[f"l{l}_{k}"]
                dr["bucket_lo"] = blo
                if l == 0:
                    dr["xsrc"] = lambda tb: [(slice(0, 8), ext["xT0"].rearrange("(c p) t -> p c t", p=128)[:, :, tb * 512:(tb + 1) * 512])]
                else:
                    dr["xsrc"] = lambda tb: [(slice(2 * fc, 2 * fc + 2),
                                              xgat[fc * 4 + tb % 4].ap()[(tb // 4) * 256:(tb // 4 + 1) * 256, :].rearrange("(c p) t -> p c t", p=128))
                                             for fc in range(4)]
                dr["ydst"] = lambda br, qb, l=l: ysrc[l][qb].ap()[br * 64:(br + 1) * 64, :]
                phase1(cx, dr, S, l)
                P.finalize()
                P.emit(sem_stack=semst)
            nc.all_engine_barrier()
            allgather(ysrc[l], ygat[l], f"y{l}")
            P = Prog(nc)
            with contextlib.ExitStack() as st:
                cx = Ctx(nc, P, st)
                dr = dict(memT=ext["memT"], sel4=ext["sel4"])
                p2keys = ["wg", "wbr", "wout", "lng", "lnb", "wq", "wkv", "wo", "w13", "w2"] + (["router", "sel", "ident"] if l % 2 == 1 else [])
                for k in p2keys:
                    dr[k] = ext[f"l{l}_{k}"]
                dr["x32"] = ext["x32"] if l == 0 else x32mid
                dr["xo32"] = x32mid if l == 0 else out
                dr["ygather"] = lambda jq, lb, l=l: ygat[l][jq * 4 + lb].ap()
                if l == 0:
                    dr["xchunk"] = lambda fc, lb: xsrc[fc * 4 + lb].ap()
                phase2(cx, dr, l % 2 == 1, NTC)
                P.finalize()
                P.emit(sem_stack=semst)
            nc.all_engine_barrier()
            if l == 0:
                allgather(xsrc, xgat, "x")
    return nc


def kernel(**inp):
    inp = {k: np.asarray(v) for k, v in inp.items()}
    B, S, Dm = inp["x"].shape
    consts = p1_consts()
    cores = list(range(8))
    shared = {}
    for l in range(2):
        for k, v in p2_shared_inputs(inp, l).items():
            shared[f"l{l}_{k}"] = v
    in_maps = []
    for c in cores:
        d = core_inputs(inp, consts, c)
        if c == 0:
            nc = build_fused(d, shared)
        d.update(shared)
        in_maps.append(d)
    res = run_bass_kernel_spmd(nc, in_maps, core_ids=cores).results
    out = np.empty((B, S, Dm), np.float32)
    for c in cores:
        out[c // 4, (c % 4) * NTC:(c % 4 + 1) * NTC, :] = np.asarray(res[c]["out"]).T
    return out
```
